# Optimizing a Trainium2 kernel written in Bass

```python
import jax, jax.numpy as jnp
from jax import lax
import numpy as np

D_MODEL = 2048
BATCH = 4
SEQ = 2048
DEPTH = 2

HEAD_DIM = 64
N_MIXERS = 4
GROUP_WIDTH = D_MODEL // N_MIXERS
N_GROUP_HEADS = GROUP_WIDTH // HEAD_DIM
D_MIX = N_MIXERS * GROUP_WIDTH
FNET_W = GROUP_WIDTH
GMLP_W = GROUP_WIDTH
CHUNK = 128
SWA_HEADS = N_GROUP_HEADS
SWA_KV_HEADS = 2
SWA_HALF_WINDOW = 128
DIL_HEADS = N_GROUP_HEADS
DIL_CONFIGS = ((128, 1), (512, 4), (2048, 16))
ROPE_THETA = 500000.0
ROPE_DIM = HEAD_DIM // 4
N_EXPERTS = 16
EXPERT_FF = D_MODEL // 2
EC_FACTOR = 2
D_IN = FNET_W + 2 * GMLP_W + (SWA_HEADS + 2 * SWA_KV_HEADS) * HEAD_DIM + 3 * DIL_HEADS * HEAD_DIM
EPS = 1e-6
NEG_INF = -1e30

kernel_name = 'hybrid_parallel_mixers_ec_moe_encoder'


def rmsnorm(x, g):
    xf = x.astype(jnp.float32)
    y = xf * lax.rsqrt(jnp.mean(xf * xf, axis=-1, keepdims=True) + EPS)
    return (y * g.astype(jnp.float32)).astype(x.dtype)


def modulate(h, shift, scale):
    return h * (1 + scale[:, None, :]) + shift[:, None, :]


def split_points():
    sizes = (FNET_W, 2 * GMLP_W, SWA_HEADS * HEAD_DIM, SWA_KV_HEADS * HEAD_DIM,
             SWA_KV_HEADS * HEAD_DIM, DIL_HEADS * HEAD_DIM, DIL_HEADS * HEAD_DIM,
             DIL_HEADS * HEAD_DIM)
    return [int(v) for v in np.cumsum(sizes)[:-1]]


def rope(x, positions):
    half = ROPE_DIM // 2
    inv = jnp.power(jnp.float32(ROPE_THETA), -jnp.arange(0, ROPE_DIM, 2, dtype=jnp.float32) / ROPE_DIM)
    ang = positions.astype(jnp.float32)[..., None] * inv
    cos = jnp.cos(ang)[:, :, None, :]
    sin = jnp.sin(ang)[:, :, None, :]
    xr = x[..., :ROPE_DIM].astype(jnp.float32)
    x1, x2 = xr[..., :half], xr[..., half:]
    rot = jnp.concatenate([x1 * cos - x2 * sin, x2 * cos + x1 * sin], axis=-1)
    return jnp.concatenate([rot.astype(x.dtype), x[..., ROPE_DIM:]], axis=-1)


def banded_attention(q, k, v, half_window, sink=None):
    n, L, h, dh = q.shape
    hk = k.shape[2]
    g = h // hk
    w = half_window
    nb = -(-L // w)
    lp = nb * w
    pad = lp - L
    qp = jnp.pad(q, ((0, 0), (0, pad), (0, 0), (0, 0)))
    kp = jnp.pad(k, ((0, 0), (w, pad + w), (0, 0), (0, 0)))
    vp = jnp.pad(v, ((0, 0), (w, pad + w), (0, 0), (0, 0)))

    def neighbour_blocks(t):
        tb = t.reshape(n, nb + 2, w, hk, dh)
        return jnp.concatenate([tb[:, :-2], tb[:, 1:-1], tb[:, 2:]], axis=2)

    kb = neighbour_blocks(kp)
    vb = neighbour_blocks(vp)
    qb = qp.reshape(n, nb, w, hk, g, dh)
    s = jnp.einsum('nbqhgd,nbkhd->nbhgqk', qb, kb,
                   preferred_element_type=jnp.float32) * (dh ** -0.5)
    qpos = jnp.arange(nb)[:, None] * w + jnp.arange(w)[None, :]
    kpos = (jnp.arange(nb)[:, None] - 1) * w + jnp.arange(3 * w)[None, :]
    kp3 = kpos[:, None, :]
    valid = (jnp.abs(kp3 - qpos[:, :, None]) <= w) & (kp3 >= 0) & (kp3 < L)
    s = jnp.where(valid[None, :, None, None, :, :], s, NEG_INF)
    if sink is None:
        lse = jax.nn.logsumexp(s, axis=-1)
    else:
        sk = jnp.broadcast_to(sink.astype(jnp.float32).reshape(hk, g)[None, None, :, :, None, None],
                              s.shape[:-1] + (1,))
        lse = jax.nn.logsumexp(jnp.concatenate([s, sk], axis=-1), axis=-1)
    p = jnp.exp(s - lse[..., None])
    out = jnp.einsum('nbhgqk,nbkhd->nbqhgd', p.astype(v.dtype), vb)
    out = out.reshape(n, lp, h, dh)[:, :L]
    lse = lse.transpose(0, 1, 4, 2, 3).reshape(n, lp, h)[:, :L]
    return out, lse


def dilated_attention(q, k, v):
    b, s, h, dh = q.shape
    outs, lses = [], []
    for window, dilation in DIL_CONFIGS:
        steps = window // (2 * dilation)
        ls = s // dilation

        def gather(t):
            return t.reshape(b, ls, dilation, h, dh).transpose(0, 2, 1, 3, 4).reshape(b * dilation, ls, h, dh)

        o, lse = banded_attention(gather(q), gather(k), gather(v), steps)
        outs.append(o.reshape(b, dilation, ls, h, dh).transpose(0, 2, 1, 3, 4).reshape(b, s, h, dh))
        lses.append(lse.reshape(b, dilation, ls, h).transpose(0, 2, 1, 3).reshape(b, s, h))
    wts = jax.nn.softmax(jnp.stack(lses, axis=0), axis=0)
    return jnp.einsum('cbsh,cbshd->bshd', wts.astype(q.dtype), jnp.stack(outs, axis=0))


def fourier_mix(xa, fnet_w):
    b, s, _ = xa.shape
    xg = xa.reshape(b, s, N_GROUP_HEADS, HEAD_DIM).astype(jnp.float32)
    f = jnp.fft.fft2(xg, axes=(1, 3), norm='ortho').real.astype(xa.dtype)
    return jnp.einsum('bshc,hcd->bshd', f, fnet_w).reshape(b, s, FNET_W)


def spatial_gating(uv, sgu_norm, sgu_w, sgu_b):
    uv = jax.nn.gelu(uv)
    u, v = jnp.split(uv, 2, axis=-1)
    v = rmsnorm(v, sgu_norm)
    b, s, _ = v.shape
    vc = v.reshape(b, s // CHUNK, CHUNK, N_GROUP_HEADS, HEAD_DIM)
    z = jnp.einsum('hpq,bnqhc->bnphc', sgu_w, vc) + sgu_b.T[None, None, :, :, None]
    return u * z.reshape(b, s, GMLP_W)


def expert_choice_ffn(h, router_w, w_gate, w_up, w_down):
    b, s, d = h.shape
    cap = max(1, EC_FACTOR * s // N_EXPERTS)
    aff = jax.nn.softmax(jnp.einsum('bsd,de->bse', h, router_w,
                                    preferred_element_type=jnp.float32), axis=-1)
    gate, idx = lax.top_k(aff.transpose(0, 2, 1), cap)
    bidx = jnp.arange(b)[:, None, None]
    xe = h[bidx, idx]
    a = jnp.einsum('becd,edf->becf', xe, w_gate)
    u = jnp.einsum('becd,edf->becf', xe, w_up)
    o = jnp.einsum('becf,efd->becd', jax.nn.silu(a) * u, w_down)
    o = o * gate[..., None].astype(o.dtype)
    return jnp.zeros_like(h).at[bidx, idx].add(o)


def hybrid_layer(x, c, positions, ada_w, ada_b, norm_mix, w_in, fnet_w, sgu_norm, sgu_w,
                 sgu_b, swa_sink, group_norm, w_out, norm_ffn, router_w, exp_w_gate,
                 exp_w_up, exp_w_down):
    b, s, _ = x.shape
    mod = jnp.dot(jax.nn.silu(c), ada_w) + ada_b
    shift_m, scale_m, gate_m, shift_f, scale_f, gate_f = jnp.split(mod, 6, axis=-1)

    h = modulate(rmsnorm(x, norm_mix), shift_m, scale_m)
    proj = jnp.einsum('bsd,de->bse', h, w_in)
    xa, uv, qc, kc, vc, qd, kd, vd = jnp.split(proj, split_points(), axis=-1)

    def heads(t, n):
        return t.reshape(b, s, n, HEAD_DIM)

    y_a = fourier_mix(xa, fnet_w)
    y_b = spatial_gating(uv, sgu_norm, sgu_w, sgu_b)
    y_c, _ = banded_attention(rope(heads(qc, SWA_HEADS), positions),
                              rope(heads(kc, SWA_KV_HEADS), positions),
                              heads(vc, SWA_KV_HEADS), SWA_HALF_WINDOW, swa_sink)
    y_d = dilated_attention(rope(heads(qd, DIL_HEADS), positions),
                            rope(heads(kd, DIL_HEADS), positions),
                            heads(vd, DIL_HEADS))
    y = jnp.concatenate([y_a, y_b, y_c.reshape(b, s, -1), y_d.reshape(b, s, -1)], axis=-1)
    y = rmsnorm(y.reshape(b, s, N_MIXERS, GROUP_WIDTH),
                group_norm.reshape(N_MIXERS, GROUP_WIDTH)).reshape(b, s, D_MIX)
    x = x + gate_m[:, None, :] * jnp.einsum('bse,ed->bsd', y, w_out)

    h = modulate(rmsnorm(x, norm_ffn), shift_f, scale_f)
    x = x + gate_f[:, None, :] * expert_choice_ffn(h, router_w, exp_w_gate, exp_w_up, exp_w_down)
    return x


def setup_inputs(seed: int = 0) -> dict:
    key = jax.random.key(seed)
    ks = jax.random.split(key, 24)
    f32 = jnp.float32

    def nrm(k, shape, scale):
        return jax.random.normal(k, shape, f32) * scale

    def gain(k, shape):
        return 1.0 + 0.02 * jax.random.normal(k, shape, f32)

    L = DEPTH
    return {
        'x': nrm(ks[0], (BATCH, SEQ, D_MODEL), 1.0),
        'c': nrm(ks[1], (BATCH, D_MODEL), 1.0),
        'positions': jnp.arange(SEQ, dtype=jnp.int32)[None, :]
                     + jax.random.randint(ks[2], (BATCH, 1), 0, 1024, dtype=jnp.int32),
        'ada_w': nrm(ks[3], (L, D_MODEL, 6 * D_MODEL), 0.5 * D_MODEL ** -0.5),
        'ada_b': nrm(ks[4], (L, 6 * D_MODEL), 0.02),
        'norm_mix': gain(ks[5], (L, D_MODEL)),
        'w_in': nrm(ks[6], (L, D_MODEL, D_IN), D_MODEL ** -0.5),
        'fnet_w': nrm(ks[7], (L, N_GROUP_HEADS, HEAD_DIM, HEAD_DIM), HEAD_DIM ** -0.5),
        'sgu_norm': gain(ks[8], (L, GMLP_W)),
        'sgu_w': nrm(ks[9], (L, N_GROUP_HEADS, CHUNK, CHUNK), 0.5 * CHUNK ** -0.5),
        'sgu_b': 1.0 + nrm(ks[10], (L, N_GROUP_HEADS, CHUNK), 0.1),
        'swa_sink': nrm(ks[11], (L, SWA_HEADS), 0.5),
        'group_norm': gain(ks[12], (L, D_MIX)),
        'w_out': nrm(ks[13], (L, D_MIX, D_MODEL), D_MIX ** -0.5),
        'norm_ffn': gain(ks[14], (L, D_MODEL)),
        'router_w': nrm(ks[15], (L, D_MODEL, N_EXPERTS), D_MODEL ** -0.5),
        'exp_w_gate': nrm(ks[16], (L, N_EXPERTS, D_MODEL, EXPERT_FF), D_MODEL ** -0.5),
        'exp_w_up': nrm(ks[17], (L, N_EXPERTS, D_MODEL, EXPERT_FF), D_MODEL ** -0.5),
        'exp_w_down': nrm(ks[18], (L, N_EXPERTS, EXPERT_FF, D_MODEL), EXPERT_FF ** -0.5),
        'final_norm': gain(ks[19], (D_MODEL,)),
    }


def reference(x, c, positions, ada_w, ada_b, norm_mix, w_in, fnet_w, sgu_norm, sgu_w, sgu_b,
              swa_sink, group_norm, w_out, norm_ffn, router_w, exp_w_gate, exp_w_up,
              exp_w_down, final_norm):
    for l in range(DEPTH):
        x = hybrid_layer(x, c, positions, ada_w[l], ada_b[l], norm_mix[l], w_in[l], fnet_w[l],
                         sgu_norm[l], sgu_w[l], sgu_b[l], swa_sink[l], group_norm[l], w_out[l],
                         norm_ffn[l], router_w[l], exp_w_gate[l], exp_w_up[l], exp_w_down[l])
    return rmsnorm(x, final_norm)
```

```python
import contextlib
import math
import numpy as np
import ml_dtypes
import concourse.bass as bass
import concourse.mybir as mybir
from concourse.bass_utils import run_bass_kernel_spmd

F32 = mybir.dt.float32
BF16 = mybir.dt.bfloat16
I32 = mybir.dt.int32
U32 = mybir.dt.uint32
AF = mybir.ActivationFunctionType
ALU = mybir.AluOpType
AX = mybir.AxisListType

D = 2048
SEQ = 2048
NT = 16
NK = 16
D_IN = 3840
EPS = 1e-6
NEG = -30000.0
N_EXP = 16
CAP = 256
FF = 1024


class Buf:
    __slots__ = ("name", "w", "r")

    def __init__(self, name=""):
        self.name = name
        self.w = None
        self.r = {}


class Sched:
    def __init__(self, nc, stack, n_dma=24):
        self.nc = nc
        self.engs = {"pe": nc.tensor, "act": nc.scalar, "dve": nc.vector, "pool": nc.gpsimd, "sp": nc.sync}
        self.sems = {k: stack.enter_context(nc.semaphore("s_" + k)) for k in self.engs}
        self.cnt = {k: 0 for k in self.engs}
        self.seen = {k: {} for k in self.engs}
        self.dsems = [stack.enter_context(nc.semaphore("d%d" % i)) for i in range(n_dma)]
        self.dcnt = [0] * n_dma
        self.dnext = 0
        self.qnext = {}
        self.marks = []
        self.nins = 0

    def _wait(self, eng, key, val):
        if key == eng == "pe":
            return
        if self.seen[eng].get(key, 0) >= val:
            return
        sem = self.sems[key] if isinstance(key, str) else self.dsems[key[1]]
        self.engs[eng].wait_ge(sem, val)
        self.seen[eng][key] = val

    def _deps(self, eng, reads, writes):
        for b in reads:
            if b.w is not None:
                self._wait(eng, *b.w)
        for b in writes:
            if b.w is not None:
                self._wait(eng, *b.w)
            for k, v in b.r.items():
                self._wait(eng, k, v)

    def op(self, eng, fn, r=(), w=()):
        self._deps(eng, r, w)
        ins = fn()
        self.cnt[eng] += 1
        self.nins += 1
        ins.then_inc(self.sems[eng], 1)
        c = self.cnt[eng]
        for b in r:
            b.r[eng] = c
        for b in w:
            b.w = (eng, c)
            b.r = {}
        return ins

    def dma(self, q, out, in_, r=(), w=(), indirect=None, **kw):
        lo, hi = (0, 8) if q == "sp" else (8, len(self.dsems))
        nxt = self.qnext.get(q, lo)
        i = nxt
        self.qnext[q] = lo + (nxt + 1 - lo) % (hi - lo)
        if self.dcnt[i] > 0:
            self._wait(q, ("d", i), self.dcnt[i])
        self._deps(q, r, w)
        only_if = kw.pop("only_if", None)
        if only_if is not None:
            eng = self.engs[q]
            with eng.If(only_if):
                ins = eng.dma_start(out=out, in_=in_, **kw)
                ins.then_inc(self.dsems[i], 16)
            with eng.Else():
                eng.memset(out, 0.0).then_inc(self.dsems[i], 16)
            self.dcnt[i] += 16
            self.nins += 1
        else:
            if indirect is None:
                ins = self.engs[q].dma_start(out=out, in_=in_, **kw)
            else:
                ins = indirect()
            self.dcnt[i] += 16
            self.nins += 1
            ins.then_inc(self.dsems[i], 16)
        key = ("d", i)
        for b in r:
            b.r[key] = self.dcnt[i]
        for b in w:
            b.w = (key, self.dcnt[i])
            b.r = {}
        return ins

    def barrier(self):
        self.marks.append(dict(self.cnt))
        for e in self.engs:
            for k in self.engs:
                if k != e and self.cnt[k] > 0:
                    self._wait(e, k, self.cnt[k])
            for i in range(len(self.dsems)):
                if self.dcnt[i] > 0:
                    self._wait(e, ("d", i), self.dcnt[i])

    def wait_all(self, eng):
        for k in self.engs:
            if k != eng and self.cnt[k] > 0:
                self._wait(eng, k, self.cnt[k])
        for i in range(len(self.dsems)):
            if self.dcnt[i] > 0:
                self._wait(eng, ("d", i), self.dcnt[i])


def host_consts():
    bf = ml_dtypes.bfloat16
    c = {}
    c["ident_bf"] = np.eye(128, dtype=np.float32).astype(bf)
    c["ident_f"] = np.eye(128, dtype=np.float32)
    c["ones_bf"] = np.ones((128, 128), np.float32).astype(bf)
    c["ones_f"] = np.ones((128, 128), np.float32)
    a = np.arange(128)[:, None]
    i = np.arange(128)[None, :]
    masks = []
    masks.append(a >= i)
    masks.append(a <= i)
    masks.append(np.abs(a - i) <= 64)
    masks.append(a >= i + 64)
    masks.append(a <= i - 64)
    m = np.stack([np.where(v, 0.0, NEG) for v in masks], axis=1).astype(np.float32)
    z = np.zeros((128, 128), np.float32)
    m3 = np.stack([np.concatenate([m[:, 0], z, m[:, 1]], axis=1),
                   np.concatenate([m[:, 3], m[:, 2], m[:, 4]], axis=1)], axis=1)
    c["maskb"] = m3.astype(bf)
    R = np.zeros((64, 64), np.float32)
    for j in range(8):
        R[j, j + 8] = -1.0
        R[j + 8, j] = 1.0
    RT = R.T
    rot = np.zeros((128, 128), np.float32)
    rot[:64, :64] = RT
    rot[64:, 64:] = RT
    c["rotT"] = rot.astype(bf)
    inv = np.zeros((128, 1), np.float32)
    for p in range(128):
        q = p % 64
        if q < 16:
            inv[p, 0] = np.float32(500000.0) ** (-np.float32(2 * (q % 8)) / np.float32(16))
    c["invf"] = inv
    s = np.arange(2048, dtype=np.float64)
    ang = 2.0 * np.pi * ((s[:, None] * s[None, :]) % 2048) / 2048.0
    c["dftc"] = (np.cos(ang) / np.sqrt(2048.0)).astype(np.float32).astype(bf)
    c["dfts"] = (np.sin(ang) / np.sqrt(2048.0)).astype(np.float32).astype(bf)
    cc = np.arange(64, dtype=np.float64)
    angc = 2.0 * np.pi * ((cc[:, None] * cc[None, :]) % 64) / 64.0
    C = np.cos(angc) / 8.0
    Sn = np.sin(angc) / 8.0
    cb = np.zeros((128, 128)); sbm = np.zeros((128, 128))
    cb[:64, :64] = C; cb[64:, 64:] = C
    sbm[:64, :64] = -Sn; sbm[64:, 64:] = -Sn
    c["dftcc"] = cb.astype(np.float32).astype(bf)
    c["dftsc"] = sbm.astype(np.float32).astype(bf)
    c["iota_f"] = np.tile(np.arange(2048, dtype=np.float32)[None, :], (128, 1))
    return c


CONST_SPECS = [
    ("ident_bf", [128, 128], BF16), ("ident_f", [128, 128], F32), ("ones_bf", [128, 128], BF16),
    ("ones_f", [128, 128], F32), ("maskb", [128, 2, 384], BF16), ("rotT", [128, 128], BF16),
    ("invf", [128, 1], F32), ("dftc", [2048, 2048], BF16), ("dfts", [2048, 2048], BF16),
    ("dftcc", [128, 128], BF16), ("dftsc", [128, 128], BF16),
]


def build(L=2, dbg=(), stop_after=None, skip=()):
    nc = bass.Bass("TRN2", target_bir_lowering=False)
    dbg = set(dbg)

    def din(name, shape, dt):
        return nc.dram_tensor(name, list(shape), dt, kind="ExternalInput").ap()

    x_in = din("x", [SEQ, D], F32)
    cT_in = din("cT", [128, 16], F32)
    posb_in = din("posb", [128, SEQ], I32)
    ada_w = din("ada_w", [L, D, 6 * D], F32)
    ada_bT = din("ada_bT", [L, 128, 96], F32)
    nmT_in = din("nmT", [L, 128, 16], F32)
    nfT_in = din("nfT", [L, 128, 16], F32)
    gnT_in = din("gnT", [L, 128, 16], F32)
    w_in = din("w_in", [L, D, D_IN], F32)
    fnet_wS = din("fnet_wS", [L, 4, 128, 64], F32)
    sgun_bc = din("sgun_bc", [L, 128, 512], F32)
    sgu_wT = din("sgu_wT", [L, 128, 8, 128], F32)
    sgub_bc = din("sgub_bc", [L, 128, 4, 128], F32)
    sinkT = din("sinkT", [L, 128, 8], F32)
    w_out = din("w_out", [L, D, D], F32)
    router_w = din("router_w", [L, D, N_EXP], F32)
    wg_in = din("exp_w_gate", [L, N_EXP, D, FF], F32)
    wu_in = din("exp_w_up", [L, N_EXP, D, FF], F32)
    wd_in = din("exp_w_down", [L, N_EXP, FF, D], F32)
    fin_bc = din("fin_bc", [128, D], F32)
    cst = {n: din(n, s, dt) for (n, s, dt) in CONST_SPECS}
    out_ap = nc.dram_tensor("out", [SEQ, D], F32, kind="ExternalOutput").ap()
    xres = nc.dram_tensor("xres", [SEQ, D], F32, kind="ExternalOutput" if "xres" in dbg else "Internal").ap()
    ynT_d = nc.dram_tensor("ynT_d", [128, NT, 16, 128], BF16, kind="ExternalOutput" if "ynT" in dbg else "Internal").ap()
    h2_d = nc.dram_tensor("h2_d", [SEQ, D], BF16, kind="Internal").ap()
    dbg_outs = {}

    with contextlib.ExitStack() as st:
        S = Sched(nc, st)

        uid = [0]

        def sbuf(stack, name, shape, dt):
            uid[0] += 1
            return stack.enter_context(nc.sbuf_tensor("sb%d_%s" % (uid[0], name), list(shape), dt))

        def psum(stack, name, shape, dt):
            uid[0] += 1
            return stack.enter_context(nc.psum_tensor("pp%d_%s" % (uid[0], name), list(shape), dt))

        def dump(name, ap, shape, dt, rbuf):
            if name not in dbg:
                return
            o = nc.dram_tensor("dbg_" + name, list(shape), dt, kind="ExternalOutput").ap()
            dbg_outs[name] = o
            S.dma("sp", o, ap, r=rbuf)

        def fin():
            S.wait_all("sp")
            dbg_outs["_marks"] = S.marks
            return nc, dbg_outs

        def mm(out, lhsT, rhs, start, stop, r, w):
            return S.op("pe", lambda: nc.tensor.matmul(out, lhsT, rhs, start=start, stop=stop), r, w)

        def tr(out, in_, ident, r, w):
            return S.op("pe", lambda: nc.tensor.transpose(out, in_, ident), r, w)

        def act(out, in_, func, r, w, eng="act", **kw):
            return S.op("act", lambda: nc.scalar.activation(out=out, in_=in_, func=func, **kw), r, w)

        def ts(out, in0, s1, s2, op0, op1=None, r=(), w=(), eng="dve"):
            e = S.engs[eng]
            if op1 is None:
                return S.op(eng, lambda: e.tensor_scalar(out=out, in0=in0, scalar1=s1, scalar2=None, op0=op0), r, w)
            return S.op(eng, lambda: e.tensor_scalar(out=out, in0=in0, scalar1=s1, scalar2=s2, op0=op0, op1=op1), r, w)

        def tt(out, in0, in1, op, r=(), w=(), eng="dve"):
            e = S.engs[eng]
            return S.op(eng, lambda: e.tensor_tensor(out=out, in0=in0, in1=in1, op=op), r, w)

        def stt(out, in0, scalar, in1, op0, op1, r=(), w=()):
            return S.op("dve", lambda: nc.vector.scalar_tensor_tensor(out=out, in0=in0, scalar=scalar, in1=in1, op0=op0, op1=op1), r, w)

        def cp(out, in_, r=(), w=(), eng="dve"):
            if eng == "act":
                return S.op("act", lambda: nc.scalar.copy(out=out, in_=in_), r, w)
            e = S.engs[eng]
            return S.op(eng, lambda: e.tensor_copy(out=out, in_=in_), r, w)

        def memset(ap, val, w, eng="pool"):
            e = S.engs[eng]
            return S.op(eng, lambda: e.memset(ap, val), (), w)

        def rstd_from_ss(out, ss_ap, inv_n, b):
            ts(out, ss_ap, inv_n, EPS, ALU.mult, ALU.add, r=[b], w=[b])
            act(out, out, AF.Sqrt, r=[b], w=[b])
            S.op("dve", lambda: nc.vector.reciprocal(out=out, in_=out), [b], [b])

        is_even = nc.gpsimd.snap(nc.gpsimd.partition_id() % 2 == 0)

        ident_bf = sbuf(st, "ident_bf", [128, 128], BF16)
        ident_f = sbuf(st, "ident_f", [128, 128], F32)
        ones_bf = sbuf(st, "ones_bf", [128, 128], BF16)
        ones_f = sbuf(st, "ones_f", [128, 128], F32)
        maskb = sbuf(st, "maskb", [128, 2, 384], BF16)
        rotT = sbuf(st, "rotT", [128, 128], BF16)
        invf = sbuf(st, "invf", [128, 1], F32)
        dftcc = sbuf(st, "dftcc", [128, 128], BF16)
        dftsc = sbuf(st, "dftsc", [128, 128], BF16)
        cB = Buf("consts")
        for nm, t_ in (("ident_bf", ident_bf), ("ident_f", ident_f), ("ones_bf", ones_bf), ("ones_f", ones_f),
                       ("maskb", maskb), ("rotT", rotT), ("invf", invf), ("dftcc", dftcc), ("dftsc", dftsc)):
            S.dma("sp", t_[:], cst[nm], w=[cB])
        cosT = sbuf(st, "cosT", [128, SEQ], F32)
        sinT = sbuf(st, "sinT", [128, SEQ], F32)
        ropeB = Buf("rope")
        hT = sbuf(st, "hT", [128, NK, SEQ], BF16)
        hT_b = [Buf("hT%d" % i) for i in range(NT)]
        modT = sbuf(st, "modT", [128, 96], F32)
        modB = Buf("mod")
        gsm = sbuf(st, "gsm", [128, 16], F32)
        gsf = sbuf(st, "gsf", [128, 16], F32)
        small = sbuf(st, "small", [128, 64], F32)
        smallB = Buf("small")
        NWB = 2
        wbuf = [sbuf(st, "wbuf%d" % i, [128, NK * 512], BF16) for i in range(NWB)]
        wbuf_b = [Buf("wbuf%d" % i) for i in range(NWB)]
        wstate = {"i": 0, "bufs": list(zip(wbuf, wbuf_b))}

        def wload(src_ap, ncols, kt=NK):
            i = wstate["i"] % len(wstate["bufs"])
            wstate["i"] = i + 1
            wt_, wb_ = wstate["bufs"][i]
            view = wt_[:, 0:kt * ncols].rearrange("p (k n) -> p k n", n=ncols)
            S.dma("pool", view, src_ap, w=[wb_])
            return view, wb_

        class WStream:
            def __init__(self, bufs):
                self.bufs = bufs
                self.free = list(range(len(bufs)))
                self.pending = []
                self.loaded = {}
                self.extra = {}

            def add_bufs(self, bufs, extra_w):
                for tb in bufs:
                    self.bufs.append(tb)
                    self.free.append(len(self.bufs) - 1)
                    self.extra[len(self.bufs) - 1] = list(extra_w)
                self.pump()

            def request(self, key, src_ap, ncols, kt=NK):
                self.pending.append((key, src_ap, ncols, kt))

            def pump(self):
                while self.pending and self.free:
                    key, src_ap, ncols, kt = self.pending.pop(0)
                    i = self.free.pop(0)
                    t_, b_ = self.bufs[i][0], self.bufs[i][1]
                    extra = self.extra.pop(i, [])
                    view = t_[:, 0:kt * ncols].rearrange("p (k n) -> p k n", n=ncols)
                    S.dma("pool", view, src_ap, w=[b_] + extra, only_if=is_even)
                    self.loaded[key] = (view, b_, i)

            def get(self, key):
                self.pump()
                v, b_, _ = self.loaded[key]
                return v, b_

            def release(self, key):
                _, _, i = self.loaded.pop(key)
                self.free.append(i)
                self.pump()

        def hT_as_wbufs():
            return [(hT[:, 4 * i:4 * i + 4, :].rearrange("p a n -> p (a n)"), Buf("hTw%d" % i)) for i in range(4)]

        ps = [psum(st, "ps%d" % i, [128, 512], F32) for i in range(8)]
        ps_b = [Buf("ps%d" % i) for i in range(8)]
        psb = [ps[6][:].bitcast(BF16), ps[7][:].bitcast(BF16)]
        psb_b = [ps_b[6], ps_b[7]]

        with contextlib.ExitStack() as ph:
            posi = sbuf(ph, "posi", [128, SEQ], I32)
            ang = sbuf(ph, "ang", [128, SEQ], F32)
            tmpa = sbuf(ph, "tmpa", [128, SEQ], F32)
            pB, aB, tB = Buf(), Buf(), Buf()
            S.dma("sp", posi[:], posb_in, w=[pB])
            cp(ang[:], posi[:], r=[pB], w=[aB])
            ts(ang[:], ang[:], invf[:, 0:1], None, ALU.mult, r=[aB, cB], w=[aB])
            ki = sbuf(ph, "ki", [128, SEQ], I32)
            kf = sbuf(ph, "kf", [128, SEQ], F32)
            kB = Buf()

            def sin_of(dst, offset):
                ts(tmpa[:], ang[:], offset, None, ALU.add, r=[aB], w=[tB])
                ts(kf[:], tmpa[:], 1.0 / (2 * math.pi), None, ALU.mult, r=[tB], w=[kB])
                cp(ki[:], kf[:], r=[kB], w=[kB])
                cp(kf[:], ki[:], r=[kB], w=[kB])
                stt(tmpa[:], kf[:], -2 * math.pi, tmpa[:], ALU.mult, ALU.add, r=[kB, tB], w=[tB])
                ts(kf[:], tmpa[:], math.pi, None, ALU.is_gt, r=[tB], w=[kB])
                stt(tmpa[:], kf[:], -2 * math.pi, tmpa[:], ALU.mult, ALU.add, r=[kB, tB], w=[tB])
                ts(kf[:], tmpa[:], -math.pi, None, ALU.is_lt, r=[tB], w=[kB])
                stt(tmpa[:], kf[:], 2 * math.pi, tmpa[:], ALU.mult, ALU.add, r=[kB, tB], w=[tB])
                ts(tmpa[:], tmpa[:], math.pi, -math.pi, ALU.min, ALU.max, r=[tB], w=[tB])
                act(dst, tmpa[:], AF.Sin, r=[tB], w=[ropeB])

            sin_of(sinT[:], 0.0)
            sin_of(cosT[:], 0.5 * math.pi)
            dump("cosT", cosT[:], [128, SEQ], F32, [ropeB])
            dump("sinT", sinT[:], [128, SEQ], F32, [ropeB])
            S.barrier()

        xres_b = [Buf("xres%d" % i) for i in range(NT)]
        xresB = Buf("xres_scatter")
        h2B = Buf("h2_d")

        def bcast_row(ph, dst, src, srcB, dstB):
            dg = [sbuf(ph, "dg%d" % i, [128, 128], F32) for i in range(2)]
            dgB = [Buf(), Buf()]
            for j in range(16):
                i = j % 2
                ts(dg[i][:], ident_f[:, :], src[:, j:j + 1], None, ALU.mult, r=[cB, srcB], w=[dgB[i]])
                mm(ps[j // 4][:, (j % 4) * 128:(j % 4 + 1) * 128], ones_f[:, :], dg[i][:], True, True, r=[cB, dgB[i]], w=[ps_b[j // 4]])
                if j % 4 == 3:
                    cp(dst[:, (j // 4) * 512:(j // 4 + 1) * 512], ps[j // 4][:, :], r=[ps_b[j // 4]], w=[dstB], eng="act")

        for l in range(L):
            x_src = x_in if l == 0 else xres
            with contextlib.ExitStack() as ph:
                cT = sbuf(ph, "cT", [128, 16], F32)
                scb = sbuf(ph, "scb", [128, 16], BF16)
                abT = sbuf(ph, "abT", [128, 96], F32)
                nm = sbuf(ph, "nm", [128, 16], F32)
                nf = sbuf(ph, "nf", [128, 16], F32)
                b1, b2, b3 = Buf(), Buf(), Buf()
                S.dma("sp", cT[:], cT_in, w=[b1])
                S.dma("sp", abT[:], ada_bT[l], w=[b3])
                S.dma("sp", nm[:], nmT_in[l], w=[b3])
                S.dma("sp", nf[:], nfT_in[l], w=[b3])
                act(scb[:], cT[:], AF.Silu, r=[b1], w=[b2])
                aw = ada_w[l].rearrange("(kt p) n -> p kt n", p=128)
                ws = WStream([(wbuf[i][:, :], wbuf_b[i]) for i in range(NWB)] + hT_as_wbufs())
                for cb in range(24):
                    ws.request(cb, aw[:, :, cb * 512:(cb + 1) * 512], 512)
                for cb in range(24):
                    wt, wb = ws.get(cb)
                    for sc in range(4):
                        j = cb * 4 + sc
                        for kt in range(NK):
                            mm(ps[0][:, j:j + 1], wt[:, kt, sc * 128:(sc + 1) * 128], scb[:, kt:kt + 1],
                               kt == 0, kt == NK - 1, r=[wb, b2], w=[ps_b[0]])
                    ws.release(cb)
                tt(modT[:], ps[0][:, 0:96], abT[:], ALU.add, r=[ps_b[0], b3], w=[modB])
                stt(gsm[:], modT[:, 16:32], 1.0, nm[:], ALU.add, ALU.mult, r=[modB, b3], w=[modB])
                stt(gsf[:], modT[:, 64:80], 1.0, nf[:], ALU.add, ALU.mult, r=[modB, b3], w=[modB])
                dump("modT%d" % l, modT[:], [128, 96], F32, [modB])
                S.barrier()
            if stop_after == "mod":
                break

            def norm_to_hT(x_src, gs, shift_off, also_h2=False):
                with contextlib.ExitStack() as ph:
                    xt = [sbuf(ph, "xt%d" % i, [128, D], F32) for i in range(2)]
                    xt_b = [Buf(), Buf()]
                    xn = [sbuf(ph, "xn%d" % i, [128, D], BF16) for i in range(2)]
                    xn_b = [Buf(), Buf()]
                    junk = sbuf(ph, "junk", [128, D], BF16)
                    jB = Buf()
                    ss = sbuf(ph, "ss", [128, 4], F32)
                    ssB = [Buf(), Buf()]
                    def ld_x(k_):
                        S.dma("sp", xt[k_ % 2][:], x_src[k_ * 128:(k_ + 1) * 128, :], r=[xres_b[k_]], w=[xt_b[k_ % 2]])

                    ld_x(0)
                    for t_ in range(NT):
                        i = t_ % 2
                        if t_ + 1 < NT:
                            ld_x(t_ + 1)
                        act(junk[:], xt[i][:], AF.Square, r=[xt_b[i]], w=[jB, ssB[i]], accum_out=ss[:, i:i + 1])
                        rstd_from_ss(ss[:, 2 + i:3 + i], ss[:, i:i + 1], 1.0 / D, ssB[i])
                        ts(xn[i][:], xt[i][:], ss[:, 2 + i:3 + i], None, ALU.mult, r=[xt_b[i], ssB[i]], w=[xn_b[i]])
                        if also_h2:
                            S.dma("sp", h2_d[t_ * 128:(t_ + 1) * 128, :], xn[i][:], r=[xn_b[i]], w=[h2B])
                        for hb in range(2):
                            for jj in range(8):
                                j = hb * 8 + jj
                                tr(psb[hb][:, jj * 128:(jj + 1) * 128], xn[i][:, j * 128:(j + 1) * 128], ident_bf[:, :],
                                   r=[xn_b[i], cB], w=[psb_b[hb]])
                            for jj in range(8):
                                j = hb * 8 + jj
                                ts(hT[:, j, t_ * 128:(t_ + 1) * 128], psb[hb][:, jj * 128:(jj + 1) * 128],
                                   gs[:, j:j + 1], modT[:, shift_off + j:shift_off + j + 1], ALU.mult, ALU.add,
                                   r=[psb_b[hb], modB], w=[hT_b[t_]])
                    S.barrier()

            norm_to_hT(x_src, gsm, 0)
            dump("hT%d" % l, hT[:], [128, NK, SEQ], BF16, hT_b)
            if stop_after == "hT":
                break

            wi = w_in[l].rearrange("(kt p) n -> p kt n", p=128)

            def gn_alloc(ph, N):
                return (sbuf(ph, "gn_sq", [128, 4, N], BF16), sbuf(ph, "gn_rs", [128, N], F32),
                        sbuf(ph, "gn_yn", [128, 4, N], BF16), Buf(), Buf(), Buf())

            def gn_store(ga, ysrc, yB, m, tok0, N, gn):
                sq, rs, yn, b_sq, b_rs, b_yn = ga
                for ch in range(4):
                    act(sq[:, ch, :], ysrc[:, ch, :], AF.Square, r=[yB], w=[b_sq])
                for ch in range(4):
                    mm(ps[5][:, 0:N], ones_bf[:, :], sq[:, ch, :], ch == 0, ch == 3, r=[b_sq, cB], w=[ps_b[5]])
                ts(rs[:], ps[5][:, 0:N], 1.0 / 512, EPS, ALU.mult, ALU.add, r=[ps_b[5]], w=[b_rs])
                act(rs[:], rs[:], AF.Ln, r=[b_rs], w=[b_rs])
                act(rs[:], rs[:], AF.Exp, r=[b_rs], w=[b_rs], scale=-0.5)
                for ch in range(4):
                    stt(yn[:, ch, :], ysrc[:, ch, :], gn[:, 4 * m + ch:4 * m + ch + 1], rs[:], ALU.mult, ALU.mult,
                        r=[yB, b_rs, gnB], w=[b_yn])
                t0 = tok0 // 128
                for ch in range(4):
                    S.dma("sp", ynT_d[:, t0:t0 + N // 128, 4 * m + ch, :],
                          yn[:, ch, :].rearrange("p (a t) -> p a t", t=128), r=[b_yn], w=[ynB])

            def gelu(dst, src_ps, xs, t1, r, w, bx, bt):
                cp(xs, src_ps, r=r, w=[bx], eng="act")
                tt(t1, xs, xs, ALU.mult, r=[bx], w=[bt])
                ts(t1, t1, 0.044715, 1.0, ALU.mult, ALU.add, r=[bt], w=[bt])
                tt(t1, t1, xs, ALU.mult, r=[bt, bx], w=[bt])
                act(t1, t1, AF.Sigmoid, r=[bt], w=[bt], scale=1.5957691216057308)
                tt(dst, t1, xs, ALU.mult, r=[bt, bx], w=w)

            gn_sb = sbuf(st, "gn%d" % l, [128, 16], F32)
            gnB = Buf()
            S.dma("sp", gn_sb[:], gnT_in[l], w=[gnB])
            ynB = Buf("ynT_d")

            if "fnet" not in skip:
              with contextlib.ExitStack() as ph:
                xa = sbuf(ph, "xa", [128, NT, 512], BF16)
                xaB = Buf()
                fw = sbuf(ph, "fw", [128, 4, 64], BF16)
                fwB = Buf()
                AB = sbuf(ph, "AB", [128, 8, 128], BF16)
                ABb = Buf()
                dC = [sbuf(ph, "dC%d" % i, [128, NK, 256], BF16) for i in range(2)]
                dS = [sbuf(ph, "dS%d" % i, [128, NK, 256], BF16) for i in range(2)]
                dB = [Buf(), Buf()]
                pq = sbuf(ph, "pq", [128, 4, 512], BF16)
                pqB = Buf()
                ya = sbuf(ph, "ya", [128, 4, 256], F32)
                yaB = Buf()
                ga = gn_alloc(ph, 256)
                S.dma("pool", fw[:], fnet_wS[l].rearrange("c p d -> p c d"), w=[fwB])
                memset(AB[:], 0.0, [ABb])
                for ch in range(4):
                    for which, tab in ((0, dftcc), (1, dftsc)):
                        mm(ps[4][:, 0:64], tab[:, :], fw[:, ch, :], True, True, r=[fwB, cB], w=[ps_b[4]])
                        cp(AB[0:64, which * 4 + ch, 0:64], ps[4][0:64, 0:64], r=[ps_b[4]], w=[ABb])
                        cp(AB[64:128, which * 4 + ch, 64:128], ps[4][64:128, 0:64], r=[ps_b[4]], w=[ABb])
                wt, wb = wload(wi[:, :, 0:512], 512)
                for t_ in range(NT):
                    pi = t_ % 2
                    for kt in range(NK):
                        mm(ps[pi][:, :], hT[:, kt, t_ * 128:(t_ + 1) * 128], wt[:, kt, :], kt == 0, kt == NK - 1,
                           r=[hT_b[t_], wb], w=[ps_b[pi]])
                    cp(xa[:, t_, :], ps[pi][:, :], r=[ps_b[pi]], w=[xaB], eng="act")
                dcv = cst["dftc"].rearrange("(kt p) n -> p kt n", p=128)
                dsv = cst["dfts"].rearrange("(kt p) n -> p kt n", p=128)
                def ld_tab(k_):
                    S.dma("sp", dC[k_ % 2][:], dcv[:, :, k_ * 256:(k_ + 1) * 256], w=[dB[k_ % 2]])
                    S.dma("sp", dS[k_ % 2][:], dsv[:, :, k_ * 256:(k_ + 1) * 256], w=[dB[k_ % 2]])

                ld_tab(0)
                for sbk in range(8):
                    bi = sbk % 2
                    if sbk + 1 < 8:
                        ld_tab(sbk + 1)
                    for ch in range(4):
                        for which, tab in ((0, dC[bi]), (1, dS[bi])):
                            for kt in range(NK):
                                mm(ps[ch][:, which * 256:(which + 1) * 256], xa[:, kt, ch * 128:(ch + 1) * 128],
                                   tab[:, kt, :], kt == 0, kt == NK - 1, r=[xaB, dB[bi]], w=[ps_b[ch]])
                        cp(pq[:, ch, :], ps[ch][:, :], r=[ps_b[ch]], w=[pqB], eng="act")
                    for ch in range(4):
                        o_ = ps[4][:, (ch % 2) * 256:(ch % 2 + 1) * 256] if ch < 2 else ps[6][:, (ch % 2) * 256:(ch % 2 + 1) * 256]
                        ob = ps_b[4] if ch < 2 else ps_b[6]
                        mm(o_, AB[:, ch, :], pq[:, ch, 0:256], True, False, r=[ABb, pqB], w=[ob])
                        mm(o_, AB[:, 4 + ch, :], pq[:, ch, 256:512], False, True, r=[ABb, pqB], w=[ob])
                        cp(ya[:, ch, :], o_, r=[ob], w=[yaB])
                    gn_store(ga, ya[:], yaB, 0, sbk * 256, 256, gn_sb)
                S.barrier()

            if "gmlp" not in skip:
              with contextlib.ExitStack() as ph:
                sgun = sbuf(ph, "sgun", [128, 512], F32)
                sguw = sbuf(ph, "sguw", [128, 8, 128], BF16)
                sgub = sbuf(ph, "sgub", [128, 4, 128], F32)
                gB = Buf()
                S.dma("sp", sgun[:], sgun_bc[l], w=[gB])
                S.dma("pool", sguw[:], sgu_wT[l], w=[gB])
                S.dma("sp", sgub[:], sgub_bc[l], w=[gB])
                wu, wub = wload(wi[:, :, 512:1024], 512)
                wv, wvb = wload(wi[:, :, 1024:1536], 512)
                uT = sbuf(ph, "uT", [128, 4, 512], F32)
                uB = Buf()
                yb = sbuf(ph, "yb", [128, 4, 512], F32)
                ybB = Buf()
                xs = sbuf(ph, "gxs", [128, 512], F32)
                t1 = sbuf(ph, "gt1", [128, 512], F32)
                bx, bt = Buf(), Buf()
                vs = sbuf(ph, "vs", [128, 512], F32)
                vB = Buf()
                vn = sbuf(ph, "vn", [128, 512], BF16)
                vnB = Buf()
                sv = sbuf(ph, "sv", [128, 2], F32)
                svB = Buf()
                zt = sbuf(ph, "zt", [128, 4, 128], F32)
                ztB = Buf()
                ga = gn_alloc(ph, 512)
                for tb in range(4):
                    for ch in range(4):
                        for kt in range(NK):
                            mm(ps[ch][:, :], wu[:, kt, ch * 128:(ch + 1) * 128], hT[:, kt, tb * 512:(tb + 1) * 512],
                               kt == 0, kt == NK - 1, r=[wub] + hT_b[tb * 4:tb * 4 + 4], w=[ps_b[ch]])
                        act(uT[:, ch, :], ps[ch][:, :], AF.Gelu_apprx_tanh, r=[ps_b[ch]], w=[uB])
                    for pc in range(4):
                        t_ = tb * 4 + pc
                        for kt in range(NK):
                            mm(ps[4][:, :], hT[:, kt, t_ * 128:(t_ + 1) * 128], wv[:, kt, :], kt == 0, kt == NK - 1,
                               r=[wvb, hT_b[t_]], w=[ps_b[4]])
                        act(vs[:], ps[4][:, :], AF.Gelu_apprx_tanh, r=[ps_b[4]], w=[vB])
                        act(t1[:], vs[:], AF.Square, r=[vB], w=[bt, svB], accum_out=sv[:, 0:1])
                        rstd_from_ss(sv[:, 1:2], sv[:, 0:1], 1.0 / 512, svB)
                        stt(vn[:], vs[:], sv[:, 1:2], sgun[:], ALU.mult, ALU.mult, r=[vB, svB, gB], w=[vnB])
                        for h_ in range(8):
                            ch, hh = h_ // 2, h_ % 2
                            mm(ps[5][hh * 64:(hh + 1) * 64, ch * 128:(ch + 1) * 128], vn[:, h_ * 64:(h_ + 1) * 64],
                               sguw[:, h_, :], True, True, r=[vnB, gB], w=[ps_b[5]])
                        tt(zt[:], ps[5][:, :].rearrange("p (c t) -> p c t", t=128), sgub[:], ALU.add, r=[ps_b[5], gB], w=[ztB])
                        tt(yb[:, :, pc * 128:(pc + 1) * 128], zt[:], uT[:, :, pc * 128:(pc + 1) * 128], ALU.mult,
                           r=[ztB, uB], w=[ybB])
                    gn_store(ga, yb[:], ybB, 1, tb * 512, 512, gn_sb)
                S.barrier()
            if stop_after == "mix01":
                break

            def proj_stream(cols):
                subs = [(wbuf[i][:, j * 2048:(j + 1) * 2048], Buf("wsub%d_%d" % (i, j))) for i in range(NWB) for j in range(4)]
                ws_ = WStream(subs)
                for c_ in cols:
                    ws_.request(c_, wi[:, :, c_:c_ + 128], 128)
                ws_.pump()
                return ws_

            def proj_fm(col0, swap=False):
                wt, wb = pstate["ws"].get(col0)
                pstate["rel"] = col0
                for tb in range(4):
                    rr = [wb] + hT_b[tb * 4:tb * 4 + 4]
                    rhs = lambda kt: hT[:, kt, tb * 512:(tb + 1) * 512]
                    if not swap:
                        for kt in range(NK):
                            mm(ps[tb][:, :], wt[:, kt, :], rhs(kt), kt == 0, kt == NK - 1, r=rr, w=[ps_b[tb]])
                    else:
                        for kt in range(NK):
                            mm(ps[tb][0:64, :], wt[:, kt, 64:128], rhs(kt), kt == 0, kt == NK - 1, r=rr, w=[ps_b[tb]])
                        for kt in range(NK):
                            mm(ps[tb][64:128, :], wt[:, kt, 0:64], rhs(kt), kt == 0, kt == NK - 1, r=rr, w=[ps_b[tb]])
                pstate["ws"].release(col0)

            def rope_evac(rs_, dst, dstB, placed=None):
                qr, qrB, t1, t1B, t2, t2B = rs_
                for tb in range(4):
                    blk = slice(tb * 512, (tb + 1) * 512)
                    pj = 4 + tb % 2
                    cp(qr[:], ps[tb][:, :], r=[ps_b[tb]], w=[qrB], eng="act")
                    mm(ps[pj][:, :], rotT[:, :], qr[:], True, True, r=[qrB, cB], w=[ps_b[pj]])
                    cp(t1[:], ps[tb][:, :], r=[ps_b[tb]], w=[t1B], eng="act")
                    cp(t2[:], ps[pj][:, :], r=[ps_b[pj]], w=[t2B], eng="act")
                    tt(t1[:], t1[:], cosT[:, blk], ALU.mult, r=[ropeB], w=[t1B])
                    tt(t2[:], t2[:], sinT[:, blk], ALU.mult, r=[ropeB], w=[t2B], eng="pool")
                    if placed is None:
                        tt(dst[:, blk], t1[:], t2[:], ALU.add, r=[t1B, t2B], w=[dstB])
                    else:
                        for (tl, tlB, sb_, tg_) in placed:
                            tt(tl[tg_:tg_ + 64, blk], t1[sb_:sb_ + 64, :], t2[sb_:sb_ + 64, :], ALU.add,
                               r=[t1B, t2B], w=[tlB])

            def plain_evac(dst, dstB):
                for tb in range(4):
                    cp(dst[:, tb * 512:(tb + 1) * 512], ps[tb][:, :], r=[ps_b[tb]], w=[dstB], eng="act")

            def gslice(r_, m_, d):
                start = r_ + d * 128 * m_
                return slice(start, start + 128) if d == 1 else slice(start, start + d * 127 + 1, d)

            def v_to_tok(vT, vTB, vtok, vtokB, d):
                nper = 16 // d
                for r_ in range(d):
                    for m_ in range(nper):
                        g = r_ * nper + m_
                        bi = (g // 8) % 2
                        o_ = psb[bi][:, (g % 8) * 128:(g % 8 + 1) * 128]
                        tr(o_, vT[:, gslice(r_, m_, d)], ident_bf[:, :], r=[vTB, cB], w=[psb_b[bi]])
                        cp(vtok[:, g, :, 0:64], o_.rearrange("p (h c) -> p h c", c=64), r=[psb_b[bi]], w=[vtokB],
                           eng=("act" if g % 2 else "dve"))

            astate = {"s": 0, "o": 0, "p": 0, "e": 0}
            pstate = {}

            def attend(pts, qT, qB, kT, kB, vtok, vtokB, vh, W, d, acc, accB, first):
                nper = 16 // d
                wi_ = 0 if W == 128 else 1
                tiles = [(r_, m_) for r_ in range(d) for m_ in range(nper)]
                otmp, otmpB = pts[0][2], pts[0][3]

                def emit_S(r_, m_):
                    qs = gslice(r_, m_, d)
                    dls = [dl for dl in (-1, 0, 1) if 0 <= m_ + dl < nper]
                    c0, n = (dls[0] + 1) * 128, len(dls) * 128
                    si = astate["s"] % 4
                    astate["s"] += 1
                    pi = astate["p"] % len(pts)
                    astate["p"] += 1
                    mm(ps[si][:, 0:n], ident_bf[:, :], maskb[:, wi_, c0:c0 + n], True, False, r=[cB], w=[ps_b[si]])
                    for j, dl in enumerate(dls):
                        mm(ps[si][:, j * 128:(j + 1) * 128], kT[:, gslice(r_, m_ + dl, d)], qT[:, qs], False, j == len(dls) - 1,
                           r=[kB, qB], w=[ps_b[si]])
                    act(pts[pi][0][:, 0:n], ps[si][:, 0:n], AF.Exp, r=[ps_b[si]], w=[pts[pi][1]], scale=0.125)
                    return dls, pi

                def emit_PV(ti, oi, r_, m_, dls, pi):
                    o_ = ps[oi][:, ti * 128:(ti + 1) * 128]
                    for j, dl in enumerate(dls):
                        g = r_ * nper + m_ + dl
                        mm(o_, vtok[:, g, vh, :], pts[pi][0][:, j * 128:(j + 1) * 128], j == 0, j == len(dls) - 1,
                           r=[vtokB, pts[pi][1]], w=[ps_b[oi]])

                def evac(grp, oi):
                    n = len(grp) * 128
                    src = ps[oi][:, 0:n]
                    r0, m0 = grp[0]
                    if d == 1:
                        dst = acc[:, m0 * 128:m0 * 128 + n]
                        view = lambda a: a
                    elif d == 4:
                        dst = acc[:, slice(r0, r0 + 4 * (n - 1) + 1, 4)]
                        view = lambda a: a
                    else:
                        dst = acc[:, :].rearrange("p (i r) -> p r i", r=16)[:, r0:r0 + len(grp), :]
                        view = lambda a: a.rearrange("p (t i) -> p t i", i=128)
                    if first:
                        cp(dst, view(src), r=[ps_b[oi]], w=[accB])
                    else:
                        oj = astate["e"] % 2
                        astate["e"] += 1
                        cp(otmp[oj][:, 0:n], src, r=[ps_b[oi]], w=[otmpB[oj]], eng="act")
                        tt(dst, dst, view(otmp[oj][:, 0:n]), ALU.add, r=[otmpB[oj], accB], w=[accB])

                prev = None
                for gi in range(0, len(tiles), 4):
                    grp = tiles[gi:gi + 4]
                    oi = 6 + astate["o"] % 2
                    astate["o"] += 1
                    for ti, (r_, m_) in enumerate(grp):
                        dls, pi = emit_S(r_, m_)
                        if prev is not None:
                            emit_PV(*prev[0])
                            if prev[1] is not None:
                                evac(*prev[1])
                        prev = ((ti, oi, r_, m_, dls, pi), (grp, oi) if ti == len(grp) - 1 else None)
                emit_PV(*prev[0])
                evac(*prev[1])

            def attn_finish(fs, acc, accB, yall, yallB, chunk, base, esink_col):
                dtmp, rd0, fB = fs
                for tb in range(4):
                    blk = slice(tb * 512, (tb + 1) * 512)
                    if esink_col is not None:
                        ts(dtmp[64:128, :], acc[64:128, blk], esink_col, None, ALU.add, r=[accB, sinkB], w=[fB])
                        act(dtmp[64:128, :], dtmp[64:128, :], AF.Ln, r=[fB], w=[fB])
                    else:
                        act(dtmp[64:128, :], acc[64:128, blk], AF.Ln, r=[accB], w=[fB])
                    act(dtmp[64:128, :], dtmp[64:128, :], AF.Exp, r=[fB], w=[fB], scale=-1.0)
                    cp(rd0[0:64, :], dtmp[64:128, :], r=[fB], w=[fB])
                    tt(yall[base:base + 64, chunk, blk], acc[0:64, blk], rd0[0:64, :], ALU.mult, r=[accB, fB], w=[yallB])

            def attn_alloc(ph):
                d_ = {}
                d_["rs"] = (sbuf(ph, "qr", [128, 512], BF16), Buf(), sbuf(ph, "rt1", [128, 512], F32), Buf(),
                            sbuf(ph, "rt2", [128, 512], F32), Buf())
                otmp = [sbuf(ph, "otmp%d" % i, [128, 512], F32) for i in range(2)]
                otmpB = [Buf(), Buf()]
                d_["pts"] = [(sbuf(ph, "pt%d" % i, [128, 384], BF16), Buf(), otmp, otmpB) for i in range(4)]
                d_["fs"] = (sbuf(ph, "dtmp", [128, 512], F32), sbuf(ph, "rd0", [128, 512], F32), Buf())
                d_["acc"] = sbuf(ph, "acc", [128, SEQ], F32)
                d_["accB"] = Buf()
                d_["yall"] = sbuf(ph, "yall", [128, 4, SEQ], BF16)
                d_["yallB"] = Buf()
                d_["ga"] = gn_alloc(ph, 512)
                d_["qm"] = [sbuf(ph, "qm%d" % i, [128, SEQ], BF16) for i in range(2)]
                d_["qmB"] = [Buf(), Buf()]
                d_["vT"] = sbuf(ph, "vT", [128, SEQ], BF16)
                d_["vTB"] = Buf()
                return d_

            if "swa" not in skip:
              with contextlib.ExitStack() as ph:
                A = attn_alloc(ph)
                kA = sbuf(ph, "kA", [128, SEQ], BF16)
                kAB = Buf()
                vtok = sbuf(ph, "vtok", [128, NT, 2, 128], BF16)
                vtokB = Buf()
                esink = sbuf(ph, "esink", [128, 8], F32)
                sinkB = Buf()
                S.dma("sp", esink[:], sinkT[l], w=[sinkB])
                act(esink[:], esink[:], AF.Exp, r=[sinkB], w=[sinkB])
                memset(vtok[:, :, :, 64:128], 1.0, [vtokB])
                pstate["ws"] = proj_stream([2048, 2176] + [1536 + c_ * 128 for c_ in range(4)])
                if stop_after == "swa_0":
                    return fin()
                proj_fm(2048)
                if stop_after == "swa_p":
                    return fin()
                rope_evac(A["rs"], kA, kAB)
                if stop_after == "swa_k":
                    return fin()
                proj_fm(2176); plain_evac(A["vT"], A["vTB"])
                v_to_tok(A["vT"], A["vTB"], vtok, vtokB, 1)
                if stop_after == "swa_v":
                    return fin()
                for chunk in range(4):
                    kv = chunk // 2
                    for hh in range(2):
                        memset(A["qm"][hh][:], 0.0, [A["qmB"][hh]])
                    proj_fm(1536 + chunk * 128)
                    rope_evac(A["rs"], None, None, placed=[(A["qm"][hh], A["qmB"][hh], hh * 64, kv * 64) for hh in range(2)])
                    for hh in range(2):
                        h_ = chunk * 2 + hh
                        if stop_after == "swa_q":
                            return fin()
                        attend(A["pts"], A["qm"][hh], A["qmB"][hh], kA, kAB, vtok, vtokB, kv, 128, 1, A["acc"], A["accB"], True)
                        if stop_after == "swa_a":
                            return fin()
                        attn_finish(A["fs"], A["acc"], A["accB"], A["yall"], A["yallB"], chunk, hh * 64, esink[64:128, h_:h_ + 1])
                        if stop_after == "swa_f":
                            return fin()
                        if stop_after == "swa_f2" and hh == 1:
                            return fin()
                for tb in range(4):
                    gn_store(A["ga"], A["yall"][:, :, tb * 512:(tb + 1) * 512], A["yallB"], 2, tb * 512, 512, gn_sb)
                S.barrier()

            if "dil" not in skip:
              with contextlib.ExitStack() as ph:
                A = attn_alloc(ph)
                kT = sbuf(ph, "kT", [128, SEQ], BF16)
                kTB = Buf()
                vtok1 = sbuf(ph, "vtokd", [128, NT, 2, 128], BF16)
                vtok1B = Buf()
                acc2 = sbuf(ph, "acc2", [128, SEQ], F32)
                accs = [(A["acc"], A["accB"]), (acc2, Buf())]
                sinkB = None
                memset(vtok1[:, :, :, 64:128], 1.0, [vtok1B])
                pstate["ws"] = proj_stream([c0_ + c_ * 128 for c_ in range(4) for c0_ in (2304, 2816, 3328)])
                for chunk in range(4):
                    if chunk == 0:
                        for hh in range(2):
                            memset(A["qm"][hh][:], 0.0, [A["qmB"][hh]])
                    proj_fm(2304 + chunk * 128)
                    rope_evac(A["rs"], None, None, placed=[(A["qm"][hh], A["qmB"][hh], hh * 64, hh * 64) for hh in range(2)])
                    proj_fm(2816 + chunk * 128); rope_evac(A["rs"], kT, kTB)
                    proj_fm(3328 + chunk * 128); plain_evac(A["vT"], A["vTB"])
                    for ci, d_ in enumerate((1, 4, 16)):
                        v_to_tok(A["vT"], A["vTB"], vtok1, vtok1B, d_)
                        for hh in range(2):
                            attend(A["pts"], A["qm"][hh], A["qmB"][hh], kT, kTB, vtok1, vtok1B, hh, 64, d_,
                                   accs[hh][0], accs[hh][1], ci == 0)
                    for hh in range(2):
                        attn_finish(A["fs"], accs[hh][0], accs[hh][1], A["yall"], A["yallB"], chunk, hh * 64, None)
                for tb in range(4):
                    gn_store(A["ga"], A["yall"][:, :, tb * 512:(tb + 1) * 512], A["yallB"], 3, tb * 512, 512, gn_sb)
                S.barrier()
            if stop_after == "mix":
                break

            with contextlib.ExitStack() as ph:
                woB = Buf()
                wo = w_out[l].rearrange("(ec p) d -> p ec d", p=128)
                woQ = [Buf("wo%d" % q4) for q4 in range(4)]
                for q4 in range(4):
                    S.dma("pool", hT[:, q4 * 4:(q4 + 1) * 4, :], wo[:, q4 * 4:(q4 + 1) * 4, :], w=[woQ[q4]])
                gmbc = sbuf(ph, "gmbc", [128, D], F32)
                gmB = Buf()
                bcast_row(ph, gmbc, modT[:, 32:48], modB, gmB)
                ynt = [sbuf(ph, "ynt%d" % i, [128, 16, 128], BF16) for i in range(2)]
                yntB = [Buf(), Buf()]
                xt = [sbuf(ph, "dxt%d" % i, [128, D], F32) for i in range(2)]
                xtB = [Buf(), Buf()]
                tmp = [sbuf(ph, "dtmp%d" % i, [128, 512], F32) for i in range(2)]
                tmpB = [Buf(), Buf()]
                def ld_tile(k_):
                    S.dma("sp", ynt[k_ % 2][:], ynT_d[:, k_], r=[ynB], w=[yntB[k_ % 2]])
                    S.dma("sp", xt[k_ % 2][:], x_src[k_ * 128:(k_ + 1) * 128, :], r=[xres_b[k_]], w=[xtB[k_ % 2]])

                ld_tile(0)
                for t_ in range(NT):
                    i = t_ % 2
                    if t_ + 1 < NT:
                        ld_tile(t_ + 1)
                    for db in range(4):
                        dblk = slice(db * 512, (db + 1) * 512)
                        ti = db % 2
                        for ec in range(16):
                            mm(ps[db][:, :], ynt[i][:, ec, :], hT[:, ec, dblk], ec == 0, ec == 15, r=[yntB[i], woQ[ec // 4]], w=[ps_b[db]])
                        cp(tmp[ti][:], ps[db][:, :], r=[ps_b[db]], w=[tmpB[ti]], eng="act")
                        tt(tmp[ti][:], tmp[ti][:], gmbc[:, dblk], ALU.mult, r=[gmB], w=[tmpB[ti]])
                        tt(xt[i][:, dblk], xt[i][:, dblk], tmp[ti][:], ALU.add, r=[tmpB[ti]], w=[xtB[i]], eng="pool")
                    S.dma("sp", xres[t_ * 128:(t_ + 1) * 128, :], xt[i][:], r=[xtB[i]], w=[xres_b[t_]])
                S.barrier()
            if stop_after == "wout":
                break

            norm_to_hT(xres, gsf, 48, also_h2=True)
            with contextlib.ExitStack() as ph:
                idxT = sbuf(ph, "idxT", [128, 2, 16], U32)
                gateT = sbuf(ph, "gateT", [128, 2, 16], F32)
                rtB = Buf()
                gfbc = sbuf(ph, "gfbc", [128, D], F32)
                gfB = Buf()
                xw = sbuf(ph, "xwbuf", [128, NK * 512], BF16)
                xwB = Buf("xw")
                ws = WStream([(wbuf[i][:, :], wbuf_b[i]) for i in range(NWB)] + [(xw[:, :], xwB)])
                for e in range(N_EXP):
                    wgv = wg_in[l, e].rearrange("(kt p) f -> p kt f", p=128)
                    wuv = wu_in[l, e].rearrange("(kt p) f -> p kt f", p=128)
                    wdv = wd_in[l, e].rearrange("(ft p) d -> p ft d", p=128)
                    for half in range(2):
                        ws.request((e, "g", half), wgv[:, :, half * 512:(half + 1) * 512], 512)
                        ws.request((e, "u", half), wuv[:, :, half * 512:(half + 1) * 512], 512)
                    for dh in range(2):
                        ws.request((e, "d", dh), wdv[:, :, dh * 1024:(dh + 1) * 1024], 1024, 8)
                ws.pump()
                with contextlib.ExitStack() as ph2:
                    bcast_row(ph2, gfbc, modT[:, 80:96], modB, gfB)
                    rw = sbuf(ph2, "rw", [128, NK, N_EXP], BF16)
                    rwB = Buf()
                    S.dma("pool", rw[:], router_w[l].rearrange("(kt p) e -> p kt e", p=128), w=[rwB])
                    affp = sbuf(ph2, "affp", [128, 128], F32)
                    affB = Buf()
                    memset(affp[:], 0.0, [affB])
                    lg = sbuf(ph2, "lg", [128, 16], F32)
                    ex = sbuf(ph2, "ex", [128, 16], F32)
                    st_ = sbuf(ph2, "st_", [128, 4], F32)
                    lgB = Buf()
                    affT = sbuf(ph2, "affT", [128, SEQ], F32)
                    affTB = Buf()
                    for t_ in range(NT):
                        for kt in range(NK):
                            mm(ps[0][:, 0:16], hT[:, kt, t_ * 128:(t_ + 1) * 128], rw[:, kt, :], kt == 0, kt == NK - 1,
                               r=[hT_b[t_], rwB], w=[ps_b[0]])
                        cp(lg[:], ps[0][:, 0:16], r=[ps_b[0]], w=[lgB], eng="act")
                        S.op("dve", lambda: nc.vector.reduce_max(out=st_[:, 0:1], in_=lg[:], axis=AX.X), [lgB], [lgB])
                        ts(st_[:, 1:2], st_[:, 0:1], -1.0, None, ALU.mult, r=[lgB], w=[lgB])
                        act(ex[:], lg[:], AF.Exp, r=[lgB], w=[lgB], bias=st_[:, 1:2], accum_out=st_[:, 2:3])
                        S.op("dve", lambda: nc.vector.reciprocal(out=st_[:, 3:4], in_=st_[:, 2:3]), [lgB], [lgB])
                        ts(affp[:, 0:16], ex[:], st_[:, 3:4], None, ALU.mult, r=[lgB], w=[affB])
                        tr(ps[1][:, 0:128], affp[:, :], ident_f[:, :], r=[affB, cB], w=[ps_b[1]])
                        cp(affT[0:16, t_ * 128:(t_ + 1) * 128], ps[1][0:16, 0:128], r=[ps_b[1]], w=[affTB], eng="act")
                    ws.add_bufs(hT_as_wbufs()[0:3], hT_b)
                    dump("affT%d" % l, affT[0:16, :], [16, SEQ], F32, [affTB])
                    vals = sbuf(ph2, "vals", [128, CAP], F32)
                    idxu = sbuf(ph2, "idxu", [128, CAP], U32)
                    idxf = sbuf(ph2, "idxf", [128, CAP], F32)
                    tkB = Buf()
                    memset(vals[:], 0.0, [tkB])
                    memset(idxf[:], 0.0, [tkB])
                    for it in range(CAP // 8):
                        sl = slice(it * 8, it * 8 + 8)
                        S.op("dve", lambda sl=sl: nc.vector.max(out=vals[0:16, sl], in_=affT[0:16, :]), [affTB], [tkB])
                        S.op("dve", lambda sl=sl: nc.vector.max_index(out=idxu[0:16, sl], in_max=vals[0:16, sl], in_values=affT[0:16, :]),
                             [affTB, tkB], [tkB])
                        S.op("dve", lambda sl=sl: nc.vector.match_replace(out=affT[0:16, :], in_to_replace=vals[0:16, sl],
                                                                     in_values=affT[0:16, :], imm_value=-1.0), [tkB], [affTB])
                    cp(idxf[0:16, :], idxu[0:16, :], r=[tkB], w=[tkB])
                    for hf in range(2):
                        tr(ps[2][:, hf * 128:(hf + 1) * 128], idxf[:, hf * 128:(hf + 1) * 128], ident_f[:, :], r=[tkB, cB], w=[ps_b[2]])
                        tr(ps[3][:, hf * 128:(hf + 1) * 128], vals[:, hf * 128:(hf + 1) * 128], ident_f[:, :], r=[tkB, cB], w=[ps_b[3]])
                        cp(idxT[:, hf, :], ps[2][:, hf * 128:hf * 128 + 16], r=[ps_b[2]], w=[rtB])
                        cp(gateT[:, hf, :], ps[3][:, hf * 128:hf * 128 + 16], r=[ps_b[3]], w=[rtB])
                    dump("idxT%d" % l, idxT[:], [128, 2, 16], U32, [rtB])
                    dump("gateT%d" % l, gateT[:], [128, 2, 16], F32, [rtB])
                    S.barrier()
                if stop_after == "route":
                    return fin()
                xe = [hT[:, 12 + i, :] for i in range(4)]
                xeB = [Buf() for _ in range(4)]
                xeT2 = [sbuf(ph, "xeT%d" % i, [128, NK, CAP], BF16) for i in range(2)]
                xeT2B = [Buf(), Buf()]
                gT = sbuf(ph, "gT", [128, 8, CAP], BF16)
                gTB = Buf()
                sa = [sbuf(ph, "sa%d" % i, [128, CAP], F32) for i in range(2)]
                saB = [Buf(), Buf()]
                osb = [sbuf(ph, "osb%d" % i, [128, D], F32) for i in range(2)]
                osbB = [Buf(), Buf()]
                ob = [sbuf(ph, "ob%d" % i, [128, 512], F32) for i in range(2)]
                obB = [Buf(), Buf()]

                def gather(e):
                    for hf in range(2):
                        k = (e % 2) * 2 + hf
                        S.dma("pool", None, None, r=[h2B, rtB], w=[xeB[k]],
                              indirect=lambda hf=hf, e=e, k=k: nc.gpsimd.indirect_dma_start(
                                  out=xe[k], out_offset=None, in_=h2_d,
                                  in_offset=bass.IndirectOffsetOnAxis(ap=idxT[:, hf, e:e + 1], axis=0)))

                def build_xeT(e):
                    for hf in range(2):
                        k = (e % 2) * 2 + hf
                        for hb in range(2):
                            for jj in range(8):
                                j = hb * 8 + jj
                                tr(psb[hb][:, jj * 128:(jj + 1) * 128], xe[k][:, j * 128:(j + 1) * 128], ident_bf[:, :],
                                   r=[xeB[k], cB], w=[psb_b[hb]])
                            for jj in range(8):
                                j = hb * 8 + jj
                                ts(xeT2[e % 2][:, j, hf * 128:(hf + 1) * 128], psb[hb][:, jj * 128:(jj + 1) * 128],
                                   gsf[:, j:j + 1], modT[:, 48 + j:49 + j], ALU.mult, ALU.add, r=[psb_b[hb], modB], w=[xeT2B[e % 2]])

                gather(0)
                ws.pump()
                build_xeT(0)
                gather(1)
                for e in range(N_EXP):
                    xeT, xeTB = xeT2[e % 2], xeT2B[e % 2]
                    for half in range(2):
                        wg_, wgb = ws.get((e, "g", half))
                        wu_, wub = ws.get((e, "u", half))
                        for fl in range(4):
                            fb = half * 4 + fl
                            pi = fb % 2
                            fsl = slice(fl * 128, (fl + 1) * 128)
                            for kt in range(NK):
                                mm(ps[2 * pi][:, 0:CAP], wg_[:, kt, fsl], xeT[:, kt, :], kt == 0, kt == NK - 1, r=[wgb, xeTB], w=[ps_b[2 * pi]])
                            for kt in range(NK):
                                mm(ps[2 * pi + 1][:, 0:CAP], wu_[:, kt, fsl], xeT[:, kt, :], kt == 0, kt == NK - 1, r=[wub, xeTB], w=[ps_b[2 * pi + 1]])
                            act(sa[pi][:], ps[2 * pi][:, 0:CAP], AF.Silu, r=[ps_b[2 * pi]], w=[saB[pi]])
                            tt(gT[:, fb, :], ps[2 * pi + 1][:, 0:CAP], sa[pi][:], ALU.mult, r=[ps_b[2 * pi + 1], saB[pi]], w=[gTB])
                        ws.release((e, "g", half))
                        ws.release((e, "u", half))
                        if half == 0 and e + 1 < N_EXP:
                            build_xeT(e + 1)
                            if e + 2 < N_EXP:
                                gather(e + 2)
                    for dh in range(2):
                        wd_, wdb = ws.get((e, "d", dh))
                        for hf in range(2):
                            for dq in range(2):
                                db = dh * 2 + dq
                                oi = (hf * 2 + dq) % 2
                                for ft in range(8):
                                    mm(ps[4 + oi][:, :], gT[:, ft, hf * 128:(hf + 1) * 128], wd_[:, ft, dq * 512:(dq + 1) * 512],
                                       ft == 0, ft == 7, r=[gTB, wdb], w=[ps_b[4 + oi]])
                                ts(ob[oi][:], ps[4 + oi][:, :], gateT[:, hf, e:e + 1], None, ALU.mult, r=[ps_b[4 + oi], rtB], w=[obB[oi]])
                                tt(osb[hf][:, db * 512:(db + 1) * 512], ob[oi][:], gfbc[:, db * 512:(db + 1) * 512], ALU.mult,
                                   r=[obB[oi], gfB], w=[osbB[hf]])
                        ws.release((e, "d", dh))
                    for hf in range(2):
                        S.dma("pool", None, None, r=[osbB[hf], rtB] + xres_b, w=[xresB],
                              indirect=lambda hf=hf, e=e: nc.gpsimd.indirect_dma_start(
                                  out=xres, out_offset=bass.IndirectOffsetOnAxis(ap=idxT[:, hf, e:e + 1], axis=0),
                                  in_=osb[hf][:], in_offset=None, compute_op=ALU.add))
                S.barrier()
                for t_ in range(NT):
                    xres_b[t_].w = xresB.w
            if stop_after == "moe":
                break

        if stop_after is None:
            with contextlib.ExitStack() as ph:
                fbc = sbuf(ph, "fbc", [128, D], F32)
                fB = Buf()
                S.dma("sp", fbc[:], fin_bc, w=[fB])
                xt = [sbuf(ph, "fxt%d" % i, [128, D], F32) for i in range(2)]
                xtB = [Buf(), Buf()]
                junk = sbuf(ph, "fjunk", [128, D], BF16)
                jB = Buf()
                ss = sbuf(ph, "fss", [128, 4], F32)
                ssB = [Buf(), Buf()]
                def ld_f(k_):
                    S.dma("sp", xt[k_ % 2][:], xres[k_ * 128:(k_ + 1) * 128, :], r=[xres_b[k_]], w=[xtB[k_ % 2]])

                ld_f(0)
                for t_ in range(NT):
                    i = t_ % 2
                    if t_ + 1 < NT:
                        ld_f(t_ + 1)
                    act(junk[:], xt[i][:], AF.Square, r=[xtB[i]], w=[jB, ssB[i]], accum_out=ss[:, i:i + 1])
                    rstd_from_ss(ss[:, 2 + i:3 + i], ss[:, i:i + 1], 1.0 / D, ssB[i])
                    ts(xt[i][:], xt[i][:], ss[:, 2 + i:3 + i], None, ALU.mult, r=[ssB[i]], w=[xtB[i]])
                    tt(xt[i][:], xt[i][:], fbc[:], ALU.mult, r=[fB], w=[xtB[i]], eng="pool")
                    S.dma("sp", out_ap[t_ * 128:(t_ + 1) * 128, :], xt[i][:], r=[xtB[i]])

        return fin()


def prep_shared(inp, L=2):
    f = np.float32
    g = {}
    g["ada_w"] = np.ascontiguousarray(inp["ada_w"][:L], dtype=f)
    g["ada_bT"] = np.ascontiguousarray(inp["ada_b"][:L].reshape(L, 96, 128).transpose(0, 2, 1), dtype=f)
    for k, n in (("nmT", "norm_mix"), ("nfT", "norm_ffn"), ("gnT", "group_norm")):
        g[k] = np.ascontiguousarray(inp[n][:L].reshape(L, 16, 128).transpose(0, 2, 1), dtype=f)
    g["w_in"] = np.ascontiguousarray(inp["w_in"][:L], dtype=f)
    g["fnet_wS"] = np.ascontiguousarray(inp["fnet_w"][:L].reshape(L, 4, 128, 64), dtype=f)
    g["sgun_bc"] = np.ascontiguousarray(np.broadcast_to(inp["sgu_norm"][:L][:, None, :], (L, 128, 512)), dtype=f)
    g["sgu_wT"] = np.ascontiguousarray(inp["sgu_w"][:L].transpose(0, 3, 1, 2), dtype=f)
    sb = inp["sgu_b"][:L]
    sbb = sb.reshape(L, 4, 2, 1, 128)
    sbb = np.broadcast_to(sbb, (L, 4, 2, 64, 128)).reshape(L, 4, 128, 128).transpose(0, 2, 1, 3)
    g["sgub_bc"] = np.ascontiguousarray(sbb, dtype=f)
    g["sinkT"] = np.ascontiguousarray(np.broadcast_to(inp["swa_sink"][:L][:, None, :], (L, 128, 8)), dtype=f)
    g["w_out"] = np.ascontiguousarray(inp["w_out"][:L], dtype=f)
    g["router_w"] = np.ascontiguousarray(inp["router_w"][:L], dtype=f)
    g["exp_w_gate"] = np.ascontiguousarray(inp["exp_w_gate"][:L], dtype=f)
    g["exp_w_up"] = np.ascontiguousarray(inp["exp_w_up"][:L], dtype=f)
    g["exp_w_down"] = np.ascontiguousarray(inp["exp_w_down"][:L], dtype=f)
    g["fin_bc"] = np.ascontiguousarray(np.broadcast_to(inp["final_norm"][None, :], (128, D)), dtype=f)
    g.update(host_consts())
    return g


def prep_core(inp, b):
    m = {}
    m["x"] = np.ascontiguousarray(inp["x"][b], dtype=np.float32)
    m["cT"] = np.ascontiguousarray(inp["c"][b].reshape(16, 128).T, dtype=np.float32)
    m["posb"] = np.ascontiguousarray(np.broadcast_to(inp["positions"][b][None, :], (128, SEQ)), dtype=np.int32)
    return m


def kernel(**inputs):
    inp = {k: np.asarray(v) for k, v in inputs.items()}
    nc, _ = build(L=2)
    shared = prep_shared(inp)
    in_maps = []
    for core in range(8):
        m = dict(shared)
        m.update(prep_core(inp, core // 2))
        in_maps.append(m)
    res = run_bass_kernel_spmd(nc, in_maps, core_ids=list(range(8)))
    out = np.stack([np.asarray(res.results[2 * b]["out"]) for b in range(4)], axis=0)
    return out.astype(np.float32)
```

```python
import contextlib
import math
import numpy as np
import ml_dtypes
import concourse.bass as bass
import concourse.mybir as mybir
from concourse.bass_utils import run_bass_kernel_spmd

F32 = mybir.dt.float32
BF16 = mybir.dt.bfloat16
I32 = mybir.dt.int32
U32 = mybir.dt.uint32
AF = mybir.ActivationFunctionType
ALU = mybir.AluOpType
AX = mybir.AxisListType

D = 2048
SEQ = 2048
NT = 16
NK = 16
D_IN = 3840
EPS = 1e-6
NEG = -30000.0
N_EXP = 16
CAP = 256
FF = 1024


class Buf:
    __slots__ = ("name", "w", "r")

    def __init__(self, name=""):
        self.name = name
        self.w = None
        self.r = {}


class Sched:
    def __init__(self, nc, stack, n_dma=24):
        self.nc = nc
        self.engs = {"pe": nc.tensor, "act": nc.scalar, "dve": nc.vector, "pool": nc.gpsimd, "sp": nc.sync}
        self.sems = {k: stack.enter_context(nc.semaphore("s_" + k)) for k in self.engs}
        self.cnt = {k: 0 for k in self.engs}
        self.seen = {k: {} for k in self.engs}
        self.dsems = [stack.enter_context(nc.semaphore("d%d" % i)) for i in range(n_dma)]
        self.dcnt = [0] * n_dma
        self.dnext = 0
        self.qnext = {}
        self.marks = []
        self.nins = 0

    def _wait(self, eng, key, val):
        if key == eng == "pe":
            return
        if self.seen[eng].get(key, 0) >= val:
            return
        sem = self.sems[key] if isinstance(key, str) else self.dsems[key[1]]
        self.engs[eng].wait_ge(sem, val)
        self.seen[eng][key] = val

    def _deps(self, eng, reads, writes):
        for b in reads:
            if b.w is not None:
                self._wait(eng, *b.w)
        for b in writes:
            if b.w is not None:
                self._wait(eng, *b.w)
            for k, v in b.r.items():
                self._wait(eng, k, v)

    def op(self, eng, fn, r=(), w=()):
        self._deps(eng, r, w)
        ins = fn()
        self.cnt[eng] += 1
        self.nins += 1
        ins.then_inc(self.sems[eng], 1)
        c = self.cnt[eng]
        for b in r:
            b.r[eng] = c
        for b in w:
            b.w = (eng, c)
            b.r = {}
        return ins

    def dma(self, q, out, in_, r=(), w=(), indirect=None, **kw):
        lo, hi = (0, 8) if q == "sp" else (8, len(self.dsems))
        nxt = self.qnext.get(q, lo)
        i = nxt
        self.qnext[q] = lo + (nxt + 1 - lo) % (hi - lo)
        if self.dcnt[i] > 0:
            self._wait(q, ("d", i), self.dcnt[i])
        self._deps(q, r, w)
        only_if = kw.pop("only_if", None)
        if only_if is not None:
            eng = self.engs[q]
            with eng.If(only_if):
                ins = eng.dma_start(out=out, in_=in_, **kw)
                ins.then_inc(self.dsems[i], 16)
            with eng.Else():
                eng.memset(out, 0.0).then_inc(self.dsems[i], 16)
            self.dcnt[i] += 16
            self.nins += 1
        else:
            if indirect is None:
                ins = self.engs[q].dma_start(out=out, in_=in_, **kw)
            else:
                ins = indirect()
            self.dcnt[i] += 16
            self.nins += 1
            ins.then_inc(self.dsems[i], 16)
        key = ("d", i)
        for b in r:
            b.r[key] = self.dcnt[i]
        for b in w:
            b.w = (key, self.dcnt[i])
            b.r = {}
        return ins

    def barrier(self):
        self.marks.append(dict(self.cnt))
        for e in self.engs:
            for k in self.engs:
                if k != e and self.cnt[k] > 0:
                    self._wait(e, k, self.cnt[k])
            for i in range(len(self.dsems)):
                if self.dcnt[i] > 0:
                    self._wait(e, ("d", i), self.dcnt[i])

    def wait_all(self, eng):
        for k in self.engs:
            if k != eng and self.cnt[k] > 0:
                self._wait(eng, k, self.cnt[k])
        for i in range(len(self.dsems)):
            if self.dcnt[i] > 0:
                self._wait(eng, ("d", i), self.dcnt[i])


def host_consts():
    bf = ml_dtypes.bfloat16
    c = {}
    c["ident_bf"] = np.eye(128, dtype=np.float32).astype(bf)
    c["ident_f"] = np.eye(128, dtype=np.float32)
    c["ones_bf"] = np.ones((128, 128), np.float32).astype(bf)
    c["ones_f"] = np.ones((128, 128), np.float32)
    a = np.arange(128)[:, None]
    i = np.arange(128)[None, :]
    masks = []
    masks.append(a >= i)
    masks.append(a <= i)
    masks.append(np.abs(a - i) <= 64)
    masks.append(a >= i + 64)
    masks.append(a <= i - 64)
    m = np.stack([np.where(v, 0.0, NEG) for v in masks], axis=1).astype(np.float32)
    z = np.zeros((128, 128), np.float32)
    m3 = np.stack([np.concatenate([m[:, 0], z, m[:, 1]], axis=1),
                   np.concatenate([m[:, 3], m[:, 2], m[:, 4]], axis=1)], axis=1)
    c["maskb"] = m3.astype(bf)
    R = np.zeros((64, 64), np.float32)
    for j in range(8):
        R[j, j + 8] = -1.0
        R[j + 8, j] = 1.0
    RT = R.T
    rot = np.zeros((128, 128), np.float32)
    rot[:64, :64] = RT
    rot[64:, 64:] = RT
    c["rotT"] = rot.astype(bf)
    inv = np.zeros((128, 1), np.float32)
    for p in range(128):
        q = p % 64
        if q < 16:
            inv[p, 0] = np.float32(500000.0) ** (-np.float32(2 * (q % 8)) / np.float32(16))
    c["invf"] = inv
    s = np.arange(2048, dtype=np.float64)
    ang = 2.0 * np.pi * ((s[:, None] * s[None, :]) % 2048) / 2048.0
    c["dftc"] = (np.cos(ang) / np.sqrt(2048.0)).astype(np.float32).astype(bf)
    c["dfts"] = (np.sin(ang) / np.sqrt(2048.0)).astype(np.float32).astype(bf)
    cc = np.arange(64, dtype=np.float64)
    angc = 2.0 * np.pi * ((cc[:, None] * cc[None, :]) % 64) / 64.0
    C = np.cos(angc) / 8.0
    Sn = np.sin(angc) / 8.0
    cb = np.zeros((128, 128)); sbm = np.zeros((128, 128))
    cb[:64, :64] = C; cb[64:, 64:] = C
    sbm[:64, :64] = -Sn; sbm[64:, 64:] = -Sn
    c["dftcc"] = cb.astype(np.float32).astype(bf)
    c["dftsc"] = sbm.astype(np.float32).astype(bf)
    c["iota_f"] = np.tile(np.arange(2048, dtype=np.float32)[None, :], (128, 1))
    return c


CONST_SPECS = [
    ("ident_bf", [128, 128], BF16), ("ident_f", [128, 128], F32), ("ones_bf", [128, 128], BF16),
    ("ones_f", [128, 128], F32), ("maskb", [128, 2, 384], BF16), ("rotT", [128, 128], BF16),
    ("invf", [128, 1], F32), ("dftc", [2048, 2048], BF16), ("dfts", [2048, 2048], BF16),
    ("dftcc", [128, 128], BF16), ("dftsc", [128, 128], BF16),
]


def build(L=2, dbg=(), stop_after=None, skip=()):
    nc = bass.Bass("TRN2", target_bir_lowering=False)
    dbg = set(dbg)

    def din(name, shape, dt):
        return nc.dram_tensor(name, list(shape), dt, kind="ExternalInput").ap()

    x_in = din("x", [SEQ, D], F32)
    cT_in = din("cT", [128, 16], F32)
    posb_in = din("posb", [128, SEQ], I32)
    ada_w = din("ada_w", [L, D, 6 * D], F32)
    ada_bT = din("ada_bT", [L, 128, 96], F32)
    nmT_in = din("nmT", [L, 128, 16], F32)
    nfT_in = din("nfT", [L, 128, 16], F32)
    gnT_in = din("gnT", [L, 128, 16], F32)
    w_in = din("w_in", [L, D, D_IN], F32)
    fnet_wS = din("fnet_wS", [L, 4, 128, 64], F32)
    sgun_bc = din("sgun_bc", [L, 128, 512], F32)
    sgu_wT = din("sgu_wT", [L, 128, 8, 128], F32)
    sgub_bc = din("sgub_bc", [L, 128, 4, 128], F32)
    sinkT = din("sinkT", [L, 128, 8], F32)
    w_out = din("w_out", [L, D, D], F32)
    router_w = din("router_w", [L, D, N_EXP], F32)
    wg_in = din("exp_w_gate", [L, N_EXP, D, FF], F32)
    wu_in = din("exp_w_up", [L, N_EXP, D, FF], F32)
    wd_in = din("exp_w_down", [L, N_EXP, FF, D], F32)
    fin_bc = din("fin_bc", [128, D], F32)
    cst = {n: din(n, s, dt) for (n, s, dt) in CONST_SPECS}
    out_ap = nc.dram_tensor("out", [SEQ, D], F32, kind="ExternalOutput").ap()
    xres = nc.dram_tensor("xres", [SEQ, D], F32, kind="ExternalOutput" if "xres" in dbg else "Internal").ap()
    ynT_d = nc.dram_tensor("ynT_d", [128, NT, 16, 128], BF16, kind="ExternalOutput" if "ynT" in dbg else "Internal").ap()
    h2_d = nc.dram_tensor("h2_d", [SEQ, D], BF16, kind="Internal").ap()
    dbg_outs = {}

    with contextlib.ExitStack() as st:
        S = Sched(nc, st)

        uid = [0]

        def sbuf(stack, name, shape, dt):
            uid[0] += 1
            return stack.enter_context(nc.sbuf_tensor("sb%d_%s" % (uid[0], name), list(shape), dt))

        def psum(stack, name, shape, dt):
            uid[0] += 1
            return stack.enter_context(nc.psum_tensor("pp%d_%s" % (uid[0], name), list(shape), dt))

        def dump(name, ap, shape, dt, rbuf):
            if name not in dbg:
                return
            o = nc.dram_tensor("dbg_" + name, list(shape), dt, kind="ExternalOutput").ap()
            dbg_outs[name] = o
            S.dma("sp", o, ap, r=rbuf)

        def fin():
            S.wait_all("sp")
            dbg_outs["_marks"] = S.marks
            return nc, dbg_outs

        def mm(out, lhsT, rhs, start, stop, r, w):
            return S.op("pe", lambda: nc.tensor.matmul(out, lhsT, rhs, start=start, stop=stop), r, w)

        def tr(out, in_, ident, r, w):
            return S.op("pe", lambda: nc.tensor.transpose(out, in_, ident), r, w)

        def act(out, in_, func, r, w, eng="act", **kw):
            return S.op("act", lambda: nc.scalar.activation(out=out, in_=in_, func=func, **kw), r, w)

        def ts(out, in0, s1, s2, op0, op1=None, r=(), w=(), eng="dve"):
            e = S.engs[eng]
            if op1 is None:
                return S.op(eng, lambda: e.tensor_scalar(out=out, in0=in0, scalar1=s1, scalar2=None, op0=op0), r, w)
            return S.op(eng, lambda: e.tensor_scalar(out=out, in0=in0, scalar1=s1, scalar2=s2, op0=op0, op1=op1), r, w)

        def tt(out, in0, in1, op, r=(), w=(), eng="dve"):
            e = S.engs[eng]
            return S.op(eng, lambda: e.tensor_tensor(out=out, in0=in0, in1=in1, op=op), r, w)

        def stt(out, in0, scalar, in1, op0, op1, r=(), w=()):
            return S.op("dve", lambda: nc.vector.scalar_tensor_tensor(out=out, in0=in0, scalar=scalar, in1=in1, op0=op0, op1=op1), r, w)

        def cp(out, in_, r=(), w=(), eng="dve"):
            if eng == "act":
                return S.op("act", lambda: nc.scalar.copy(out=out, in_=in_), r, w)
            e = S.engs[eng]
            return S.op(eng, lambda: e.tensor_copy(out=out, in_=in_), r, w)

        def memset(ap, val, w, eng="pool"):
            e = S.engs[eng]
            return S.op(eng, lambda: e.memset(ap, val), (), w)

        def rstd_from_ss(out, ss_ap, inv_n, b):
            ts(out, ss_ap, inv_n, EPS, ALU.mult, ALU.add, r=[b], w=[b])
            act(out, out, AF.Sqrt, r=[b], w=[b])
            S.op("dve", lambda: nc.vector.reciprocal(out=out, in_=out), [b], [b])

        is_even = nc.gpsimd.snap(nc.gpsimd.partition_id() % 2 == 0)

        ident_bf = sbuf(st, "ident_bf", [128, 128], BF16)
        ident_f = sbuf(st, "ident_f", [128, 128], F32)
        ones_bf = sbuf(st, "ones_bf", [128, 128], BF16)
        ones_f = sbuf(st, "ones_f", [128, 128], F32)
        maskb = sbuf(st, "maskb", [128, 2, 384], BF16)
        rotT = sbuf(st, "rotT", [128, 128], BF16)
        invf = sbuf(st, "invf", [128, 1], F32)
        dftcc = sbuf(st, "dftcc", [128, 128], BF16)
        dftsc = sbuf(st, "dftsc", [128, 128], BF16)
        cB = Buf("consts")
        for nm, t_ in (("ident_bf", ident_bf), ("ident_f", ident_f), ("ones_bf", ones_bf), ("ones_f", ones_f),
                       ("maskb", maskb), ("rotT", rotT), ("invf", invf), ("dftcc", dftcc), ("dftsc", dftsc)):
            S.dma("sp", t_[:], cst[nm], w=[cB])
        cosT = sbuf(st, "cosT", [128, SEQ], F32)
        sinT = sbuf(st, "sinT", [128, SEQ], F32)
        ropeB = Buf("rope")
        hT = sbuf(st, "hT", [128, NK, SEQ], BF16)
        hT_b = [Buf("hT%d" % i) for i in range(NT)]
        modT = sbuf(st, "modT", [128, 96], F32)
        modB = Buf("mod")
        gsm = sbuf(st, "gsm", [128, 16], F32)
        gsf = sbuf(st, "gsf", [128, 16], F32)
        small = sbuf(st, "small", [128, 64], F32)
        smallB = Buf("small")
        NWB = 2
        wbuf = [sbuf(st, "wbuf%d" % i, [128, NK * 512], BF16) for i in range(NWB)]
        wbuf_b = [Buf("wbuf%d" % i) for i in range(NWB)]
        wstate = {"i": 0, "bufs": list(zip(wbuf, wbuf_b))}

        def wload(src_ap, ncols, kt=NK):
            i = wstate["i"] % len(wstate["bufs"])
            wstate["i"] = i + 1
            wt_, wb_ = wstate["bufs"][i]
            view = wt_[:, 0:kt * ncols].rearrange("p (k n) -> p k n", n=ncols)
            S.dma("pool", view, src_ap, w=[wb_])
            return view, wb_

        class WStream:
            def __init__(self, bufs):
                self.bufs = bufs
                self.free = list(range(len(bufs)))
                self.pending = []
                self.loaded = {}
                self.extra = {}

            def add_bufs(self, bufs, extra_w):
                for tb in bufs:
                    self.bufs.append(tb)
                    self.free.append(len(self.bufs) - 1)
                    self.extra[len(self.bufs) - 1] = list(extra_w)
                self.pump()

            def request(self, key, src_ap, ncols, kt=NK):
                self.pending.append((key, src_ap, ncols, kt))

            def pump(self):
                while self.pending and self.free:
                    key, src_ap, ncols, kt = self.pending.pop(0)
                    i = self.free.pop(0)
                    t_, b_ = self.bufs[i][0], self.bufs[i][1]
                    extra = self.extra.pop(i, [])
                    view = t_[:, 0:kt * ncols].rearrange("p (k n) -> p k n", n=ncols)
                    S.dma("pool", view, src_ap, w=[b_] + extra, only_if=is_even)
                    self.loaded[key] = (view, b_, i)

            def get(self, key):
                self.pump()
                v, b_, _ = self.loaded[key]
                return v, b_

            def release(self, key):
                _, _, i = self.loaded.pop(key)
                self.free.append(i)
                self.pump()

        def hT_as_wbufs():
            return [(hT[:, 4 * i:4 * i + 4, :].rearrange("p a n -> p (a n)"), Buf("hTw%d" % i)) for i in range(4)]

        ps = [psum(st, "ps%d" % i, [128, 512], F32) for i in range(8)]
        ps_b = [Buf("ps%d" % i) for i in range(8)]
        psb = [ps[6][:].bitcast(BF16), ps[7][:].bitcast(BF16)]
        psb_b = [ps_b[6], ps_b[7]]

        with contextlib.ExitStack() as ph:
            posi = sbuf(ph, "posi", [128, SEQ], I32)
            ang = sbuf(ph, "ang", [128, SEQ], F32)
            tmpa = sbuf(ph, "tmpa", [128, SEQ], F32)
            pB, aB, tB = Buf(), Buf(), Buf()
            S.dma("sp", posi[:], posb_in, w=[pB])
            cp(ang[:], posi[:], r=[pB], w=[aB])
            ts(ang[:], ang[:], invf[:, 0:1], None, ALU.mult, r=[aB, cB], w=[aB])
            ki = sbuf(ph, "ki", [128, SEQ], I32)
            kf = sbuf(ph, "kf", [128, SEQ], F32)
            kB = Buf()

            def sin_of(dst, offset):
                ts(tmpa[:], ang[:], offset, None, ALU.add, r=[aB], w=[tB])
                ts(kf[:], tmpa[:], 1.0 / (2 * math.pi), None, ALU.mult, r=[tB], w=[kB])
                cp(ki[:], kf[:], r=[kB], w=[kB])
                cp(kf[:], ki[:], r=[kB], w=[kB])
                stt(tmpa[:], kf[:], -2 * math.pi, tmpa[:], ALU.mult, ALU.add, r=[kB, tB], w=[tB])
                ts(kf[:], tmpa[:], math.pi, None, ALU.is_gt, r=[tB], w=[kB])
                stt(tmpa[:], kf[:], -2 * math.pi, tmpa[:], ALU.mult, ALU.add, r=[kB, tB], w=[tB])
                ts(kf[:], tmpa[:], -math.pi, None, ALU.is_lt, r=[tB], w=[kB])
                stt(tmpa[:], kf[:], 2 * math.pi, tmpa[:], ALU.mult, ALU.add, r=[kB, tB], w=[tB])
                ts(tmpa[:], tmpa[:], math.pi, -math.pi, ALU.min, ALU.max, r=[tB], w=[tB])
                act(dst, tmpa[:], AF.Sin, r=[tB], w=[ropeB])

            sin_of(sinT[:], 0.0)
            sin_of(cosT[:], 0.5 * math.pi)
            dump("cosT", cosT[:], [128, SEQ], F32, [ropeB])
            dump("sinT", sinT[:], [128, SEQ], F32, [ropeB])
            S.barrier()

        xres_b = [Buf("xres%d" % i) for i in range(NT)]
        xresB = Buf("xres_scatter")
        h2B = Buf("h2_d")

        def bcast_row(ph, dst, src, srcB, dstB):
            dg = [sbuf(ph, "dg%d" % i, [128, 128], F32) for i in range(2)]
            dgB = [Buf(), Buf()]
            for j in range(16):
                i = j % 2
                ts(dg[i][:], ident_f[:, :], src[:, j:j + 1], None, ALU.mult, r=[cB, srcB], w=[dgB[i]])
                mm(ps[j // 4][:, (j % 4) * 128:(j % 4 + 1) * 128], ones_f[:, :], dg[i][:], True, True, r=[cB, dgB[i]], w=[ps_b[j // 4]])
                if j % 4 == 3:
                    cp(dst[:, (j // 4) * 512:(j // 4 + 1) * 512], ps[j // 4][:, :], r=[ps_b[j // 4]], w=[dstB], eng="act")

        for l in range(L):
            x_src = x_in if l == 0 else xres
            with contextlib.ExitStack() as ph:
                cT = sbuf(ph, "cT", [128, 16], F32)
                scb = sbuf(ph, "scb", [128, 16], BF16)
                abT = sbuf(ph, "abT", [128, 96], F32)
                nm = sbuf(ph, "nm", [128, 16], F32)
                nf = sbuf(ph, "nf", [128, 16], F32)
                b1, b2, b3 = Buf(), Buf(), Buf()
                S.dma("sp", cT[:], cT_in, w=[b1])
                S.dma("sp", abT[:], ada_bT[l], w=[b3])
                S.dma("sp", nm[:], nmT_in[l], w=[b3])
                S.dma("sp", nf[:], nfT_in[l], w=[b3])
                act(scb[:], cT[:], AF.Silu, r=[b1], w=[b2])
                aw = ada_w[l].rearrange("(kt p) n -> p kt n", p=128)
                ws = WStream([(wbuf[i][:, :], wbuf_b[i]) for i in range(NWB)] + hT_as_wbufs())
                for cb in range(24):
                    ws.request(cb, aw[:, :, cb * 512:(cb + 1) * 512], 512)
                for cb in range(24):
                    wt, wb = ws.get(cb)
                    for sc in range(4):
                        j = cb * 4 + sc
                        for kt in range(NK):
                            mm(ps[0][:, j:j + 1], wt[:, kt, sc * 128:(sc + 1) * 128], scb[:, kt:kt + 1],
                               kt == 0, kt == NK - 1, r=[wb, b2], w=[ps_b[0]])
                    ws.release(cb)
                tt(modT[:], ps[0][:, 0:96], abT[:], ALU.add, r=[ps_b[0], b3], w=[modB])
                stt(gsm[:], modT[:, 16:32], 1.0, nm[:], ALU.add, ALU.mult, r=[modB, b3], w=[modB])
                stt(gsf[:], modT[:, 64:80], 1.0, nf[:], ALU.add, ALU.mult, r=[modB, b3], w=[modB])
                dump("modT%d" % l, modT[:], [128, 96], F32, [modB])
                S.barrier()
            if stop_after == "mod":
                break

            def norm_to_hT(x_src, gs, shift_off, also_h2=False):
                with contextlib.ExitStack() as ph:
                    xt = [sbuf(ph, "xt%d" % i, [128, D], F32) for i in range(2)]
                    xt_b = [Buf(), Buf()]
                    xn = [sbuf(ph, "xn%d" % i, [128, D], BF16) for i in range(2)]
                    xn_b = [Buf(), Buf()]
                    junk = sbuf(ph, "junk", [128, D], BF16)
                    jB = Buf()
                    ss = sbuf(ph, "ss", [128, 4], F32)
                    ssB = [Buf(), Buf()]
                    def ld_x(k_):
                        S.dma("sp", xt[k_ % 2][:], x_src[k_ * 128:(k_ + 1) * 128, :], r=[xres_b[k_]], w=[xt_b[k_ % 2]])

                    ld_x(0)
                    for t_ in range(NT):
                        i = t_ % 2
                        if t_ + 1 < NT:
                            ld_x(t_ + 1)
                        act(junk[:], xt[i][:], AF.Square, r=[xt_b[i]], w=[jB, ssB[i]], accum_out=ss[:, i:i + 1])
                        rstd_from_ss(ss[:, 2 + i:3 + i], ss[:, i:i + 1], 1.0 / D, ssB[i])
                        ts(xn[i][:], xt[i][:], ss[:, 2 + i:3 + i], None, ALU.mult, r=[xt_b[i], ssB[i]], w=[xn_b[i]])
                        if also_h2:
                            S.dma("sp", h2_d[t_ * 128:(t_ + 1) * 128, :], xn[i][:], r=[xn_b[i]], w=[h2B])
                        for hb in range(2):
                            for jj in range(8):
                                j = hb * 8 + jj
                                tr(psb[hb][:, jj * 128:(jj + 1) * 128], xn[i][:, j * 128:(j + 1) * 128], ident_bf[:, :],
                                   r=[xn_b[i], cB], w=[psb_b[hb]])
                            for jj in range(8):
                                j = hb * 8 + jj
                                if hb == 0:
                                    ts(hT[:, j, t_ * 128:(t_ + 1) * 128], psb[hb][:, jj * 128:(jj + 1) * 128],
                                       gs[:, j:j + 1], modT[:, shift_off + j:shift_off + j + 1], ALU.mult, ALU.add,
                                       r=[psb_b[hb], modB], w=[hT_b[t_]])
                                else:
                                    act(hT[:, j, t_ * 128:(t_ + 1) * 128], psb[hb][:, jj * 128:(jj + 1) * 128], AF.Identity,
                                        r=[psb_b[hb], modB], w=[hT_b[t_]], scale=gs[:, j:j + 1],
                                        bias=modT[:, shift_off + j:shift_off + j + 1])
                    S.barrier()

            norm_to_hT(x_src, gsm, 0)
            dump("hT%d" % l, hT[:], [128, NK, SEQ], BF16, hT_b)
            if stop_after == "hT":
                break

            wi = w_in[l].rearrange("(kt p) n -> p kt n", p=128)

            def gn_alloc(ph, N):
                return (sbuf(ph, "gn_sq", [128, 4, N], BF16), sbuf(ph, "gn_rs", [128, N], F32),
                        sbuf(ph, "gn_yn", [128, 4, N], BF16), Buf(), Buf(), Buf())

            def gn_store(ga, ysrc, yB, m, tok0, N, gn):
                sq, rs, yn, b_sq, b_rs, b_yn = ga
                for ch in range(4):
                    act(sq[:, ch, :], ysrc[:, ch, :], AF.Square, r=[yB], w=[b_sq])
                for ch in range(4):
                    mm(ps[5][:, 0:N], ones_bf[:, :], sq[:, ch, :], ch == 0, ch == 3, r=[b_sq, cB], w=[ps_b[5]])
                ts(rs[:], ps[5][:, 0:N], 1.0 / 512, EPS, ALU.mult, ALU.add, r=[ps_b[5]], w=[b_rs])
                act(rs[:], rs[:], AF.Ln, r=[b_rs], w=[b_rs])
                act(rs[:], rs[:], AF.Exp, r=[b_rs], w=[b_rs], scale=-0.5)
                for ch in range(4):
                    stt(yn[:, ch, :], ysrc[:, ch, :], gn[:, 4 * m + ch:4 * m + ch + 1], rs[:], ALU.mult, ALU.mult,
                        r=[yB, b_rs, gnB], w=[b_yn])
                t0 = tok0 // 128
                for ch in range(4):
                    S.dma("sp", ynT_d[:, t0:t0 + N // 128, 4 * m + ch, :],
                          yn[:, ch, :].rearrange("p (a t) -> p a t", t=128), r=[b_yn], w=[ynB])

            def gelu(dst, src_ps, xs, t1, r, w, bx, bt):
                cp(xs, src_ps, r=r, w=[bx], eng="act")
                tt(t1, xs, xs, ALU.mult, r=[bx], w=[bt])
                ts(t1, t1, 0.044715, 1.0, ALU.mult, ALU.add, r=[bt], w=[bt])
                tt(t1, t1, xs, ALU.mult, r=[bt, bx], w=[bt])
                act(t1, t1, AF.Sigmoid, r=[bt], w=[bt], scale=1.5957691216057308)
                tt(dst, t1, xs, ALU.mult, r=[bt, bx], w=w)

            gn_sb = sbuf(st, "gn%d" % l, [128, 16], F32)
            gnB = Buf()
            S.dma("sp", gn_sb[:], gnT_in[l], w=[gnB])
            ynB = Buf("ynT_d")

            if "fnet" not in skip:
              with contextlib.ExitStack() as ph:
                xa = sbuf(ph, "xa", [128, NT, 512], BF16)
                xaB = Buf()
                fw = sbuf(ph, "fw", [128, 4, 64], BF16)
                fwB = Buf()
                AB = sbuf(ph, "AB", [128, 8, 128], BF16)
                ABb = Buf()
                dC = [sbuf(ph, "dC%d" % i, [128, NK, 256], BF16) for i in range(2)]
                dS = [sbuf(ph, "dS%d" % i, [128, NK, 256], BF16) for i in range(2)]
                dB = [Buf(), Buf()]
                pq = sbuf(ph, "pq", [128, 4, 512], BF16)
                pqB = Buf()
                ya = sbuf(ph, "ya", [128, 4, 256], F32)
                yaB = Buf()
                ga = gn_alloc(ph, 256)
                S.dma("pool", fw[:], fnet_wS[l].rearrange("c p d -> p c d"), w=[fwB])
                memset(AB[:], 0.0, [ABb])
                for ch in range(4):
                    for which, tab in ((0, dftcc), (1, dftsc)):
                        mm(ps[4][:, 0:64], tab[:, :], fw[:, ch, :], True, True, r=[fwB, cB], w=[ps_b[4]])
                        cp(AB[0:64, which * 4 + ch, 0:64], ps[4][0:64, 0:64], r=[ps_b[4]], w=[ABb])
                        cp(AB[64:128, which * 4 + ch, 64:128], ps[4][64:128, 0:64], r=[ps_b[4]], w=[ABb])
                wt, wb = wload(wi[:, :, 0:512], 512)
                for t_ in range(NT):
                    pi = t_ % 2
                    for kt in range(NK):
                        mm(ps[pi][:, :], hT[:, kt, t_ * 128:(t_ + 1) * 128], wt[:, kt, :], kt == 0, kt == NK - 1,
                           r=[hT_b[t_], wb], w=[ps_b[pi]])
                    cp(xa[:, t_, :], ps[pi][:, :], r=[ps_b[pi]], w=[xaB], eng="act")
                dcv = cst["dftc"].rearrange("(kt p) n -> p kt n", p=128)
                dsv = cst["dfts"].rearrange("(kt p) n -> p kt n", p=128)
                def ld_tab(k_):
                    S.dma("sp", dC[k_ % 2][:], dcv[:, :, k_ * 256:(k_ + 1) * 256], w=[dB[k_ % 2]])
                    S.dma("sp", dS[k_ % 2][:], dsv[:, :, k_ * 256:(k_ + 1) * 256], w=[dB[k_ % 2]])

                ld_tab(0)
                for sbk in range(8):
                    bi = sbk % 2
                    if sbk + 1 < 8:
                        ld_tab(sbk + 1)
                    for ch in range(4):
                        for which, tab in ((0, dC[bi]), (1, dS[bi])):
                            for kt in range(NK):
                                mm(ps[ch][:, which * 256:(which + 1) * 256], xa[:, kt, ch * 128:(ch + 1) * 128],
                                   tab[:, kt, :], kt == 0, kt == NK - 1, r=[xaB, dB[bi]], w=[ps_b[ch]])
                        cp(pq[:, ch, :], ps[ch][:, :], r=[ps_b[ch]], w=[pqB], eng="act")
                    for ch in range(4):
                        o_ = ps[4][:, (ch % 2) * 256:(ch % 2 + 1) * 256] if ch < 2 else ps[6][:, (ch % 2) * 256:(ch % 2 + 1) * 256]
                        ob = ps_b[4] if ch < 2 else ps_b[6]
                        mm(o_, AB[:, ch, :], pq[:, ch, 0:256], True, False, r=[ABb, pqB], w=[ob])
                        mm(o_, AB[:, 4 + ch, :], pq[:, ch, 256:512], False, True, r=[ABb, pqB], w=[ob])
                        cp(ya[:, ch, :], o_, r=[ob], w=[yaB])
                    gn_store(ga, ya[:], yaB, 0, sbk * 256, 256, gn_sb)
                S.barrier()

            if "gmlp" not in skip:
              with contextlib.ExitStack() as ph:
                sgun = sbuf(ph, "sgun", [128, 512], F32)
                sguw = sbuf(ph, "sguw", [128, 8, 128], BF16)
                sgub = sbuf(ph, "sgub", [128, 4, 128], F32)
                gB = Buf()
                S.dma("sp", sgun[:], sgun_bc[l], w=[gB])
                S.dma("pool", sguw[:], sgu_wT[l], w=[gB])
                S.dma("sp", sgub[:], sgub_bc[l], w=[gB])
                wu, wub = wload(wi[:, :, 512:1024], 512)
                wv, wvb = wload(wi[:, :, 1024:1536], 512)
                uT = sbuf(ph, "uT", [128, 4, 512], F32)
                uB = Buf()
                yb = sbuf(ph, "yb", [128, 4, 512], F32)
                ybB = Buf()
                xs = sbuf(ph, "gxs", [128, 512], F32)
                t1 = sbuf(ph, "gt1", [128, 512], F32)
                bx, bt = Buf(), Buf()
                vs = sbuf(ph, "vs", [128, 512], F32)
                vB = Buf()
                vn = sbuf(ph, "vn", [128, 512], BF16)
                vnB = Buf()
                sv = sbuf(ph, "sv", [128, 2], F32)
                svB = Buf()
                zt = sbuf(ph, "zt", [128, 4, 128], F32)
                ztB = Buf()
                ga = gn_alloc(ph, 512)
                for tb in range(4):
                    for ch in range(4):
                        for kt in range(NK):
                            mm(ps[ch][:, :], wu[:, kt, ch * 128:(ch + 1) * 128], hT[:, kt, tb * 512:(tb + 1) * 512],
                               kt == 0, kt == NK - 1, r=[wub] + hT_b[tb * 4:tb * 4 + 4], w=[ps_b[ch]])
                        act(uT[:, ch, :], ps[ch][:, :], AF.Gelu_apprx_tanh, r=[ps_b[ch]], w=[uB])
                    for pc in range(4):
                        t_ = tb * 4 + pc
                        for kt in range(NK):
                            mm(ps[4][:, :], hT[:, kt, t_ * 128:(t_ + 1) * 128], wv[:, kt, :], kt == 0, kt == NK - 1,
                               r=[wvb, hT_b[t_]], w=[ps_b[4]])
                        act(vs[:], ps[4][:, :], AF.Gelu_apprx_tanh, r=[ps_b[4]], w=[vB])
                        act(t1[:], vs[:], AF.Square, r=[vB], w=[bt, svB], accum_out=sv[:, 0:1])
                        rstd_from_ss(sv[:, 1:2], sv[:, 0:1], 1.0 / 512, svB)
                        stt(vn[:], vs[:], sv[:, 1:2], sgun[:], ALU.mult, ALU.mult, r=[vB, svB, gB], w=[vnB])
                        for h_ in range(8):
                            ch, hh = h_ // 2, h_ % 2
                            mm(ps[5][hh * 64:(hh + 1) * 64, ch * 128:(ch + 1) * 128], vn[:, h_ * 64:(h_ + 1) * 64],
                               sguw[:, h_, :], True, True, r=[vnB, gB], w=[ps_b[5]])
                        tt(zt[:], ps[5][:, :].rearrange("p (c t) -> p c t", t=128), sgub[:], ALU.add, r=[ps_b[5], gB], w=[ztB])
                        tt(yb[:, :, pc * 128:(pc + 1) * 128], zt[:], uT[:, :, pc * 128:(pc + 1) * 128], ALU.mult,
                           r=[ztB, uB], w=[ybB])
                    gn_store(ga, yb[:], ybB, 1, tb * 512, 512, gn_sb)
                S.barrier()
            if stop_after == "mix01":
                break

            def proj_stream(cols):
                subs = [(wbuf[i][:, j * 2048:(j + 1) * 2048], Buf("wsub%d_%d" % (i, j))) for i in range(NWB) for j in range(4)]
                ws_ = WStream(subs)
                for c_ in cols:
                    ws_.request(c_, wi[:, :, c_:c_ + 128], 128)
                ws_.pump()
                return ws_

            def proj_fm(col0, swap=False):
                wt, wb = pstate["ws"].get(col0)
                pstate["rel"] = col0
                for tb in range(4):
                    rr = [wb] + hT_b[tb * 4:tb * 4 + 4]
                    rhs = lambda kt: hT[:, kt, tb * 512:(tb + 1) * 512]
                    if not swap:
                        for kt in range(NK):
                            mm(ps[tb][:, :], wt[:, kt, :], rhs(kt), kt == 0, kt == NK - 1, r=rr, w=[ps_b[tb]])
                    else:
                        for kt in range(NK):
                            mm(ps[tb][0:64, :], wt[:, kt, 64:128], rhs(kt), kt == 0, kt == NK - 1, r=rr, w=[ps_b[tb]])
                        for kt in range(NK):
                            mm(ps[tb][64:128, :], wt[:, kt, 0:64], rhs(kt), kt == 0, kt == NK - 1, r=rr, w=[ps_b[tb]])
                pstate["ws"].release(col0)

            def rope_evac(rs_, dst, dstB, placed=None):
                qr, qrB, t1, t1B, t2, t2B = rs_
                for tb in range(4):
                    blk = slice(tb * 512, (tb + 1) * 512)
                    pj = 4 + tb % 2
                    cp(qr[:], ps[tb][:, :], r=[ps_b[tb]], w=[qrB], eng="act")
                    mm(ps[pj][:, :], rotT[:, :], qr[:], True, True, r=[qrB, cB], w=[ps_b[pj]])
                    cp(t1[:], ps[tb][:, :], r=[ps_b[tb]], w=[t1B], eng="act")
                    cp(t2[:], ps[pj][:, :], r=[ps_b[pj]], w=[t2B], eng="act")
                    tt(t1[:], t1[:], cosT[:, blk], ALU.mult, r=[ropeB], w=[t1B])
                    tt(t2[:], t2[:], sinT[:, blk], ALU.mult, r=[ropeB], w=[t2B], eng="pool")
                    if placed is None:
                        tt(dst[:, blk], t1[:], t2[:], ALU.add, r=[t1B, t2B], w=[dstB])
                    else:
                        for (tl, tlB, sb_, tg_) in placed:
                            tt(tl[tg_:tg_ + 64, blk], t1[sb_:sb_ + 64, :], t2[sb_:sb_ + 64, :], ALU.add,
                               r=[t1B, t2B], w=[tlB])

            def plain_evac(dst, dstB):
                for tb in range(4):
                    cp(dst[:, tb * 512:(tb + 1) * 512], ps[tb][:, :], r=[ps_b[tb]], w=[dstB], eng="act")

            def gslice(r_, m_, d):
                start = r_ + d * 128 * m_
                return slice(start, start + 128) if d == 1 else slice(start, start + d * 127 + 1, d)

            def v_to_tok(vT, vTB, vtok, vtokB, d):
                nper = 16 // d
                for r_ in range(d):
                    for m_ in range(nper):
                        g = r_ * nper + m_
                        bi = (g // 8) % 2
                        o_ = psb[bi][:, (g % 8) * 128:(g % 8 + 1) * 128]
                        tr(o_, vT[:, gslice(r_, m_, d)], ident_bf[:, :], r=[vTB, cB], w=[psb_b[bi]])
                        cp(vtok[:, g, :, 0:64], o_.rearrange("p (h c) -> p h c", c=64), r=[psb_b[bi]], w=[vtokB],
                           eng=("act" if g % 2 else "dve"))

            astate = {"s": 0, "o": 0, "p": 0, "e": 0}
            pstate = {}

            def attend(pts, qT, qB, kT, kB, vtok, vtokB, vh, W, d, acc, accB, first):
                nper = 16 // d
                wi_ = 0 if W == 128 else 1
                tiles = [(r_, m_) for r_ in range(d) for m_ in range(nper)]
                otmp, otmpB = pts[0][2], pts[0][3]

                def emit_S(r_, m_):
                    qs = gslice(r_, m_, d)
                    dls = [dl for dl in (-1, 0, 1) if 0 <= m_ + dl < nper]
                    c0, n = (dls[0] + 1) * 128, len(dls) * 128
                    si = astate["s"] % 4
                    astate["s"] += 1
                    pi = astate["p"] % len(pts)
                    astate["p"] += 1
                    mm(ps[si][:, 0:n], ident_bf[:, :], maskb[:, wi_, c0:c0 + n], True, False, r=[cB], w=[ps_b[si]])
                    for j, dl in enumerate(dls):
                        mm(ps[si][:, j * 128:(j + 1) * 128], kT[:, gslice(r_, m_ + dl, d)], qT[:, qs], False, j == len(dls) - 1,
                           r=[kB, qB], w=[ps_b[si]])
                    act(pts[pi][0][:, 0:n], ps[si][:, 0:n], AF.Exp, r=[ps_b[si]], w=[pts[pi][1]], scale=0.125)
                    return dls, pi

                def emit_PV(ti, oi, r_, m_, dls, pi):
                    o_ = ps[oi][:, ti * 128:(ti + 1) * 128]
                    for j, dl in enumerate(dls):
                        g = r_ * nper + m_ + dl
                        mm(o_, vtok[:, g, vh, :], pts[pi][0][:, j * 128:(j + 1) * 128], j == 0, j == len(dls) - 1,
                           r=[vtokB, pts[pi][1]], w=[ps_b[oi]])

                def evac(grp, oi):
                    n = len(grp) * 128
                    src = ps[oi][:, 0:n]
                    r0, m0 = grp[0]
                    if d == 1:
                        dst = acc[:, m0 * 128:m0 * 128 + n]
                        view = lambda a: a
                    elif d == 4:
                        dst = acc[:, slice(r0, r0 + 4 * (n - 1) + 1, 4)]
                        view = lambda a: a
                    else:
                        dst = acc[:, :].rearrange("p (i r) -> p r i", r=16)[:, r0:r0 + len(grp), :]
                        view = lambda a: a.rearrange("p (t i) -> p t i", i=128)
                    if first:
                        cp(dst, view(src), r=[ps_b[oi]], w=[accB])
                    else:
                        oj = astate["e"] % 2
                        astate["e"] += 1
                        cp(otmp[oj][:, 0:n], src, r=[ps_b[oi]], w=[otmpB[oj]], eng="act")
                        tt(dst, dst, view(otmp[oj][:, 0:n]), ALU.add, r=[otmpB[oj], accB], w=[accB])

                prev = None
                for gi in range(0, len(tiles), 4):
                    grp = tiles[gi:gi + 4]
                    oi = 6 + astate["o"] % 2
                    astate["o"] += 1
                    for ti, (r_, m_) in enumerate(grp):
                        dls, pi = emit_S(r_, m_)
                        if prev is not None:
                            emit_PV(*prev[0])
                            if prev[1] is not None:
                                evac(*prev[1])
                        prev = ((ti, oi, r_, m_, dls, pi), (grp, oi) if ti == len(grp) - 1 else None)
                emit_PV(*prev[0])
                evac(*prev[1])

            def attn_finish(fs, acc, accB, yall, yallB, chunk, base, esink_col):
                dtmp, rd0, fB = fs
                for tb in range(4):
                    blk = slice(tb * 512, (tb + 1) * 512)
                    if esink_col is not None:
                        ts(dtmp[64:128, :], acc[64:128, blk], esink_col, None, ALU.add, r=[accB, sinkB], w=[fB])
                        act(dtmp[64:128, :], dtmp[64:128, :], AF.Ln, r=[fB], w=[fB])
                    else:
                        act(dtmp[64:128, :], acc[64:128, blk], AF.Ln, r=[accB], w=[fB])
                    act(dtmp[64:128, :], dtmp[64:128, :], AF.Exp, r=[fB], w=[fB], scale=-1.0)
                    cp(rd0[0:64, :], dtmp[64:128, :], r=[fB], w=[fB])
                    tt(yall[base:base + 64, chunk, blk], acc[0:64, blk], rd0[0:64, :], ALU.mult, r=[accB, fB], w=[yallB])

            def attn_alloc(ph):
                d_ = {}
                d_["rs"] = (sbuf(ph, "qr", [128, 512], BF16), Buf(), sbuf(ph, "rt1", [128, 512], F32), Buf(),
                            sbuf(ph, "rt2", [128, 512], F32), Buf())
                otmp = [sbuf(ph, "otmp%d" % i, [128, 512], F32) for i in range(2)]
                otmpB = [Buf(), Buf()]
                d_["pts"] = [(sbuf(ph, "pt%d" % i, [128, 384], BF16), Buf(), otmp, otmpB) for i in range(4)]
                d_["fs"] = (sbuf(ph, "dtmp", [128, 512], F32), sbuf(ph, "rd0", [128, 512], F32), Buf())
                d_["acc"] = sbuf(ph, "acc", [128, SEQ], F32)
                d_["accB"] = Buf()
                d_["yall"] = sbuf(ph, "yall", [128, 4, SEQ], BF16)
                d_["yallB"] = Buf()
                d_["ga"] = gn_alloc(ph, 512)
                d_["qm"] = [sbuf(ph, "qm%d" % i, [128, SEQ], BF16) for i in range(2)]
                d_["qmB"] = [Buf(), Buf()]
                d_["vT"] = sbuf(ph, "vT", [128, SEQ], BF16)
                d_["vTB"] = Buf()
                return d_

            if "swa" not in skip:
              with contextlib.ExitStack() as ph:
                A = attn_alloc(ph)
                kA = sbuf(ph, "kA", [128, SEQ], BF16)
                kAB = Buf()
                vtok = sbuf(ph, "vtok", [128, NT, 2, 128], BF16)
                vtokB = Buf()
                esink = sbuf(ph, "esink", [128, 8], F32)
                sinkB = Buf()
                S.dma("sp", esink[:], sinkT[l], w=[sinkB])
                act(esink[:], esink[:], AF.Exp, r=[sinkB], w=[sinkB])
                memset(vtok[:, :, :, 64:128], 1.0, [vtokB])
                pstate["ws"] = proj_stream([2048, 2176] + [1536 + c_ * 128 for c_ in range(4)])
                if stop_after == "swa_0":
                    return fin()
                proj_fm(2048)
                if stop_after == "swa_p":
                    return fin()
                rope_evac(A["rs"], kA, kAB)
                if stop_after == "swa_k":
                    return fin()
                proj_fm(2176); plain_evac(A["vT"], A["vTB"])
                v_to_tok(A["vT"], A["vTB"], vtok, vtokB, 1)
                if stop_after == "swa_v":
                    return fin()
                for chunk in range(4):
                    kv = chunk // 2
                    for hh in range(2):
                        memset(A["qm"][hh][:], 0.0, [A["qmB"][hh]])
                    proj_fm(1536 + chunk * 128)
                    rope_evac(A["rs"], None, None, placed=[(A["qm"][hh], A["qmB"][hh], hh * 64, kv * 64) for hh in range(2)])
                    for hh in range(2):
                        h_ = chunk * 2 + hh
                        if stop_after == "swa_q":
                            return fin()
                        attend(A["pts"], A["qm"][hh], A["qmB"][hh], kA, kAB, vtok, vtokB, kv, 128, 1, A["acc"], A["accB"], True)
                        if stop_after == "swa_a":
                            return fin()
                        attn_finish(A["fs"], A["acc"], A["accB"], A["yall"], A["yallB"], chunk, hh * 64, esink[64:128, h_:h_ + 1])
                        if stop_after == "swa_f":
                            return fin()
                        if stop_after == "swa_f2" and hh == 1:
                            return fin()
                for tb in range(4):
                    gn_store(A["ga"], A["yall"][:, :, tb * 512:(tb + 1) * 512], A["yallB"], 2, tb * 512, 512, gn_sb)
                S.barrier()

            if "dil" not in skip:
              with contextlib.ExitStack() as ph:
                A = attn_alloc(ph)
                kT = sbuf(ph, "kT", [128, SEQ], BF16)
                kTB = Buf()
                vtok1 = sbuf(ph, "vtokd", [128, NT, 2, 128], BF16)
                vtok1B = Buf()
                acc2 = sbuf(ph, "acc2", [128, SEQ], F32)
                accs = [(A["acc"], A["accB"]), (acc2, Buf())]
                sinkB = None
                memset(vtok1[:, :, :, 64:128], 1.0, [vtok1B])
                pstate["ws"] = proj_stream([c0_ + c_ * 128 for c_ in range(4) for c0_ in (2304, 2816, 3328)])
                for chunk in range(4):
                    if chunk == 0:
                        for hh in range(2):
                            memset(A["qm"][hh][:], 0.0, [A["qmB"][hh]])
                    proj_fm(2304 + chunk * 128)
                    rope_evac(A["rs"], None, None, placed=[(A["qm"][hh], A["qmB"][hh], hh * 64, hh * 64) for hh in range(2)])
                    proj_fm(2816 + chunk * 128); rope_evac(A["rs"], kT, kTB)
                    proj_fm(3328 + chunk * 128); plain_evac(A["vT"], A["vTB"])
                    for ci, d_ in enumerate((1, 4, 16)):
                        v_to_tok(A["vT"], A["vTB"], vtok1, vtok1B, d_)
                        for hh in range(2):
                            attend(A["pts"], A["qm"][hh], A["qmB"][hh], kT, kTB, vtok1, vtok1B, hh, 64, d_,
                                   accs[hh][0], accs[hh][1], ci == 0)
                    for hh in range(2):
                        attn_finish(A["fs"], accs[hh][0], accs[hh][1], A["yall"], A["yallB"], chunk, hh * 64, None)
                for tb in range(4):
                    gn_store(A["ga"], A["yall"][:, :, tb * 512:(tb + 1) * 512], A["yallB"], 3, tb * 512, 512, gn_sb)
                S.barrier()
            if stop_after == "mix":
                break

            with contextlib.ExitStack() as ph:
                woB = Buf()
                wo = w_out[l].rearrange("(ec p) d -> p ec d", p=128)
                woQ = [Buf("wo%d" % q4) for q4 in range(4)]
                for q4 in range(4):
                    S.dma("pool", hT[:, q4 * 4:(q4 + 1) * 4, :], wo[:, q4 * 4:(q4 + 1) * 4, :], w=[woQ[q4]])
                gmbc = sbuf(ph, "gmbc", [128, D], F32)
                gmB = Buf()
                bcast_row(ph, gmbc, modT[:, 32:48], modB, gmB)
                ynt = [sbuf(ph, "ynt%d" % i, [128, 16, 128], BF16) for i in range(2)]
                yntB = [Buf(), Buf()]
                xt = [sbuf(ph, "dxt%d" % i, [128, D], F32) for i in range(2)]
                xtB = [Buf(), Buf()]
                tmp = [sbuf(ph, "dtmp%d" % i, [128, 512], F32) for i in range(2)]
                tmpB = [Buf(), Buf()]
                def ld_tile(k_):
                    S.dma("sp", ynt[k_ % 2][:], ynT_d[:, k_], r=[ynB], w=[yntB[k_ % 2]])
                    S.dma("sp", xt[k_ % 2][:], x_src[k_ * 128:(k_ + 1) * 128, :], r=[xres_b[k_]], w=[xtB[k_ % 2]])

                ld_tile(0)
                for t_ in range(NT):
                    i = t_ % 2
                    if t_ + 1 < NT:
                        ld_tile(t_ + 1)
                    for db in range(4):
                        dblk = slice(db * 512, (db + 1) * 512)
                        ti = db % 2
                        for ec in range(16):
                            mm(ps[db][:, :], ynt[i][:, ec, :], hT[:, ec, dblk], ec == 0, ec == 15, r=[yntB[i], woQ[ec // 4]], w=[ps_b[db]])
                        cp(tmp[ti][:], ps[db][:, :], r=[ps_b[db]], w=[tmpB[ti]], eng="act")
                        tt(tmp[ti][:], tmp[ti][:], gmbc[:, dblk], ALU.mult, r=[gmB], w=[tmpB[ti]])
                        tt(xt[i][:, dblk], xt[i][:, dblk], tmp[ti][:], ALU.add, r=[tmpB[ti]], w=[xtB[i]], eng="pool")
                    S.dma("sp", xres[t_ * 128:(t_ + 1) * 128, :], xt[i][:], r=[xtB[i]], w=[xres_b[t_]])
                S.barrier()
            if stop_after == "wout":
                break

            norm_to_hT(xres, gsf, 48, also_h2=True)
            with contextlib.ExitStack() as ph:
                idxT = sbuf(ph, "idxT", [128, 2, 16], U32)
                gateT = sbuf(ph, "gateT", [128, 2, 16], F32)
                rtB = Buf()
                gfbc = sbuf(ph, "gfbc", [128, D], F32)
                gfB = Buf()
                xw = sbuf(ph, "xwbuf", [128, NK * 512], BF16)
                xwB = Buf("xw")
                ws = WStream([(wbuf[i][:, :], wbuf_b[i]) for i in range(NWB)] + [(xw[:, :], xwB)])
                for e in range(N_EXP):
                    wgv = wg_in[l, e].rearrange("(kt p) f -> p kt f", p=128)
                    wuv = wu_in[l, e].rearrange("(kt p) f -> p kt f", p=128)
                    wdv = wd_in[l, e].rearrange("(ft p) d -> p ft d", p=128)
                    for half in range(2):
                        ws.request((e, "g", half), wgv[:, :, half * 512:(half + 1) * 512], 512)
                        ws.request((e, "u", half), wuv[:, :, half * 512:(half + 1) * 512], 512)
                    for dh in range(2):
                        ws.request((e, "d", dh), wdv[:, :, dh * 1024:(dh + 1) * 1024], 1024, 8)
                ws.pump()
                with contextlib.ExitStack() as ph2:
                    bcast_row(ph2, gfbc, modT[:, 80:96], modB, gfB)
                    rw = sbuf(ph2, "rw", [128, NK, N_EXP], BF16)
                    rwB = Buf()
                    S.dma("pool", rw[:], router_w[l].rearrange("(kt p) e -> p kt e", p=128), w=[rwB])
                    affp = sbuf(ph2, "affp", [128, 128], F32)
                    affB = Buf()
                    memset(affp[:], 0.0, [affB])
                    lg = sbuf(ph2, "lg", [128, 16], F32)
                    ex = sbuf(ph2, "ex", [128, 16], F32)
                    st_ = sbuf(ph2, "st_", [128, 4], F32)
                    lgB = Buf()
                    affT = sbuf(ph2, "affT", [128, SEQ], F32)
                    affTB = Buf()
                    for t_ in range(NT):
                        for kt in range(NK):
                            mm(ps[0][:, 0:16], hT[:, kt, t_ * 128:(t_ + 1) * 128], rw[:, kt, :], kt == 0, kt == NK - 1,
                               r=[hT_b[t_], rwB], w=[ps_b[0]])
                        cp(lg[:], ps[0][:, 0:16], r=[ps_b[0]], w=[lgB], eng="act")
                        S.op("dve", lambda: nc.vector.reduce_max(out=st_[:, 0:1], in_=lg[:], axis=AX.X), [lgB], [lgB])
                        ts(st_[:, 1:2], st_[:, 0:1], -1.0, None, ALU.mult, r=[lgB], w=[lgB])
                        act(ex[:], lg[:], AF.Exp, r=[lgB], w=[lgB], bias=st_[:, 1:2], accum_out=st_[:, 2:3])
                        S.op("dve", lambda: nc.vector.reciprocal(out=st_[:, 3:4], in_=st_[:, 2:3]), [lgB], [lgB])
                        ts(affp[:, 0:16], ex[:], st_[:, 3:4], None, ALU.mult, r=[lgB], w=[affB])
                        tr(ps[1][:, 0:128], affp[:, :], ident_f[:, :], r=[affB, cB], w=[ps_b[1]])
                        cp(affT[0:16, t_ * 128:(t_ + 1) * 128], ps[1][0:16, 0:128], r=[ps_b[1]], w=[affTB], eng="act")
                    ws.add_bufs(hT_as_wbufs()[0:3], hT_b)
                    dump("affT%d" % l, affT[0:16, :], [16, SEQ], F32, [affTB])
                    vals = sbuf(ph2, "vals", [128, CAP], F32)
                    idxu = sbuf(ph2, "idxu", [128, CAP], U32)
                    idxf = sbuf(ph2, "idxf", [128, CAP], F32)
                    tkB = Buf()
                    memset(vals[:], 0.0, [tkB])
                    memset(idxf[:], 0.0, [tkB])
                    for it in range(CAP // 8):
                        sl = slice(it * 8, it * 8 + 8)
                        S.op("dve", lambda sl=sl: nc.vector.max(out=vals[0:16, sl], in_=affT[0:16, :]), [affTB], [tkB])
                        S.op("dve", lambda sl=sl: nc.vector.max_index(out=idxu[0:16, sl], in_max=vals[0:16, sl], in_values=affT[0:16, :]),
                             [affTB, tkB], [tkB])
                        S.op("dve", lambda sl=sl: nc.vector.match_replace(out=affT[0:16, :], in_to_replace=vals[0:16, sl],
                                                                     in_values=affT[0:16, :], imm_value=-1.0), [tkB], [affTB])
                    cp(idxf[0:16, :], idxu[0:16, :], r=[tkB], w=[tkB])
                    for hf in range(2):
                        tr(ps[2][:, hf * 128:(hf + 1) * 128], idxf[:, hf * 128:(hf + 1) * 128], ident_f[:, :], r=[tkB, cB], w=[ps_b[2]])
                        tr(ps[3][:, hf * 128:(hf + 1) * 128], vals[:, hf * 128:(hf + 1) * 128], ident_f[:, :], r=[tkB, cB], w=[ps_b[3]])
                        cp(idxT[:, hf, :], ps[2][:, hf * 128:hf * 128 + 16], r=[ps_b[2]], w=[rtB])
                        cp(gateT[:, hf, :], ps[3][:, hf * 128:hf * 128 + 16], r=[ps_b[3]], w=[rtB])
                    dump("idxT%d" % l, idxT[:], [128, 2, 16], U32, [rtB])
                    dump("gateT%d" % l, gateT[:], [128, 2, 16], F32, [rtB])
                    S.barrier()
                if stop_after == "route":
                    return fin()
                xe = [sbuf(ph, "xe%d" % i, [128, D], BF16)[:, :] for i in range(2)] + [hT[:, 12, :], hT[:, 13, :]]
                xeB = [Buf() for _ in range(4)]
                xeT = sbuf(ph, "xeT", [128, NK, CAP], BF16)
                xeTB = Buf()
                gT = sbuf(ph, "gT", [128, 8, CAP], BF16)
                gTB = Buf()
                sa = [sbuf(ph, "sa%d" % i, [128, CAP], F32) for i in range(2)]
                ub = [sbuf(ph, "ub%d" % i, [128, CAP], F32) for i in range(2)]
                saB = [Buf(), Buf()]
                osb = [sbuf(ph, "osb%d" % i, [128, D], F32) for i in range(2)]
                osbB = [Buf(), Buf()]
                ob = [sbuf(ph, "ob%d" % i, [128, 512], F32) for i in range(2)]
                obB = [Buf(), Buf()]

                def gather(e):
                    for hf in range(2):
                        k = (e % 2) * 2 + hf
                        S.dma("pool", None, None, r=[h2B, rtB], w=[xeB[k]],
                              indirect=lambda hf=hf, e=e, k=k: nc.gpsimd.indirect_dma_start(
                                  out=xe[k], out_offset=None, in_=h2_d,
                                  in_offset=bass.IndirectOffsetOnAxis(ap=idxT[:, hf, e:e + 1], axis=0)))

                gather(0)
                ws.pump()
                for e in range(N_EXP):
                    for hf in range(2):
                        k = (e % 2) * 2 + hf
                        for hb in range(2):
                            for jj in range(8):
                                j = hb * 8 + jj
                                tr(psb[hb][:, jj * 128:(jj + 1) * 128], xe[k][:, j * 128:(j + 1) * 128], ident_bf[:, :],
                                   r=[xeB[k], cB], w=[psb_b[hb]])
                            for jj in range(8):
                                j = hb * 8 + jj
                                ts(xeT[:, j, hf * 128:(hf + 1) * 128], psb[hb][:, jj * 128:(jj + 1) * 128],
                                   gsf[:, j:j + 1], modT[:, 48 + j:49 + j], ALU.mult, ALU.add, r=[psb_b[hb], modB], w=[xeTB])
                    if e + 1 < N_EXP:
                        gather(e + 1)
                    for half in range(2):
                        wg_, wgb = ws.get((e, "g", half))
                        wu_, wub = ws.get((e, "u", half))
                        for fl in range(4):
                            fb = half * 4 + fl
                            pi = fb % 2
                            fsl = slice(fl * 128, (fl + 1) * 128)
                            for kt in range(NK):
                                mm(ps[2 * pi][:, 0:CAP], wg_[:, kt, fsl], xeT[:, kt, :], kt == 0, kt == NK - 1, r=[wgb, xeTB], w=[ps_b[2 * pi]])
                            for kt in range(NK):
                                mm(ps[2 * pi + 1][:, 0:CAP], wu_[:, kt, fsl], xeT[:, kt, :], kt == 0, kt == NK - 1, r=[wub, xeTB], w=[ps_b[2 * pi + 1]])
                            act(sa[pi][:], ps[2 * pi][:, 0:CAP], AF.Silu, r=[ps_b[2 * pi]], w=[saB[pi]])
                            tt(gT[:, fb, :], ps[2 * pi + 1][:, 0:CAP], sa[pi][:], ALU.mult, r=[ps_b[2 * pi + 1], saB[pi]], w=[gTB])
                        ws.release((e, "g", half))
                        ws.release((e, "u", half))
                    for dh in range(2):
                        wd_, wdb = ws.get((e, "d", dh))
                        for hf in range(2):
                            for dq in range(2):
                                db = dh * 2 + dq
                                oi = (hf * 2 + dq) % 2
                                for ft in range(8):
                                    mm(ps[4 + oi][:, :], gT[:, ft, hf * 128:(hf + 1) * 128], wd_[:, ft, dq * 512:(dq + 1) * 512],
                                       ft == 0, ft == 7, r=[gTB, wdb], w=[ps_b[4 + oi]])
                                ts(ob[oi][:], ps[4 + oi][:, :], gateT[:, hf, e:e + 1], None, ALU.mult, r=[ps_b[4 + oi], rtB], w=[obB[oi]])
                                tt(osb[hf][:, db * 512:(db + 1) * 512], ob[oi][:], gfbc[:, db * 512:(db + 1) * 512], ALU.mult,
                                   r=[obB[oi], gfB], w=[osbB[hf]])
                        ws.release((e, "d", dh))
                    for hf in range(2):
                        S.dma("pool", None, None, r=[osbB[hf], rtB] + xres_b, w=[xresB],
                              indirect=lambda hf=hf, e=e: nc.gpsimd.indirect_dma_start(
                                  out=xres, out_offset=bass.IndirectOffsetOnAxis(ap=idxT[:, hf, e:e + 1], axis=0),
                                  in_=osb[hf][:], in_offset=None, compute_op=ALU.add))
                S.barrier()
                for t_ in range(NT):
                    xres_b[t_].w = xresB.w
            if stop_after == "moe":
                break

        if stop_after is None:
            with contextlib.ExitStack() as ph:
                fbc = sbuf(ph, "fbc", [128, D], F32)
                fB = Buf()
                S.dma("sp", fbc[:], fin_bc, w=[fB])
                xt = [sbuf(ph, "fxt%d" % i, [128, D], F32) for i in range(2)]
                xtB = [Buf(), Buf()]
                junk = sbuf(ph, "fjunk", [128, D], BF16)
                jB = Buf()
                ss = sbuf(ph, "fss", [128, 4], F32)
                ssB = [Buf(), Buf()]
                def ld_f(k_):
                    S.dma("sp", xt[k_ % 2][:], xres[k_ * 128:(k_ + 1) * 128, :], r=[xres_b[k_]], w=[xtB[k_ % 2]])

                ld_f(0)
                for t_ in range(NT):
                    i = t_ % 2
                    if t_ + 1 < NT:
                        ld_f(t_ + 1)
                    act(junk[:], xt[i][:], AF.Square, r=[xtB[i]], w=[jB, ssB[i]], accum_out=ss[:, i:i + 1])
                    rstd_from_ss(ss[:, 2 + i:3 + i], ss[:, i:i + 1], 1.0 / D, ssB[i])
                    ts(xt[i][:], xt[i][:], ss[:, 2 + i:3 + i], None, ALU.mult, r=[ssB[i]], w=[xtB[i]])
                    tt(xt[i][:], xt[i][:], fbc[:], ALU.mult, r=[fB], w=[xtB[i]], eng="pool")
                    S.dma("sp", out_ap[t_ * 128:(t_ + 1) * 128, :], xt[i][:], r=[xtB[i]])

        return fin()


def prep_shared(inp, L=2):
    f = np.float32
    g = {}
    g["ada_w"] = np.ascontiguousarray(inp["ada_w"][:L], dtype=f)
    g["ada_bT"] = np.ascontiguousarray(inp["ada_b"][:L].reshape(L, 96, 128).transpose(0, 2, 1), dtype=f)
    for k, n in (("nmT", "norm_mix"), ("nfT", "norm_ffn"), ("gnT", "group_norm")):
        g[k] = np.ascontiguousarray(inp[n][:L].reshape(L, 16, 128).transpose(0, 2, 1), dtype=f)
    g["w_in"] = np.ascontiguousarray(inp["w_in"][:L], dtype=f)
    g["fnet_wS"] = np.ascontiguousarray(inp["fnet_w"][:L].reshape(L, 4, 128, 64), dtype=f)
    g["sgun_bc"] = np.ascontiguousarray(np.broadcast_to(inp["sgu_norm"][:L][:, None, :], (L, 128, 512)), dtype=f)
    g["sgu_wT"] = np.ascontiguousarray(inp["sgu_w"][:L].transpose(0, 3, 1, 2), dtype=f)
    sb = inp["sgu_b"][:L]
    sbb = sb.reshape(L, 4, 2, 1, 128)
    sbb = np.broadcast_to(sbb, (L, 4, 2, 64, 128)).reshape(L, 4, 128, 128).transpose(0, 2, 1, 3)
    g["sgub_bc"] = np.ascontiguousarray(sbb, dtype=f)
    g["sinkT"] = np.ascontiguousarray(np.broadcast_to(inp["swa_sink"][:L][:, None, :], (L, 128, 8)), dtype=f)
    g["w_out"] = np.ascontiguousarray(inp["w_out"][:L], dtype=f)
    g["router_w"] = np.ascontiguousarray(inp["router_w"][:L], dtype=f)
    g["exp_w_gate"] = np.ascontiguousarray(inp["exp_w_gate"][:L], dtype=f)
    g["exp_w_up"] = np.ascontiguousarray(inp["exp_w_up"][:L], dtype=f)
    g["exp_w_down"] = np.ascontiguousarray(inp["exp_w_down"][:L], dtype=f)
    g["fin_bc"] = np.ascontiguousarray(np.broadcast_to(inp["final_norm"][None, :], (128, D)), dtype=f)
    g.update(host_consts())
    return g


def prep_core(inp, b):
    m = {}
    m["x"] = np.ascontiguousarray(inp["x"][b], dtype=np.float32)
    m["cT"] = np.ascontiguousarray(inp["c"][b].reshape(16, 128).T, dtype=np.float32)
    m["posb"] = np.ascontiguousarray(np.broadcast_to(inp["positions"][b][None, :], (128, SEQ)), dtype=np.int32)
    return m


def kernel(**inputs):
    inp = {k: np.asarray(v) for k, v in inputs.items()}
    nc, _ = build(L=2)
    shared = prep_shared(inp)
    in_maps = []
    for core in range(8):
        m = dict(shared)
        m.update(prep_core(inp, core // 2))
        in_maps.append(m)
    res = run_bass_kernel_spmd(nc, in_maps, core_ids=list(range(8)))
    out = np.stack([np.asarray(res.results[2 * b]["out"]) for b in range(4)], axis=0)
    return out.astype(np.float32)
```

```python
import contextlib
import math
import numpy as np
import ml_dtypes
import concourse.bass as bass
import concourse.mybir as mybir
from concourse.bass_utils import run_bass_kernel_spmd

F32 = mybir.dt.float32
BF16 = mybir.dt.bfloat16
I32 = mybir.dt.int32
U32 = mybir.dt.uint32
AF = mybir.ActivationFunctionType
ALU = mybir.AluOpType
AX = mybir.AxisListType

D = 2048
SEQ = 2048
NT = 16
NK = 16
D_IN = 3840
EPS = 1e-6
NEG = -30000.0
N_EXP = 16
CAP = 256
FF = 1024


class Buf:
    __slots__ = ("name", "w", "r")

    def __init__(self, name=""):
        self.name = name
        self.w = None
        self.r = {}


class Sched:
    def __init__(self, nc, stack, n_dma=24):
        self.nc = nc
        self.engs = {"pe": nc.tensor, "act": nc.scalar, "dve": nc.vector, "pool": nc.gpsimd, "sp": nc.sync}
        self.sems = {k: stack.enter_context(nc.semaphore("s_" + k)) for k in self.engs}
        self.cnt = {k: 0 for k in self.engs}
        self.seen = {k: {} for k in self.engs}
        self.dsems = [stack.enter_context(nc.semaphore("d%d" % i)) for i in range(n_dma)]
        self.dcnt = [0] * n_dma
        self.dnext = 0
        self.qnext = {}
        self.marks = []
        self.nins = 0

    def _wait(self, eng, key, val):
        if key == eng == "pe":
            return
        if self.seen[eng].get(key, 0) >= val:
            return
        sem = self.sems[key] if isinstance(key, str) else self.dsems[key[1]]
        self.engs[eng].wait_ge(sem, val)
        self.seen[eng][key] = val

    def _deps(self, eng, reads, writes):
        for b in reads:
            if b.w is not None:
                self._wait(eng, *b.w)
        for b in writes:
            if b.w is not None:
                self._wait(eng, *b.w)
            for k, v in b.r.items():
                self._wait(eng, k, v)

    def op(self, eng, fn, r=(), w=()):
        self._deps(eng, r, w)
        ins = fn()
        self.cnt[eng] += 1
        self.nins += 1
        ins.then_inc(self.sems[eng], 1)
        c = self.cnt[eng]
        for b in r:
            b.r[eng] = c
        for b in w:
            b.w = (eng, c)
            b.r = {}
        return ins

    def dma(self, q, out, in_, r=(), w=(), indirect=None, **kw):
        lo, hi = (0, 8) if q == "sp" else (8, len(self.dsems))
        nxt = self.qnext.get(q, lo)
        i = nxt
        self.qnext[q] = lo + (nxt + 1 - lo) % (hi - lo)
        if self.dcnt[i] > 0:
            self._wait(q, ("d", i), self.dcnt[i])
        self._deps(q, r, w)
        only_if = kw.pop("only_if", None)
        if only_if is not None:
            eng = self.engs[q]
            with eng.If(only_if):
                ins = eng.dma_start(out=out, in_=in_, **kw)
                ins.then_inc(self.dsems[i], 16)
            with eng.Else():
                eng.memset(out, 0.0).then_inc(self.dsems[i], 16)
            self.dcnt[i] += 16
            self.nins += 1
        else:
            if indirect is None:
                ins = self.engs[q].dma_start(out=out, in_=in_, **kw)
            else:
                ins = indirect()
            self.dcnt[i] += 16
            self.nins += 1
            ins.then_inc(self.dsems[i], 16)
        key = ("d", i)
        for b in r:
            b.r[key] = self.dcnt[i]
        for b in w:
            b.w = (key, self.dcnt[i])
            b.r = {}
        return ins

    def barrier(self):
        self.marks.append(dict(self.cnt))
        for e in self.engs:
            for k in self.engs:
                if k != e and self.cnt[k] > 0:
                    self._wait(e, k, self.cnt[k])
            for i in range(len(self.dsems)):
                if self.dcnt[i] > 0:
                    self._wait(e, ("d", i), self.dcnt[i])

    def wait_all(self, eng):
        for k in self.engs:
            if k != eng and self.cnt[k] > 0:
                self._wait(eng, k, self.cnt[k])
        for i in range(len(self.dsems)):
            if self.dcnt[i] > 0:
                self._wait(eng, ("d", i), self.dcnt[i])


def host_consts():
    bf = ml_dtypes.bfloat16
    c = {}
    c["ident_bf"] = np.eye(128, dtype=np.float32).astype(bf)
    c["ident_f"] = np.eye(128, dtype=np.float32)
    c["ones_bf"] = np.ones((128, 128), np.float32).astype(bf)
    c["ones_f"] = np.ones((128, 128), np.float32)
    a = np.arange(128)[:, None]
    i = np.arange(128)[None, :]
    masks = []
    masks.append(a >= i)
    masks.append(a <= i)
    masks.append(np.abs(a - i) <= 64)
    masks.append(a >= i + 64)
    masks.append(a <= i - 64)
    m = np.stack([np.where(v, 0.0, NEG) for v in masks], axis=1).astype(np.float32)
    z = np.zeros((128, 128), np.float32)
    m3 = np.stack([np.concatenate([m[:, 0], z, m[:, 1]], axis=1),
                   np.concatenate([m[:, 3], m[:, 2], m[:, 4]], axis=1)], axis=1)
    c["maskb"] = m3.astype(bf)
    R = np.zeros((64, 64), np.float32)
    for j in range(8):
        R[j, j + 8] = -1.0
        R[j + 8, j] = 1.0
    RT = R.T
    rot = np.zeros((128, 128), np.float32)
    rot[:64, :64] = RT
    rot[64:, 64:] = RT
    c["rotT"] = rot.astype(bf)
    inv = np.zeros((128, 1), np.float32)
    for p in range(128):
        q = p % 64
        if q < 16:
            inv[p, 0] = np.float32(500000.0) ** (-np.float32(2 * (q % 8)) / np.float32(16))
    c["invf"] = inv
    s = np.arange(2048, dtype=np.float64)
    ang = 2.0 * np.pi * ((s[:, None] * s[None, :]) % 2048) / 2048.0
    c["dftc"] = (np.cos(ang) / np.sqrt(2048.0)).astype(np.float32).astype(bf)
    c["dfts"] = (np.sin(ang) / np.sqrt(2048.0)).astype(np.float32).astype(bf)
    cc = np.arange(64, dtype=np.float64)
    angc = 2.0 * np.pi * ((cc[:, None] * cc[None, :]) % 64) / 64.0
    C = np.cos(angc) / 8.0
    Sn = np.sin(angc) / 8.0
    cb = np.zeros((128, 128)); sbm = np.zeros((128, 128))
    cb[:64, :64] = C; cb[64:, 64:] = C
    sbm[:64, :64] = -Sn; sbm[64:, 64:] = -Sn
    c["dftcc"] = cb.astype(np.float32).astype(bf)
    c["dftsc"] = sbm.astype(np.float32).astype(bf)
    c["iota_f"] = np.tile(np.arange(2048, dtype=np.float32)[None, :], (128, 1))
    return c


CONST_SPECS = [
    ("ident_bf", [128, 128], BF16), ("ident_f", [128, 128], F32), ("ones_bf", [128, 128], BF16),
    ("ones_f", [128, 128], F32), ("maskb", [128, 2, 384], BF16), ("rotT", [128, 128], BF16),
    ("invf", [128, 1], F32), ("dftc", [2048, 2048], BF16), ("dfts", [2048, 2048], BF16),
    ("dftcc", [128, 128], BF16), ("dftsc", [128, 128], BF16),
]


def build(L=2, dbg=(), stop_after=None, skip=()):
    nc = bass.Bass("TRN2", target_bir_lowering=False)
    dbg = set(dbg)

    def din(name, shape, dt):
        return nc.dram_tensor(name, list(shape), dt, kind="ExternalInput").ap()

    x_in = din("x", [SEQ, D], F32)
    cT_in = din("cT", [128, 16], F32)
    posb_in = din("posb", [128, SEQ], I32)
    ada_w = din("ada_w", [L, D, 6 * D], F32)
    ada_bT = din("ada_bT", [L, 128, 96], F32)
    nmT_in = din("nmT", [L, 128, 16], F32)
    nfT_in = din("nfT", [L, 128, 16], F32)
    gnT_in = din("gnT", [L, 128, 16], F32)
    w_in = din("w_in", [L, D, D_IN], F32)
    fnet_wS = din("fnet_wS", [L, 4, 128, 64], F32)
    sgun_bc = din("sgun_bc", [L, 128, 512], F32)
    sgu_wT = din("sgu_wT", [L, 128, 8, 128], F32)
    sgub_bc = din("sgub_bc", [L, 128, 4, 128], F32)
    sinkT = din("sinkT", [L, 128, 8], F32)
    w_out = din("w_out", [L, D, D], F32)
    router_w = din("router_w", [L, D, N_EXP], F32)
    wg_in = din("exp_w_gate", [L, N_EXP, D, FF], F32)
    wu_in = din("exp_w_up", [L, N_EXP, D, FF], F32)
    wd_in = din("exp_w_down", [L, N_EXP, FF, D], F32)
    fin_bc = din("fin_bc", [128, D], F32)
    cst = {n: din(n, s, dt) for (n, s, dt) in CONST_SPECS}
    out_ap = nc.dram_tensor("out", [SEQ, D], F32, kind="ExternalOutput").ap()
    xres = nc.dram_tensor("xres", [SEQ, D], F32, kind="ExternalOutput" if "xres" in dbg else "Internal").ap()
    ynT_d = nc.dram_tensor("ynT_d", [128, NT, 16, 128], BF16, kind="ExternalOutput" if "ynT" in dbg else "Internal").ap()
    h2_d = nc.dram_tensor("h2_d", [SEQ, D], BF16, kind="Internal").ap()
    dbg_outs = {}

    with contextlib.ExitStack() as st:
        S = Sched(nc, st)

        uid = [0]

        def sbuf(stack, name, shape, dt):
            uid[0] += 1
            return stack.enter_context(nc.sbuf_tensor("sb%d_%s" % (uid[0], name), list(shape), dt))

        def psum(stack, name, shape, dt):
            uid[0] += 1
            return stack.enter_context(nc.psum_tensor("pp%d_%s" % (uid[0], name), list(shape), dt))

        def dump(name, ap, shape, dt, rbuf):
            if name not in dbg:
                return
            o = nc.dram_tensor("dbg_" + name, list(shape), dt, kind="ExternalOutput").ap()
            dbg_outs[name] = o
            S.dma("sp", o, ap, r=rbuf)

        def fin():
            S.wait_all("sp")
            dbg_outs["_marks"] = S.marks
            return nc, dbg_outs

        def mm(out, lhsT, rhs, start, stop, r, w):
            return S.op("pe", lambda: nc.tensor.matmul(out, lhsT, rhs, start=start, stop=stop), r, w)

        def tr(out, in_, ident, r, w):
            return S.op("pe", lambda: nc.tensor.transpose(out, in_, ident), r, w)

        def act(out, in_, func, r, w, eng="act", **kw):
            return S.op("act", lambda: nc.scalar.activation(out=out, in_=in_, func=func, **kw), r, w)

        def ts(out, in0, s1, s2, op0, op1=None, r=(), w=(), eng="dve"):
            e = S.engs[eng]
            if op1 is None:
                return S.op(eng, lambda: e.tensor_scalar(out=out, in0=in0, scalar1=s1, scalar2=None, op0=op0), r, w)
            return S.op(eng, lambda: e.tensor_scalar(out=out, in0=in0, scalar1=s1, scalar2=s2, op0=op0, op1=op1), r, w)

        def tt(out, in0, in1, op, r=(), w=(), eng="dve"):
            e = S.engs[eng]
            return S.op(eng, lambda: e.tensor_tensor(out=out, in0=in0, in1=in1, op=op), r, w)

        def stt(out, in0, scalar, in1, op0, op1, r=(), w=()):
            return S.op("dve", lambda: nc.vector.scalar_tensor_tensor(out=out, in0=in0, scalar=scalar, in1=in1, op0=op0, op1=op1), r, w)

        def cp(out, in_, r=(), w=(), eng="dve"):
            if eng == "act":
                return S.op("act", lambda: nc.scalar.copy(out=out, in_=in_), r, w)
            e = S.engs[eng]
            return S.op(eng, lambda: e.tensor_copy(out=out, in_=in_), r, w)

        def memset(ap, val, w, eng="pool"):
            e = S.engs[eng]
            return S.op(eng, lambda: e.memset(ap, val), (), w)

        def rstd_from_ss(out, ss_ap, inv_n, b):
            ts(out, ss_ap, inv_n, EPS, ALU.mult, ALU.add, r=[b], w=[b])
            act(out, out, AF.Sqrt, r=[b], w=[b])
            S.op("dve", lambda: nc.vector.reciprocal(out=out, in_=out), [b], [b])

        is_even = nc.gpsimd.snap(nc.gpsimd.partition_id() % 2 == 0)

        ident_bf = sbuf(st, "ident_bf", [128, 128], BF16)
        ident_f = sbuf(st, "ident_f", [128, 128], F32)
        ones_bf = sbuf(st, "ones_bf", [128, 128], BF16)
        ones_f = sbuf(st, "ones_f", [128, 128], F32)
        maskb = sbuf(st, "maskb", [128, 2, 384], BF16)
        rotT = sbuf(st, "rotT", [128, 128], BF16)
        invf = sbuf(st, "invf", [128, 1], F32)
        dftcc = sbuf(st, "dftcc", [128, 128], BF16)
        dftsc = sbuf(st, "dftsc", [128, 128], BF16)
        cB = Buf("consts")
        for nm, t_ in (("ident_bf", ident_bf), ("ident_f", ident_f), ("ones_bf", ones_bf), ("ones_f", ones_f),
                       ("maskb", maskb), ("rotT", rotT), ("invf", invf), ("dftcc", dftcc), ("dftsc", dftsc)):
            S.dma("sp", t_[:], cst[nm], w=[cB])
        cosT = sbuf(st, "cosT", [128, SEQ], F32)
        sinT = sbuf(st, "sinT", [128, SEQ], F32)
        ropeB = Buf("rope")
        hT = sbuf(st, "hT", [128, NK, SEQ], BF16)
        hT_b = [Buf("hT%d" % i) for i in range(NT)]
        modT = sbuf(st, "modT", [128, 96], F32)
        modB = Buf("mod")
        gsm = sbuf(st, "gsm", [128, 16], F32)
        gsf = sbuf(st, "gsf", [128, 16], F32)
        small = sbuf(st, "small", [128, 64], F32)
        smallB = Buf("small")
        NWB = 2
        wbuf = [sbuf(st, "wbuf%d" % i, [128, NK * 512], BF16) for i in range(NWB)]
        wbuf_b = [Buf("wbuf%d" % i) for i in range(NWB)]
        wstate = {"i": 0, "bufs": list(zip(wbuf, wbuf_b))}

        def wload(src_ap, ncols, kt=NK):
            i = wstate["i"] % len(wstate["bufs"])
            wstate["i"] = i + 1
            wt_, wb_ = wstate["bufs"][i]
            view = wt_[:, 0:kt * ncols].rearrange("p (k n) -> p k n", n=ncols)
            S.dma("pool", view, src_ap, w=[wb_])
            return view, wb_

        class WStream:
            def __init__(self, bufs):
                self.bufs = bufs
                self.free = list(range(len(bufs)))
                self.pending = []
                self.loaded = {}
                self.extra = {}

            def add_bufs(self, bufs, extra_w):
                for tb in bufs:
                    self.bufs.append(tb)
                    self.free.append(len(self.bufs) - 1)
                    self.extra[len(self.bufs) - 1] = list(extra_w)
                self.pump()

            def request(self, key, src_ap, ncols, kt=NK):
                self.pending.append((key, src_ap, ncols, kt))

            def pump(self):
                while self.pending and self.free:
                    key, src_ap, ncols, kt = self.pending.pop(0)
                    i = self.free.pop(0)
                    t_, b_ = self.bufs[i][0], self.bufs[i][1]
                    extra = self.extra.pop(i, [])
                    view = t_[:, 0:kt * ncols].rearrange("p (k n) -> p k n", n=ncols)
                    S.dma("pool", view, src_ap, w=[b_] + extra, only_if=is_even)
                    self.loaded[key] = (view, b_, i)

            def get(self, key):
                self.pump()
                v, b_, _ = self.loaded[key]
                return v, b_

            def release(self, key):
                _, _, i = self.loaded.pop(key)
                self.free.append(i)
                self.pump()

        def hT_as_wbufs():
            return [(hT[:, 4 * i:4 * i + 4, :].rearrange("p a n -> p (a n)"), Buf("hTw%d" % i)) for i in range(4)]

        ps = [psum(st, "ps%d" % i, [128, 512], F32) for i in range(8)]
        ps_b = [Buf("ps%d" % i) for i in range(8)]
        psb = [ps[6][:].bitcast(BF16), ps[7][:].bitcast(BF16)]
        psb_b = [ps_b[6], ps_b[7]]

        with contextlib.ExitStack() as ph:
            posi = sbuf(ph, "posi", [128, SEQ], I32)
            ang = sbuf(ph, "ang", [128, SEQ], F32)
            tmpa = sbuf(ph, "tmpa", [128, SEQ], F32)
            pB, aB, tB = Buf(), Buf(), Buf()
            S.dma("sp", posi[:], posb_in, w=[pB])
            cp(ang[:], posi[:], r=[pB], w=[aB])
            ts(ang[:], ang[:], invf[:, 0:1], None, ALU.mult, r=[aB, cB], w=[aB])
            ki = sbuf(ph, "ki", [128, SEQ], I32)
            kf = sbuf(ph, "kf", [128, SEQ], F32)
            kB = Buf()

            def sin_of(dst, offset):
                ts(tmpa[:], ang[:], offset, None, ALU.add, r=[aB], w=[tB])
                ts(kf[:], tmpa[:], 1.0 / (2 * math.pi), None, ALU.mult, r=[tB], w=[kB])
                cp(ki[:], kf[:], r=[kB], w=[kB])
                cp(kf[:], ki[:], r=[kB], w=[kB])
                stt(tmpa[:], kf[:], -2 * math.pi, tmpa[:], ALU.mult, ALU.add, r=[kB, tB], w=[tB])
                ts(kf[:], tmpa[:], math.pi, None, ALU.is_gt, r=[tB], w=[kB])
                stt(tmpa[:], kf[:], -2 * math.pi, tmpa[:], ALU.mult, ALU.add, r=[kB, tB], w=[tB])
                ts(kf[:], tmpa[:], -math.pi, None, ALU.is_lt, r=[tB], w=[kB])
                stt(tmpa[:], kf[:], 2 * math.pi, tmpa[:], ALU.mult, ALU.add, r=[kB, tB], w=[tB])
                ts(tmpa[:], tmpa[:], math.pi, -math.pi, ALU.min, ALU.max, r=[tB], w=[tB])
                act(dst, tmpa[:], AF.Sin, r=[tB], w=[ropeB])

            sin_of(sinT[:], 0.0)
            sin_of(cosT[:], 0.5 * math.pi)
            dump("cosT", cosT[:], [128, SEQ], F32, [ropeB])
            dump("sinT", sinT[:], [128, SEQ], F32, [ropeB])
            S.barrier()

        xres_b = [Buf("xres%d" % i) for i in range(NT)]
        xresB = Buf("xres_scatter")
        h2B = Buf("h2_d")

        def bcast_row(ph, dst, src, srcB, dstB):
            dg = [sbuf(ph, "dg%d" % i, [128, 128], F32) for i in range(2)]
            dgB = [Buf(), Buf()]
            for j in range(16):
                i = j % 2
                ts(dg[i][:], ident_f[:, :], src[:, j:j + 1], None, ALU.mult, r=[cB, srcB], w=[dgB[i]])
                mm(ps[j // 4][:, (j % 4) * 128:(j % 4 + 1) * 128], ones_f[:, :], dg[i][:], True, True, r=[cB, dgB[i]], w=[ps_b[j // 4]])
                if j % 4 == 3:
                    cp(dst[:, (j // 4) * 512:(j // 4 + 1) * 512], ps[j // 4][:, :], r=[ps_b[j // 4]], w=[dstB], eng="act")

        for l in range(L):
            x_src = x_in if l == 0 else xres
            with contextlib.ExitStack() as ph:
                cT = sbuf(ph, "cT", [128, 16], F32)
                scb = sbuf(ph, "scb", [128, 16], BF16)
                abT = sbuf(ph, "abT", [128, 96], F32)
                nm = sbuf(ph, "nm", [128, 16], F32)
                nf = sbuf(ph, "nf", [128, 16], F32)
                b1, b2, b3 = Buf(), Buf(), Buf()
                S.dma("sp", cT[:], cT_in, w=[b1])
                S.dma("sp", abT[:], ada_bT[l], w=[b3])
                S.dma("sp", nm[:], nmT_in[l], w=[b3])
                S.dma("sp", nf[:], nfT_in[l], w=[b3])
                act(scb[:], cT[:], AF.Silu, r=[b1], w=[b2])
                aw = ada_w[l].rearrange("(kt p) n -> p kt n", p=128)
                ws = WStream([(wbuf[i][:, :], wbuf_b[i]) for i in range(NWB)] + hT_as_wbufs())
                for cb in range(24):
                    ws.request(cb, aw[:, :, cb * 512:(cb + 1) * 512], 512)
                for cb in range(24):
                    wt, wb = ws.get(cb)
                    for sc in range(4):
                        j = cb * 4 + sc
                        for kt in range(NK):
                            mm(ps[0][:, j:j + 1], wt[:, kt, sc * 128:(sc + 1) * 128], scb[:, kt:kt + 1],
                               kt == 0, kt == NK - 1, r=[wb, b2], w=[ps_b[0]])
                    ws.release(cb)
                tt(modT[:], ps[0][:, 0:96], abT[:], ALU.add, r=[ps_b[0], b3], w=[modB])
                stt(gsm[:], modT[:, 16:32], 1.0, nm[:], ALU.add, ALU.mult, r=[modB, b3], w=[modB])
                stt(gsf[:], modT[:, 64:80], 1.0, nf[:], ALU.add, ALU.mult, r=[modB, b3], w=[modB])
                dump("modT%d" % l, modT[:], [128, 96], F32, [modB])
                S.barrier()
            if stop_after == "mod":
                break

            def norm_to_hT(x_src, gs, shift_off, also_h2=False):
                with contextlib.ExitStack() as ph:
                    xt = [sbuf(ph, "xt%d" % i, [128, D], F32) for i in range(2)]
                    xt_b = [Buf(), Buf()]
                    xn = [sbuf(ph, "xn%d" % i, [128, D], BF16) for i in range(2)]
                    xn_b = [Buf(), Buf()]
                    junk = sbuf(ph, "junk", [128, D], BF16)
                    jB = Buf()
                    ss = sbuf(ph, "ss", [128, 4], F32)
                    ssB = [Buf(), Buf()]
                    def ld_x(k_):
                        S.dma("sp", xt[k_ % 2][:], x_src[k_ * 128:(k_ + 1) * 128, :], r=[xres_b[k_]], w=[xt_b[k_ % 2]])

                    ld_x(0)
                    for t_ in range(NT):
                        i = t_ % 2
                        if t_ + 1 < NT:
                            ld_x(t_ + 1)
                        act(junk[:], xt[i][:], AF.Square, r=[xt_b[i]], w=[jB, ssB[i]], accum_out=ss[:, i:i + 1])
                        rstd_from_ss(ss[:, 2 + i:3 + i], ss[:, i:i + 1], 1.0 / D, ssB[i])
                        ts(xn[i][:], xt[i][:], ss[:, 2 + i:3 + i], None, ALU.mult, r=[xt_b[i], ssB[i]], w=[xn_b[i]])
                        if also_h2:
                            S.dma("sp", h2_d[t_ * 128:(t_ + 1) * 128, :], xn[i][:], r=[xn_b[i]], w=[h2B])
                        for hb in range(2):
                            for jj in range(8):
                                j = hb * 8 + jj
                                tr(psb[hb][:, jj * 128:(jj + 1) * 128], xn[i][:, j * 128:(j + 1) * 128], ident_bf[:, :],
                                   r=[xn_b[i], cB], w=[psb_b[hb]])
                            for jj in range(8):
                                j = hb * 8 + jj
                                ts(hT[:, j, t_ * 128:(t_ + 1) * 128], psb[hb][:, jj * 128:(jj + 1) * 128],
                                   gs[:, j:j + 1], modT[:, shift_off + j:shift_off + j + 1], ALU.mult, ALU.add,
                                   r=[psb_b[hb], modB], w=[hT_b[t_]])
                    S.barrier()

            norm_to_hT(x_src, gsm, 0)
            dump("hT%d" % l, hT[:], [128, NK, SEQ], BF16, hT_b)
            if stop_after == "hT":
                break

            wi = w_in[l].rearrange("(kt p) n -> p kt n", p=128)

            def gn_alloc(ph, N):
                return (sbuf(ph, "gn_sq", [128, 4, N], BF16), sbuf(ph, "gn_rs", [128, N], F32),
                        sbuf(ph, "gn_yn", [128, 4, N], BF16), Buf(), Buf(), Buf())

            def gn_store(ga, ysrc, yB, m, tok0, N, gn):
                sq, rs, yn, b_sq, b_rs, b_yn = ga
                for ch in range(4):
                    act(sq[:, ch, :], ysrc[:, ch, :], AF.Square, r=[yB], w=[b_sq])
                for ch in range(4):
                    mm(ps[5][:, 0:N], ones_bf[:, :], sq[:, ch, :], ch == 0, ch == 3, r=[b_sq, cB], w=[ps_b[5]])
                ts(rs[:], ps[5][:, 0:N], 1.0 / 512, EPS, ALU.mult, ALU.add, r=[ps_b[5]], w=[b_rs])
                act(rs[:], rs[:], AF.Ln, r=[b_rs], w=[b_rs])
                act(rs[:], rs[:], AF.Exp, r=[b_rs], w=[b_rs], scale=-0.5)
                for ch in range(4):
                    stt(yn[:, ch, :], ysrc[:, ch, :], gn[:, 4 * m + ch:4 * m + ch + 1], rs[:], ALU.mult, ALU.mult,
                        r=[yB, b_rs, gnB], w=[b_yn])
                t0 = tok0 // 128
                for ch in range(4):
                    S.dma("sp", ynT_d[:, t0:t0 + N // 128, 4 * m + ch, :],
                          yn[:, ch, :].rearrange("p (a t) -> p a t", t=128), r=[b_yn], w=[ynB])

            def gelu(dst, src_ps, xs, t1, r, w, bx, bt):
                cp(xs, src_ps, r=r, w=[bx], eng="act")
                tt(t1, xs, xs, ALU.mult, r=[bx], w=[bt])
                ts(t1, t1, 0.044715, 1.0, ALU.mult, ALU.add, r=[bt], w=[bt])
                tt(t1, t1, xs, ALU.mult, r=[bt, bx], w=[bt])
                act(t1, t1, AF.Sigmoid, r=[bt], w=[bt], scale=1.5957691216057308)
                tt(dst, t1, xs, ALU.mult, r=[bt, bx], w=w)

            gn_sb = sbuf(st, "gn%d" % l, [128, 16], F32)
            gnB = Buf()
            S.dma("sp", gn_sb[:], gnT_in[l], w=[gnB])
            ynB = Buf("ynT_d")

            if "fnet" not in skip:
              with contextlib.ExitStack() as ph:
                xa = sbuf(ph, "xa", [128, NT, 512], BF16)
                xaB = Buf()
                fw = sbuf(ph, "fw", [128, 4, 64], BF16)
                fwB = Buf()
                AB = sbuf(ph, "AB", [128, 8, 128], BF16)
                ABb = Buf()
                dC = [sbuf(ph, "dC%d" % i, [128, NK, 256], BF16) for i in range(2)]
                dS = [sbuf(ph, "dS%d" % i, [128, NK, 256], BF16) for i in range(2)]
                dB = [Buf(), Buf()]
                pq = sbuf(ph, "pq", [128, 4, 512], BF16)
                pqB = Buf()
                ya = sbuf(ph, "ya", [128, 4, 256], F32)
                yaB = Buf()
                ga = gn_alloc(ph, 256)
                S.dma("pool", fw[:], fnet_wS[l].rearrange("c p d -> p c d"), w=[fwB])
                memset(AB[:], 0.0, [ABb])
                for ch in range(4):
                    for which, tab in ((0, dftcc), (1, dftsc)):
                        mm(ps[4][:, 0:64], tab[:, :], fw[:, ch, :], True, True, r=[fwB, cB], w=[ps_b[4]])
                        cp(AB[0:64, which * 4 + ch, 0:64], ps[4][0:64, 0:64], r=[ps_b[4]], w=[ABb])
                        cp(AB[64:128, which * 4 + ch, 64:128], ps[4][64:128, 0:64], r=[ps_b[4]], w=[ABb])
                wt, wb = wload(wi[:, :, 0:512], 512)
                for t_ in range(NT):
                    pi = t_ % 2
                    for kt in range(NK):
                        mm(ps[pi][:, :], hT[:, kt, t_ * 128:(t_ + 1) * 128], wt[:, kt, :], kt == 0, kt == NK - 1,
                           r=[hT_b[t_], wb], w=[ps_b[pi]])
                    cp(xa[:, t_, :], ps[pi][:, :], r=[ps_b[pi]], w=[xaB], eng="act")
                dcv = cst["dftc"].rearrange("(kt p) n -> p kt n", p=128)
                dsv = cst["dfts"].rearrange("(kt p) n -> p kt n", p=128)
                def ld_tab(k_):
                    S.dma("sp", dC[k_ % 2][:], dcv[:, :, k_ * 256:(k_ + 1) * 256], w=[dB[k_ % 2]])
                    S.dma("sp", dS[k_ % 2][:], dsv[:, :, k_ * 256:(k_ + 1) * 256], w=[dB[k_ % 2]])

                ld_tab(0)
                for sbk in range(8):
                    bi = sbk % 2
                    if sbk + 1 < 8:
                        ld_tab(sbk + 1)
                    for ch in range(4):
                        for which, tab in ((0, dC[bi]), (1, dS[bi])):
                            for kt in range(NK):
                                mm(ps[ch][:, which * 256:(which + 1) * 256], xa[:, kt, ch * 128:(ch + 1) * 128],
                                   tab[:, kt, :], kt == 0, kt == NK - 1, r=[xaB, dB[bi]], w=[ps_b[ch]])
                        cp(pq[:, ch, :], ps[ch][:, :], r=[ps_b[ch]], w=[pqB], eng="act")
                    for ch in range(4):
                        o_ = ps[4][:, (ch % 2) * 256:(ch % 2 + 1) * 256] if ch < 2 else ps[6][:, (ch % 2) * 256:(ch % 2 + 1) * 256]
                        ob = ps_b[4] if ch < 2 else ps_b[6]
                        mm(o_, AB[:, ch, :], pq[:, ch, 0:256], True, False, r=[ABb, pqB], w=[ob])
                        mm(o_, AB[:, 4 + ch, :], pq[:, ch, 256:512], False, True, r=[ABb, pqB], w=[ob])
                        cp(ya[:, ch, :], o_, r=[ob], w=[yaB])
                    gn_store(ga, ya[:], yaB, 0, sbk * 256, 256, gn_sb)
                S.barrier()

            if "gmlp" not in skip:
              with contextlib.ExitStack() as ph:
                sgun = sbuf(ph, "sgun", [128, 512], F32)
                sguw = sbuf(ph, "sguw", [128, 8, 128], BF16)
                sgub = sbuf(ph, "sgub", [128, 4, 128], F32)
                gB = Buf()
                S.dma("sp", sgun[:], sgun_bc[l], w=[gB])
                S.dma("pool", sguw[:], sgu_wT[l], w=[gB])
                S.dma("sp", sgub[:], sgub_bc[l], w=[gB])
                wu, wub = wload(wi[:, :, 512:1024], 512)
                wv, wvb = wload(wi[:, :, 1024:1536], 512)
                uT = sbuf(ph, "uT", [128, 4, 512], F32)
                uB = Buf()
                yb = sbuf(ph, "yb", [128, 4, 512], F32)
                ybB = Buf()
                xs = sbuf(ph, "gxs", [128, 512], F32)
                t1 = sbuf(ph, "gt1", [128, 512], F32)
                bx, bt = Buf(), Buf()
                vs = sbuf(ph, "vs", [128, 512], F32)
                vB = Buf()
                vn = sbuf(ph, "vn", [128, 512], BF16)
                vnB = Buf()
                sv = sbuf(ph, "sv", [128, 2], F32)
                svB = Buf()
                zt = sbuf(ph, "zt", [128, 4, 128], F32)
                ztB = Buf()
                ga = gn_alloc(ph, 512)
                for tb in range(4):
                    for ch in range(4):
                        for kt in range(NK):
                            mm(ps[ch][:, :], wu[:, kt, ch * 128:(ch + 1) * 128], hT[:, kt, tb * 512:(tb + 1) * 512],
                               kt == 0, kt == NK - 1, r=[wub] + hT_b[tb * 4:tb * 4 + 4], w=[ps_b[ch]])
                        act(uT[:, ch, :], ps[ch][:, :], AF.Gelu_apprx_tanh, r=[ps_b[ch]], w=[uB])
                    for pc in range(4):
                        t_ = tb * 4 + pc
                        for kt in range(NK):
                            mm(ps[4][:, :], hT[:, kt, t_ * 128:(t_ + 1) * 128], wv[:, kt, :], kt == 0, kt == NK - 1,
                               r=[wvb, hT_b[t_]], w=[ps_b[4]])
                        act(vs[:], ps[4][:, :], AF.Gelu_apprx_tanh, r=[ps_b[4]], w=[vB])
                        act(t1[:], vs[:], AF.Square, r=[vB], w=[bt, svB], accum_out=sv[:, 0:1])
                        rstd_from_ss(sv[:, 1:2], sv[:, 0:1], 1.0 / 512, svB)
                        stt(vn[:], vs[:], sv[:, 1:2], sgun[:], ALU.mult, ALU.mult, r=[vB, svB, gB], w=[vnB])
                        for h_ in range(8):
                            ch, hh = h_ // 2, h_ % 2
                            mm(ps[5][hh * 64:(hh + 1) * 64, ch * 128:(ch + 1) * 128], vn[:, h_ * 64:(h_ + 1) * 64],
                               sguw[:, h_, :], True, True, r=[vnB, gB], w=[ps_b[5]])
                        tt(zt[:], ps[5][:, :].rearrange("p (c t) -> p c t", t=128), sgub[:], ALU.add, r=[ps_b[5], gB], w=[ztB])
                        tt(yb[:, :, pc * 128:(pc + 1) * 128], zt[:], uT[:, :, pc * 128:(pc + 1) * 128], ALU.mult,
                           r=[ztB, uB], w=[ybB])
                    gn_store(ga, yb[:], ybB, 1, tb * 512, 512, gn_sb)
                S.barrier()
            if stop_after == "mix01":
                break

            def proj_stream(cols):
                subs = [(wbuf[i][:, j * 2048:(j + 1) * 2048], Buf("wsub%d_%d" % (i, j))) for i in range(NWB) for j in range(4)]
                ws_ = WStream(subs)
                for c_ in cols:
                    ws_.request(c_, wi[:, :, c_:c_ + 128], 128)
                ws_.pump()
                return ws_

            def proj_fm(col0, evac=None):
                wt, wb = pstate["ws"].get(col0)

                def proj(tb):
                    rr = [wb] + hT_b[tb * 4:tb * 4 + 4]
                    for kt in range(NK):
                        mm(ps[tb][:, :], wt[:, kt, :], hT[:, kt, tb * 512:(tb + 1) * 512], kt == 0, kt == NK - 1, r=rr, w=[ps_b[tb]])

                proj(0)
                proj(1)
                for tb in range(4):
                    if evac is not None:
                        evac(tb)
                    if tb + 2 < 4:
                        proj(tb + 2)
                pstate["ws"].release(col0)

            def rope_tb(rs_, dst, dstB, placed=None):
                qr, qrB, t1, t1B, t2, t2B = rs_

                def f(tb):
                    blk = slice(tb * 512, (tb + 1) * 512)
                    pj = 4 + tb % 2
                    cp(qr[:], ps[tb][:, :], r=[ps_b[tb]], w=[qrB], eng="act")
                    mm(ps[pj][:, :], rotT[:, :], qr[:], True, True, r=[qrB, cB], w=[ps_b[pj]])
                    cp(t1[:], ps[tb][:, :], r=[ps_b[tb]], w=[t1B], eng="act")
                    cp(t2[:], ps[pj][:, :], r=[ps_b[pj]], w=[t2B], eng="act")
                    tt(t1[:], t1[:], cosT[:, blk], ALU.mult, r=[ropeB], w=[t1B])
                    tt(t2[:], t2[:], sinT[:, blk], ALU.mult, r=[ropeB], w=[t2B], eng="pool")
                    if placed is None:
                        tt(dst[:, blk], t1[:], t2[:], ALU.add, r=[t1B, t2B], w=[dstB])
                    else:
                        for (tl, tlB, sb_, tg_) in placed:
                            tt(tl[tg_:tg_ + 64, blk], t1[sb_:sb_ + 64, :], t2[sb_:sb_ + 64, :], ALU.add,
                               r=[t1B, t2B], w=[tlB])
                return f

            def plain_tb(dst, dstB):
                def f(tb):
                    cp(dst[:, tb * 512:(tb + 1) * 512], ps[tb][:, :], r=[ps_b[tb]], w=[dstB], eng="act")
                return f

            def gslice(r_, m_, d):
                start = r_ + d * 128 * m_
                return slice(start, start + 128) if d == 1 else slice(start, start + d * 127 + 1, d)

            def v_to_tok(vT, vTB, vtok, vtokB, d):
                nper = 16 // d
                for r_ in range(d):
                    for m_ in range(nper):
                        g = r_ * nper + m_
                        bi = (g // 8) % 2
                        o_ = psb[bi][:, (g % 8) * 128:(g % 8 + 1) * 128]
                        tr(o_, vT[:, gslice(r_, m_, d)], ident_bf[:, :], r=[vTB, cB], w=[psb_b[bi]])
                        cp(vtok[:, g, :, 0:64], o_.rearrange("p (h c) -> p h c", c=64), r=[psb_b[bi]], w=[vtokB],
                           eng=("act" if g % 2 else "dve"))

            astate = {"s": 0, "o": 0, "p": 0, "e": 0}
            pstate = {}

            def attend(pts, qT, qB, kT, kB, vtok, vtokB, vh, W, d, acc, accB, first):
                nper = 16 // d
                wi_ = 0 if W == 128 else 1
                tiles = [(r_, m_) for r_ in range(d) for m_ in range(nper)]
                otmp, otmpB = pts[0][2], pts[0][3]

                def emit_S(r_, m_):
                    qs = gslice(r_, m_, d)
                    dls = [dl for dl in (-1, 0, 1) if 0 <= m_ + dl < nper]
                    c0, n = (dls[0] + 1) * 128, len(dls) * 128
                    si = astate["s"] % 4
                    astate["s"] += 1
                    pi = astate["p"] % len(pts)
                    astate["p"] += 1
                    mm(ps[si][:, 0:n], ident_bf[:, :], maskb[:, wi_, c0:c0 + n], True, False, r=[cB], w=[ps_b[si]])
                    for j, dl in enumerate(dls):
                        mm(ps[si][:, j * 128:(j + 1) * 128], kT[:, gslice(r_, m_ + dl, d)], qT[:, qs], False, j == len(dls) - 1,
                           r=[kB, qB], w=[ps_b[si]])
                    act(pts[pi][0][:, 0:n], ps[si][:, 0:n], AF.Exp, r=[ps_b[si]], w=[pts[pi][1]], scale=0.125)
                    return dls, pi

                def emit_PV(ti, oi, r_, m_, dls, pi):
                    o_ = ps[oi][:, ti * 128:(ti + 1) * 128]
                    for j, dl in enumerate(dls):
                        g = r_ * nper + m_ + dl
                        mm(o_, vtok[:, g, vh, :], pts[pi][0][:, j * 128:(j + 1) * 128], j == 0, j == len(dls) - 1,
                           r=[vtokB, pts[pi][1]], w=[ps_b[oi]])

                def evac(grp, oi):
                    n = len(grp) * 128
                    src = ps[oi][:, 0:n]
                    r0, m0 = grp[0]
                    if d == 1:
                        dst = acc[:, m0 * 128:m0 * 128 + n]
                        view = lambda a: a
                    elif d == 4:
                        dst = acc[:, slice(r0, r0 + 4 * (n - 1) + 1, 4)]
                        view = lambda a: a
                    else:
                        dst = acc[:, :].rearrange("p (i r) -> p r i", r=16)[:, r0:r0 + len(grp), :]
                        view = lambda a: a.rearrange("p (t i) -> p t i", i=128)
                    if first:
                        cp(dst, view(src), r=[ps_b[oi]], w=[accB])
                    else:
                        oj = astate["e"] % 2
                        astate["e"] += 1
                        cp(otmp[oj][:, 0:n], src, r=[ps_b[oi]], w=[otmpB[oj]], eng="act")
                        tt(dst, dst, view(otmp[oj][:, 0:n]), ALU.add, r=[otmpB[oj], accB], w=[accB])

                pend = []

                def drain(keep):
                    while len(pend) > keep:
                        pv, ev = pend.pop(0)
                        emit_PV(*pv)
                        if ev is not None:
                            evac(*ev)

                for gi in range(0, len(tiles), 4):
                    grp = tiles[gi:gi + 4]
                    oi = 6 + astate["o"] % 2
                    astate["o"] += 1
                    for ti, (r_, m_) in enumerate(grp):
                        dls, pi = emit_S(r_, m_)
                        pend.append(((ti, oi, r_, m_, dls, pi), (grp, oi) if ti == len(grp) - 1 else None))
                        drain(2)
                drain(0)

            def attn_finish(fs, acc, accB, yall, yallB, chunk, base, esink_col):
                dtmp, rd0, fB = fs
                for tb in range(4):
                    blk = slice(tb * 512, (tb + 1) * 512)
                    if esink_col is not None:
                        ts(dtmp[64:128, :], acc[64:128, blk], esink_col, None, ALU.add, r=[accB, sinkB], w=[fB])
                        act(dtmp[64:128, :], dtmp[64:128, :], AF.Ln, r=[fB], w=[fB])
                    else:
                        act(dtmp[64:128, :], acc[64:128, blk], AF.Ln, r=[accB], w=[fB])
                    act(dtmp[64:128, :], dtmp[64:128, :], AF.Exp, r=[fB], w=[fB], scale=-1.0)
                    cp(rd0[0:64, :], dtmp[64:128, :], r=[fB], w=[fB])
                    tt(yall[base:base + 64, chunk, blk], acc[0:64, blk], rd0[0:64, :], ALU.mult, r=[accB, fB], w=[yallB])

            def attn_alloc(ph):
                d_ = {}
                d_["rs"] = (sbuf(ph, "qr", [128, 512], BF16), Buf(), sbuf(ph, "rt1", [128, 512], F32), Buf(),
                            sbuf(ph, "rt2", [128, 512], F32), Buf())
                otmp = [sbuf(ph, "otmp%d" % i, [128, 512], F32) for i in range(2)]
                otmpB = [Buf(), Buf()]
                d_["pts"] = [(sbuf(ph, "pt%d" % i, [128, 384], BF16), Buf(), otmp, otmpB) for i in range(4)]
                d_["fs"] = (sbuf(ph, "dtmp", [128, 512], F32), sbuf(ph, "rd0", [128, 512], F32), Buf())
                d_["acc"] = sbuf(ph, "acc", [128, SEQ], F32)
                d_["accB"] = Buf()
                d_["yall"] = sbuf(ph, "yall", [128, 4, SEQ], BF16)
                d_["yallB"] = Buf()
                d_["ga"] = gn_alloc(ph, 512)
                d_["qm"] = [sbuf(ph, "qm%d" % i, [128, SEQ], BF16) for i in range(2)]
                d_["qmB"] = [Buf(), Buf()]
                d_["vT"] = sbuf(ph, "vT", [128, SEQ], BF16)
                d_["vTB"] = Buf()
                return d_

            if "swa" not in skip:
              with contextlib.ExitStack() as ph:
                A = attn_alloc(ph)
                kA = sbuf(ph, "kA", [128, SEQ], BF16)
                kAB = Buf()
                vtok = sbuf(ph, "vtok", [128, NT, 2, 128], BF16)
                vtokB = Buf()
                esink = sbuf(ph, "esink", [128, 8], F32)
                sinkB = Buf()
                S.dma("sp", esink[:], sinkT[l], w=[sinkB])
                act(esink[:], esink[:], AF.Exp, r=[sinkB], w=[sinkB])
                memset(vtok[:, :, :, 64:128], 1.0, [vtokB])
                pstate["ws"] = proj_stream([2048, 2176] + [1536 + c_ * 128 for c_ in range(4)])
                if stop_after == "swa_0":
                    return fin()
                proj_fm(2048, rope_tb(A["rs"], kA, kAB))
                if stop_after == "swa_k":
                    return fin()
                proj_fm(2176, plain_tb(A["vT"], A["vTB"]))
                v_to_tok(A["vT"], A["vTB"], vtok, vtokB, 1)
                if stop_after == "swa_v":
                    return fin()
                for chunk in range(4):
                    kv = chunk // 2
                    for hh in range(2):
                        memset(A["qm"][hh][:], 0.0, [A["qmB"][hh]])
                    proj_fm(1536 + chunk * 128,
                            rope_tb(A["rs"], None, None, placed=[(A["qm"][hh], A["qmB"][hh], hh * 64, kv * 64) for hh in range(2)]))
                    for hh in range(2):
                        h_ = chunk * 2 + hh
                        if stop_after == "swa_q":
                            return fin()
                        attend(A["pts"], A["qm"][hh], A["qmB"][hh], kA, kAB, vtok, vtokB, kv, 128, 1, A["acc"], A["accB"], True)
                        if stop_after == "swa_a":
                            return fin()
                        attn_finish(A["fs"], A["acc"], A["accB"], A["yall"], A["yallB"], chunk, hh * 64, esink[64:128, h_:h_ + 1])
                        if stop_after == "swa_f":
                            return fin()
                        if stop_after == "swa_f2" and hh == 1:
                            return fin()
                for tb in range(4):
                    gn_store(A["ga"], A["yall"][:, :, tb * 512:(tb + 1) * 512], A["yallB"], 2, tb * 512, 512, gn_sb)
                S.barrier()

            if "dil" not in skip:
              with contextlib.ExitStack() as ph:
                A = attn_alloc(ph)
                kT = sbuf(ph, "kT", [128, SEQ], BF16)
                kTB = Buf()
                vtok1 = sbuf(ph, "vtokd", [128, NT, 2, 128], BF16)
                vtok1B = Buf()
                acc2 = sbuf(ph, "acc2", [128, SEQ], F32)
                accs = [(A["acc"], A["accB"]), (acc2, Buf())]
                sinkB = None
                memset(vtok1[:, :, :, 64:128], 1.0, [vtok1B])
                pstate["ws"] = proj_stream([c0_ + c_ * 128 for c_ in range(4) for c0_ in (2304, 2816, 3328)])
                for chunk in range(4):
                    if chunk == 0:
                        for hh in range(2):
                            memset(A["qm"][hh][:], 0.0, [A["qmB"][hh]])
                    proj_fm(2304 + chunk * 128,
                            rope_tb(A["rs"], None, None, placed=[(A["qm"][hh], A["qmB"][hh], hh * 64, hh * 64) for hh in range(2)]))
                    proj_fm(2816 + chunk * 128, rope_tb(A["rs"], kT, kTB))
                    proj_fm(3328 + chunk * 128, plain_tb(A["vT"], A["vTB"]))
                    for ci, d_ in enumerate((1, 4, 16)):
                        v_to_tok(A["vT"], A["vTB"], vtok1, vtok1B, d_)
                        for hh in range(2):
                            attend(A["pts"], A["qm"][hh], A["qmB"][hh], kT, kTB, vtok1, vtok1B, hh, 64, d_,
                                   accs[hh][0], accs[hh][1], ci == 0)
                    for hh in range(2):
                        attn_finish(A["fs"], accs[hh][0], accs[hh][1], A["yall"], A["yallB"], chunk, hh * 64, None)
                for tb in range(4):
                    gn_store(A["ga"], A["yall"][:, :, tb * 512:(tb + 1) * 512], A["yallB"], 3, tb * 512, 512, gn_sb)
                S.barrier()
            if stop_after == "mix":
                break

            with contextlib.ExitStack() as ph:
                woB = Buf()
                wo = w_out[l].rearrange("(ec p) d -> p ec d", p=128)
                woQ = [Buf("wo%d" % q4) for q4 in range(4)]
                for q4 in range(4):
                    S.dma("pool", hT[:, q4 * 4:(q4 + 1) * 4, :], wo[:, q4 * 4:(q4 + 1) * 4, :], w=[woQ[q4]])
                gmbc = sbuf(ph, "gmbc", [128, D], F32)
                gmB = Buf()
                bcast_row(ph, gmbc, modT[:, 32:48], modB, gmB)
                ynt = [sbuf(ph, "ynt%d" % i, [128, 16, 128], BF16) for i in range(2)]
                yntB = [Buf(), Buf()]
                xt = [sbuf(ph, "dxt%d" % i, [128, D], F32) for i in range(2)]
                xtB = [Buf(), Buf()]
                tmp = [sbuf(ph, "dtmp%d" % i, [128, 512], F32) for i in range(2)]
                tmpB = [Buf(), Buf()]
                def ld_tile(k_):
                    S.dma("sp", ynt[k_ % 2][:], ynT_d[:, k_], r=[ynB], w=[yntB[k_ % 2]])
                    S.dma("sp", xt[k_ % 2][:], x_src[k_ * 128:(k_ + 1) * 128, :], r=[xres_b[k_]], w=[xtB[k_ % 2]])

                ld_tile(0)
                for t_ in range(NT):
                    i = t_ % 2
                    if t_ + 1 < NT:
                        ld_tile(t_ + 1)
                    for db in range(4):
                        dblk = slice(db * 512, (db + 1) * 512)
                        ti = db % 2
                        for ec in range(16):
                            mm(ps[db][:, :], ynt[i][:, ec, :], hT[:, ec, dblk], ec == 0, ec == 15, r=[yntB[i], woQ[ec // 4]], w=[ps_b[db]])
                        cp(tmp[ti][:], ps[db][:, :], r=[ps_b[db]], w=[tmpB[ti]], eng="act")
                        tt(tmp[ti][:], tmp[ti][:], gmbc[:, dblk], ALU.mult, r=[gmB], w=[tmpB[ti]])
                        tt(xt[i][:, dblk], xt[i][:, dblk], tmp[ti][:], ALU.add, r=[tmpB[ti]], w=[xtB[i]], eng="pool")
                    S.dma("sp", xres[t_ * 128:(t_ + 1) * 128, :], xt[i][:], r=[xtB[i]], w=[xres_b[t_]])
                S.barrier()
            if stop_after == "wout":
                break

            norm_to_hT(xres, gsf, 48, also_h2=True)
            with contextlib.ExitStack() as ph:
                idxT = sbuf(ph, "idxT", [128, 2, 16], U32)
                gateT = sbuf(ph, "gateT", [128, 2, 16], F32)
                rtB = Buf()
                gfbc = sbuf(ph, "gfbc", [128, D], F32)
                gfB = Buf()
                xw = sbuf(ph, "xwbuf", [128, NK * 512], BF16)
                xwB = Buf("xw")
                ws = WStream([(wbuf[i][:, :], wbuf_b[i]) for i in range(NWB)] + [(xw[:, :], xwB)])
                for e in range(N_EXP):
                    wgv = wg_in[l, e].rearrange("(kt p) f -> p kt f", p=128)
                    wuv = wu_in[l, e].rearrange("(kt p) f -> p kt f", p=128)
                    wdv = wd_in[l, e].rearrange("(ft p) d -> p ft d", p=128)
                    for half in range(2):
                        ws.request((e, "g", half), wgv[:, :, half * 512:(half + 1) * 512], 512)
                        ws.request((e, "u", half), wuv[:, :, half * 512:(half + 1) * 512], 512)
                    for dh in range(2):
                        ws.request((e, "d", dh), wdv[:, :, dh * 1024:(dh + 1) * 1024], 1024, 8)
                ws.pump()
                with contextlib.ExitStack() as ph2:
                    bcast_row(ph2, gfbc, modT[:, 80:96], modB, gfB)
                    rw = sbuf(ph2, "rw", [128, NK, N_EXP], BF16)
                    rwB = Buf()
                    S.dma("pool", rw[:], router_w[l].rearrange("(kt p) e -> p kt e", p=128), w=[rwB])
                    affp = sbuf(ph2, "affp", [128, 128], F32)
                    affB = Buf()
                    memset(affp[:], 0.0, [affB])
                    lg = sbuf(ph2, "lg", [128, 16], F32)
                    ex = sbuf(ph2, "ex", [128, 16], F32)
                    st_ = sbuf(ph2, "st_", [128, 4], F32)
                    lgB = Buf()
                    affT = sbuf(ph2, "affT", [128, SEQ], F32)
                    affTB = Buf()
                    for t_ in range(NT):
                        for kt in range(NK):
                            mm(ps[0][:, 0:16], hT[:, kt, t_ * 128:(t_ + 1) * 128], rw[:, kt, :], kt == 0, kt == NK - 1,
                               r=[hT_b[t_], rwB], w=[ps_b[0]])
                        cp(lg[:], ps[0][:, 0:16], r=[ps_b[0]], w=[lgB], eng="act")
                        S.op("dve", lambda: nc.vector.reduce_max(out=st_[:, 0:1], in_=lg[:], axis=AX.X), [lgB], [lgB])
                        ts(st_[:, 1:2], st_[:, 0:1], -1.0, None, ALU.mult, r=[lgB], w=[lgB])
                        act(ex[:], lg[:], AF.Exp, r=[lgB], w=[lgB], bias=st_[:, 1:2], accum_out=st_[:, 2:3])
                        S.op("dve", lambda: nc.vector.reciprocal(out=st_[:, 3:4], in_=st_[:, 2:3]), [lgB], [lgB])
                        ts(affp[:, 0:16], ex[:], st_[:, 3:4], None, ALU.mult, r=[lgB], w=[affB])
                        tr(ps[1][:, 0:128], affp[:, :], ident_f[:, :], r=[affB, cB], w=[ps_b[1]])
                        cp(affT[0:16, t_ * 128:(t_ + 1) * 128], ps[1][0:16, 0:128], r=[ps_b[1]], w=[affTB], eng="act")
                    ws.add_bufs(hT_as_wbufs()[0:3], hT_b)
                    dump("affT%d" % l, affT[0:16, :], [16, SEQ], F32, [affTB])
                    vals = sbuf(ph2, "vals", [128, CAP], F32)
                    idxu = sbuf(ph2, "idxu", [128, CAP], U32)
                    idxf = sbuf(ph2, "idxf", [128, CAP], F32)
                    tkB = Buf()
                    memset(vals[:], 0.0, [tkB])
                    memset(idxf[:], 0.0, [tkB])
                    for it in range(CAP // 8):
                        sl = slice(it * 8, it * 8 + 8)
                        S.op("dve", lambda sl=sl: nc.vector.max(out=vals[0:16, sl], in_=affT[0:16, :]), [affTB], [tkB])
                        S.op("dve", lambda sl=sl: nc.vector.max_index(out=idxu[0:16, sl], in_max=vals[0:16, sl], in_values=affT[0:16, :]),
                             [affTB, tkB], [tkB])
                        S.op("dve", lambda sl=sl: nc.vector.match_replace(out=affT[0:16, :], in_to_replace=vals[0:16, sl],
                                                                     in_values=affT[0:16, :], imm_value=-1.0), [tkB], [affTB])
                    cp(idxf[0:16, :], idxu[0:16, :], r=[tkB], w=[tkB])
                    for hf in range(2):
                        tr(ps[2][:, hf * 128:(hf + 1) * 128], idxf[:, hf * 128:(hf + 1) * 128], ident_f[:, :], r=[tkB, cB], w=[ps_b[2]])
                        tr(ps[3][:, hf * 128:(hf + 1) * 128], vals[:, hf * 128:(hf + 1) * 128], ident_f[:, :], r=[tkB, cB], w=[ps_b[3]])
                        cp(idxT[:, hf, :], ps[2][:, hf * 128:hf * 128 + 16], r=[ps_b[2]], w=[rtB])
                        cp(gateT[:, hf, :], ps[3][:, hf * 128:hf * 128 + 16], r=[ps_b[3]], w=[rtB])
                    dump("idxT%d" % l, idxT[:], [128, 2, 16], U32, [rtB])
                    dump("gateT%d" % l, gateT[:], [128, 2, 16], F32, [rtB])
                    S.barrier()
                if stop_after == "route":
                    return fin()
                xe = [sbuf(ph, "xe%d" % i, [128, D], BF16)[:, :] for i in range(2)] + [hT[:, 12, :], hT[:, 13, :]]
                xeB = [Buf() for _ in range(4)]
                xeT = sbuf(ph, "xeT", [128, NK, CAP], BF16)
                xeTB = Buf()
                gT = sbuf(ph, "gT", [128, 8, CAP], BF16)
                gTB = Buf()
                sa = [sbuf(ph, "sa%d" % i, [128, CAP], F32) for i in range(2)]
                ub = [sbuf(ph, "ub%d" % i, [128, CAP], F32) for i in range(2)]
                saB = [Buf(), Buf()]
                osb = [sbuf(ph, "osb%d" % i, [128, D], F32) for i in range(2)]
                osbB = [Buf(), Buf()]
                ob = [sbuf(ph, "ob%d" % i, [128, 512], F32) for i in range(2)]
                obB = [Buf(), Buf()]

                def gather(e):
                    for hf in range(2):
                        k = (e % 2) * 2 + hf
                        S.dma("pool", None, None, r=[h2B, rtB], w=[xeB[k]],
                              indirect=lambda hf=hf, e=e, k=k: nc.gpsimd.indirect_dma_start(
                                  out=xe[k], out_offset=None, in_=h2_d,
                                  in_offset=bass.IndirectOffsetOnAxis(ap=idxT[:, hf, e:e + 1], axis=0)))

                gather(0)
                ws.pump()
                for e in range(N_EXP):
                    for hf in range(2):
                        k = (e % 2) * 2 + hf
                        for hb in range(2):
                            for jj in range(8):
                                j = hb * 8 + jj
                                tr(psb[hb][:, jj * 128:(jj + 1) * 128], xe[k][:, j * 128:(j + 1) * 128], ident_bf[:, :],
                                   r=[xeB[k], cB], w=[psb_b[hb]])
                            for jj in range(8):
                                j = hb * 8 + jj
                                ts(xeT[:, j, hf * 128:(hf + 1) * 128], psb[hb][:, jj * 128:(jj + 1) * 128],
                                   gsf[:, j:j + 1], modT[:, 48 + j:49 + j], ALU.mult, ALU.add, r=[psb_b[hb], modB], w=[xeTB])
                    if e + 1 < N_EXP:
                        gather(e + 1)
                    for half in range(2):
                        wg_, wgb = ws.get((e, "g", half))
                        wu_, wub = ws.get((e, "u", half))
                        for fl in range(4):
                            fb = half * 4 + fl
                            pi = fb % 2
                            fsl = slice(fl * 128, (fl + 1) * 128)
                            for kt in range(NK):
                                mm(ps[2 * pi][:, 0:CAP], wg_[:, kt, fsl], xeT[:, kt, :], kt == 0, kt == NK - 1, r=[wgb, xeTB], w=[ps_b[2 * pi]])
                            for kt in range(NK):
                                mm(ps[2 * pi + 1][:, 0:CAP], wu_[:, kt, fsl], xeT[:, kt, :], kt == 0, kt == NK - 1, r=[wub, xeTB], w=[ps_b[2 * pi + 1]])
                            act(sa[pi][:], ps[2 * pi][:, 0:CAP], AF.Silu, r=[ps_b[2 * pi]], w=[saB[pi]])
                            tt(gT[:, fb, :], ps[2 * pi + 1][:, 0:CAP], sa[pi][:], ALU.mult, r=[ps_b[2 * pi + 1], saB[pi]], w=[gTB])
                        ws.release((e, "g", half))
                        ws.release((e, "u", half))
                    for dh in range(2):
                        wd_, wdb = ws.get((e, "d", dh))
                        for hf in range(2):
                            for dq in range(2):
                                db = dh * 2 + dq
                                oi = (hf * 2 + dq) % 2
                                for ft in range(8):
                                    mm(ps[4 + oi][:, :], gT[:, ft, hf * 128:(hf + 1) * 128], wd_[:, ft, dq * 512:(dq + 1) * 512],
                                       ft == 0, ft == 7, r=[gTB, wdb], w=[ps_b[4 + oi]])
                                ts(ob[oi][:], ps[4 + oi][:, :], gateT[:, hf, e:e + 1], None, ALU.mult, r=[ps_b[4 + oi], rtB], w=[obB[oi]])
                                tt(osb[hf][:, db * 512:(db + 1) * 512], ob[oi][:], gfbc[:, db * 512:(db + 1) * 512], ALU.mult,
                                   r=[obB[oi], gfB], w=[osbB[hf]])
                        ws.release((e, "d", dh))
                    for hf in range(2):
                        S.dma("pool", None, None, r=[osbB[hf], rtB] + xres_b, w=[xresB],
                              indirect=lambda hf=hf, e=e: nc.gpsimd.indirect_dma_start(
                                  out=xres, out_offset=bass.IndirectOffsetOnAxis(ap=idxT[:, hf, e:e + 1], axis=0),
                                  in_=osb[hf][:], in_offset=None, compute_op=ALU.add))
                S.barrier()
                for t_ in range(NT):
                    xres_b[t_].w = xresB.w
            if stop_after == "moe":
                break

        if stop_after is None:
            with contextlib.ExitStack() as ph:
                fbc = sbuf(ph, "fbc", [128, D], F32)
                fB = Buf()
                S.dma("sp", fbc[:], fin_bc, w=[fB])
                xt = [sbuf(ph, "fxt%d" % i, [128, D], F32) for i in range(2)]
                xtB = [Buf(), Buf()]
                junk = sbuf(ph, "fjunk", [128, D], BF16)
                jB = Buf()
                ss = sbuf(ph, "fss", [128, 4], F32)
                ssB = [Buf(), Buf()]
                def ld_f(k_):
                    S.dma("sp", xt[k_ % 2][:], xres[k_ * 128:(k_ + 1) * 128, :], r=[xres_b[k_]], w=[xtB[k_ % 2]])

                ld_f(0)
                for t_ in range(NT):
                    i = t_ % 2
                    if t_ + 1 < NT:
                        ld_f(t_ + 1)
                    act(junk[:], xt[i][:], AF.Square, r=[xtB[i]], w=[jB, ssB[i]], accum_out=ss[:, i:i + 1])
                    rstd_from_ss(ss[:, 2 + i:3 + i], ss[:, i:i + 1], 1.0 / D, ssB[i])
                    ts(xt[i][:], xt[i][:], ss[:, 2 + i:3 + i], None, ALU.mult, r=[ssB[i]], w=[xtB[i]])
                    tt(xt[i][:], xt[i][:], fbc[:], ALU.mult, r=[fB], w=[xtB[i]], eng="pool")
                    S.dma("sp", out_ap[t_ * 128:(t_ + 1) * 128, :], xt[i][:], r=[xtB[i]])

        return fin()


def prep_shared(inp, L=2):
    f = np.float32
    g = {}
    g["ada_w"] = np.ascontiguousarray(inp["ada_w"][:L], dtype=f)
    g["ada_bT"] = np.ascontiguousarray(inp["ada_b"][:L].reshape(L, 96, 128).transpose(0, 2, 1), dtype=f)
    for k, n in (("nmT", "norm_mix"), ("nfT", "norm_ffn"), ("gnT", "group_norm")):
        g[k] = np.ascontiguousarray(inp[n][:L].reshape(L, 16, 128).transpose(0, 2, 1), dtype=f)
    g["w_in"] = np.ascontiguousarray(inp["w_in"][:L], dtype=f)
    g["fnet_wS"] = np.ascontiguousarray(inp["fnet_w"][:L].reshape(L, 4, 128, 64), dtype=f)
    g["sgun_bc"] = np.ascontiguousarray(np.broadcast_to(inp["sgu_norm"][:L][:, None, :], (L, 128, 512)), dtype=f)
    g["sgu_wT"] = np.ascontiguousarray(inp["sgu_w"][:L].transpose(0, 3, 1, 2), dtype=f)
    sb = inp["sgu_b"][:L]
    sbb = sb.reshape(L, 4, 2, 1, 128)
    sbb = np.broadcast_to(sbb, (L, 4, 2, 64, 128)).reshape(L, 4, 128, 128).transpose(0, 2, 1, 3)
    g["sgub_bc"] = np.ascontiguousarray(sbb, dtype=f)
    g["sinkT"] = np.ascontiguousarray(np.broadcast_to(inp["swa_sink"][:L][:, None, :], (L, 128, 8)), dtype=f)
    g["w_out"] = np.ascontiguousarray(inp["w_out"][:L], dtype=f)
    g["router_w"] = np.ascontiguousarray(inp["router_w"][:L], dtype=f)
    g["exp_w_gate"] = np.ascontiguousarray(inp["exp_w_gate"][:L], dtype=f)
    g["exp_w_up"] = np.ascontiguousarray(inp["exp_w_up"][:L], dtype=f)
    g["exp_w_down"] = np.ascontiguousarray(inp["exp_w_down"][:L], dtype=f)
    g["fin_bc"] = np.ascontiguousarray(np.broadcast_to(inp["final_norm"][None, :], (128, D)), dtype=f)
    g.update(host_consts())
    return g


def prep_core(inp, b):
    m = {}
    m["x"] = np.ascontiguousarray(inp["x"][b], dtype=np.float32)
    m["cT"] = np.ascontiguousarray(inp["c"][b].reshape(16, 128).T, dtype=np.float32)
    m["posb"] = np.ascontiguousarray(np.broadcast_to(inp["positions"][b][None, :], (128, SEQ)), dtype=np.int32)
    return m


def kernel(**inputs):
    inp = {k: np.asarray(v) for k, v in inputs.items()}
    nc, _ = build(L=2)
    shared = prep_shared(inp)
    in_maps = []
    for core in range(8):
        m = dict(shared)
        m.update(prep_core(inp, core // 2))
        in_maps.append(m)
    res = run_bass_kernel_spmd(nc, in_maps, core_ids=list(range(8)))
    out = np.stack([np.asarray(res.results[2 * b]["out"]) for b in range(4)], axis=0)
    return out.astype(np.float32)
```

```python
import contextlib
import math
import numpy as np
import ml_dtypes
import concourse.bass as bass
import concourse.mybir as mybir
from concourse.bass_utils import run_bass_kernel_spmd

F32 = mybir.dt.float32
BF16 = mybir.dt.bfloat16
I32 = mybir.dt.int32
U32 = mybir.dt.uint32
AF = mybir.ActivationFunctionType
ALU = mybir.AluOpType
AX = mybir.AxisListType

D = 2048
SEQ = 2048
NT = 16
NK = 16
D_IN = 3840
EPS = 1e-6
NEG = -30000.0
N_EXP = 16
CAP = 256
FF = 1024


class Buf:
    __slots__ = ("name", "w", "r")

    def __init__(self, name=""):
        self.name = name
        self.w = None
        self.r = {}


class Sched:
    def __init__(self, nc, stack, n_dma=24):
        self.nc = nc
        self.engs = {"pe": nc.tensor, "act": nc.scalar, "dve": nc.vector, "pool": nc.gpsimd, "sp": nc.sync}
        self.sems = {k: stack.enter_context(nc.semaphore("s_" + k)) for k in self.engs}
        self.cnt = {k: 0 for k in self.engs}
        self.seen = {k: {} for k in self.engs}
        self.dsems = [stack.enter_context(nc.semaphore("d%d" % i)) for i in range(n_dma)]
        self.dcnt = [0] * n_dma
        self.dnext = 0
        self.qnext = {}
        self.marks = []
        self.nins = 0

    def _wait(self, eng, key, val):
        if key == eng == "pe":
            return
        if self.seen[eng].get(key, 0) >= val:
            return
        sem = self.sems[key] if isinstance(key, str) else self.dsems[key[1]]
        self.engs[eng].wait_ge(sem, val)
        self.seen[eng][key] = val

    def _deps(self, eng, reads, writes):
        for b in reads:
            if b.w is not None:
                self._wait(eng, *b.w)
        for b in writes:
            if b.w is not None:
                self._wait(eng, *b.w)
            for k, v in b.r.items():
                self._wait(eng, k, v)

    def op(self, eng, fn, r=(), w=()):
        self._deps(eng, r, w)
        ins = fn()
        self.cnt[eng] += 1
        self.nins += 1
        ins.then_inc(self.sems[eng], 1)
        c = self.cnt[eng]
        for b in r:
            b.r[eng] = c
        for b in w:
            b.w = (eng, c)
            b.r = {}
        return ins

    def dma(self, q, out, in_, r=(), w=(), indirect=None, **kw):
        lo, hi = (0, 8) if q == "sp" else (8, len(self.dsems))
        nxt = self.qnext.get(q, lo)
        i = nxt
        self.qnext[q] = lo + (nxt + 1 - lo) % (hi - lo)
        if self.dcnt[i] > 0:
            self._wait(q, ("d", i), self.dcnt[i])
        self._deps(q, r, w)
        only_if = kw.pop("only_if", None)
        if only_if is not None:
            eng = self.engs[q]
            with eng.If(only_if):
                ins = eng.dma_start(out=out, in_=in_, **kw)
                ins.then_inc(self.dsems[i], 16)
            with eng.Else():
                eng.memset(out, 0.0).then_inc(self.dsems[i], 16)
            self.dcnt[i] += 16
            self.nins += 1
        else:
            if indirect is None:
                ins = self.engs[q].dma_start(out=out, in_=in_, **kw)
            else:
                ins = indirect()
            self.dcnt[i] += 16
            self.nins += 1
            ins.then_inc(self.dsems[i], 16)
        key = ("d", i)
        for b in r:
            b.r[key] = self.dcnt[i]
        for b in w:
            b.w = (key, self.dcnt[i])
            b.r = {}
        return ins

    def barrier(self):
        self.marks.append(dict(self.cnt))
        for e in self.engs:
            for k in self.engs:
                if k != e and self.cnt[k] > 0:
                    self._wait(e, k, self.cnt[k])
            for i in range(len(self.dsems)):
                if self.dcnt[i] > 0:
                    self._wait(e, ("d", i), self.dcnt[i])

    def wait_all(self, eng):
        for k in self.engs:
            if k != eng and self.cnt[k] > 0:
                self._wait(eng, k, self.cnt[k])
        for i in range(len(self.dsems)):
            if self.dcnt[i] > 0:
                self._wait(eng, ("d", i), self.dcnt[i])


def host_consts():
    bf = ml_dtypes.bfloat16
    c = {}
    c["ident_bf"] = np.eye(128, dtype=np.float32).astype(bf)
    c["ident_f"] = np.eye(128, dtype=np.float32)
    c["ones_bf"] = np.ones((128, 128), np.float32).astype(bf)
    c["ones_f"] = np.ones((128, 128), np.float32)
    a = np.arange(128)[:, None]
    i = np.arange(128)[None, :]
    masks = []
    masks.append(a >= i)
    masks.append(a <= i)
    masks.append(np.abs(a - i) <= 64)
    masks.append(a >= i + 64)
    masks.append(a <= i - 64)
    m = np.stack([np.where(v, 0.0, NEG) for v in masks], axis=1).astype(np.float32)
    z = np.zeros((128, 128), np.float32)
    m3 = np.stack([np.concatenate([m[:, 0], z, m[:, 1]], axis=1),
                   np.concatenate([m[:, 3], m[:, 2], m[:, 4]], axis=1)], axis=1)
    c["maskb"] = m3.astype(bf)
    R = np.zeros((64, 64), np.float32)
    for j in range(8):
        R[j, j + 8] = -1.0
        R[j + 8, j] = 1.0
    RT = R.T
    rot = np.zeros((128, 128), np.float32)
    rot[:64, :64] = RT
    rot[64:, 64:] = RT
    c["rotT"] = rot.astype(bf)
    inv = np.zeros((128, 1), np.float32)
    for p in range(128):
        q = p % 64
        if q < 16:
            inv[p, 0] = np.float32(500000.0) ** (-np.float32(2 * (q % 8)) / np.float32(16))
    c["invf"] = inv
    s = np.arange(2048, dtype=np.float64)
    ang = 2.0 * np.pi * ((s[:, None] * s[None, :]) % 2048) / 2048.0
    c["dftc"] = (np.cos(ang) / np.sqrt(2048.0)).astype(np.float32).astype(bf)
    c["dfts"] = (np.sin(ang) / np.sqrt(2048.0)).astype(np.float32).astype(bf)
    cc = np.arange(64, dtype=np.float64)
    angc = 2.0 * np.pi * ((cc[:, None] * cc[None, :]) % 64) / 64.0
    C = np.cos(angc) / 8.0
    Sn = np.sin(angc) / 8.0
    cb = np.zeros((128, 128)); sbm = np.zeros((128, 128))
    cb[:64, :64] = C; cb[64:, 64:] = C
    sbm[:64, :64] = -Sn; sbm[64:, 64:] = -Sn
    c["dftcc"] = cb.astype(np.float32).astype(bf)
    c["dftsc"] = sbm.astype(np.float32).astype(bf)
    c["iota_f"] = np.tile(np.arange(2048, dtype=np.float32)[None, :], (128, 1))
    return c


CONST_SPECS = [
    ("ident_bf", [128, 128], BF16), ("ident_f", [128, 128], F32), ("ones_bf", [128, 128], BF16),
    ("ones_f", [128, 128], F32), ("maskb", [128, 2, 384], BF16), ("rotT", [128, 128], BF16),
    ("invf", [128, 1], F32), ("dftc", [2048, 2048], BF16), ("dfts", [2048, 2048], BF16),
    ("dftcc", [128, 128], BF16), ("dftsc", [128, 128], BF16),
]


def build(L=2, dbg=(), stop_after=None, skip=()):
    nc = bass.Bass("TRN2", target_bir_lowering=False)
    dbg = set(dbg)

    def din(name, shape, dt):
        return nc.dram_tensor(name, list(shape), dt, kind="ExternalInput").ap()

    x_in = din("x", [SEQ, D], F32)
    cT_in = din("cT", [128, 16], F32)
    posb_in = din("posb", [128, SEQ], I32)
    ada_w = din("ada_w", [L, D, 6 * D], F32)
    ada_bT = din("ada_bT", [L, 128, 96], F32)
    nmT_in = din("nmT", [L, 128, 16], F32)
    nfT_in = din("nfT", [L, 128, 16], F32)
    gnT_in = din("gnT", [L, 128, 16], F32)
    w_in = din("w_in", [L, D, D_IN], F32)
    fnet_wS = din("fnet_wS", [L, 4, 128, 64], F32)
    sgun_bc = din("sgun_bc", [L, 128, 512], F32)
    sgu_wT = din("sgu_wT", [L, 128, 8, 128], F32)
    sgub_bc = din("sgub_bc", [L, 128, 4, 128], F32)
    sinkT = din("sinkT", [L, 128, 8], F32)
    w_out = din("w_out", [L, D, D], F32)
    router_w = din("router_w", [L, D, N_EXP], F32)
    wg_in = din("exp_w_gate", [L, N_EXP, D, FF], F32)
    wu_in = din("exp_w_up", [L, N_EXP, D, FF], F32)
    wd_in = din("exp_w_down", [L, N_EXP, FF, D], F32)
    fin_bc = din("fin_bc", [128, D], F32)
    cst = {n: din(n, s, dt) for (n, s, dt) in CONST_SPECS}
    out_ap = nc.dram_tensor("out", [SEQ, D], F32, kind="ExternalOutput").ap()
    xres = nc.dram_tensor("xres", [SEQ, D], F32, kind="ExternalOutput" if "xres" in dbg else "Internal").ap()
    ynT_d = nc.dram_tensor("ynT_d", [128, NT, 16, 128], BF16, kind="ExternalOutput" if "ynT" in dbg else "Internal").ap()
    h2_d = nc.dram_tensor("h2_d", [SEQ, D], BF16, kind="Internal").ap()
    dbg_outs = {}

    with contextlib.ExitStack() as st:
        S = Sched(nc, st)

        uid = [0]

        def sbuf(stack, name, shape, dt):
            uid[0] += 1
            return stack.enter_context(nc.sbuf_tensor("sb%d_%s" % (uid[0], name), list(shape), dt))

        def psum(stack, name, shape, dt):
            uid[0] += 1
            return stack.enter_context(nc.psum_tensor("pp%d_%s" % (uid[0], name), list(shape), dt))

        def dump(name, ap, shape, dt, rbuf):
            if name not in dbg:
                return
            o = nc.dram_tensor("dbg_" + name, list(shape), dt, kind="ExternalOutput").ap()
            dbg_outs[name] = o
            S.dma("sp", o, ap, r=rbuf)

        def fin():
            S.wait_all("sp")
            dbg_outs["_marks"] = S.marks
            return nc, dbg_outs

        def mm(out, lhsT, rhs, start, stop, r, w):
            return S.op("pe", lambda: nc.tensor.matmul(out, lhsT, rhs, start=start, stop=stop), r, w)

        def tr(out, in_, ident, r, w):
            return S.op("pe", lambda: nc.tensor.transpose(out, in_, ident), r, w)

        def act(out, in_, func, r, w, eng="act", **kw):
            return S.op("act", lambda: nc.scalar.activation(out=out, in_=in_, func=func, **kw), r, w)

        def ts(out, in0, s1, s2, op0, op1=None, r=(), w=(), eng="dve"):
            e = S.engs[eng]
            if op1 is None:
                return S.op(eng, lambda: e.tensor_scalar(out=out, in0=in0, scalar1=s1, scalar2=None, op0=op0), r, w)
            return S.op(eng, lambda: e.tensor_scalar(out=out, in0=in0, scalar1=s1, scalar2=s2, op0=op0, op1=op1), r, w)

        def tt(out, in0, in1, op, r=(), w=(), eng="dve"):
            e = S.engs[eng]
            return S.op(eng, lambda: e.tensor_tensor(out=out, in0=in0, in1=in1, op=op), r, w)

        def stt(out, in0, scalar, in1, op0, op1, r=(), w=()):
            return S.op("dve", lambda: nc.vector.scalar_tensor_tensor(out=out, in0=in0, scalar=scalar, in1=in1, op0=op0, op1=op1), r, w)

        def cp(out, in_, r=(), w=(), eng="dve"):
            if eng == "act":
                return S.op("act", lambda: nc.scalar.copy(out=out, in_=in_), r, w)
            e = S.engs[eng]
            return S.op(eng, lambda: e.tensor_copy(out=out, in_=in_), r, w)

        def memset(ap, val, w, eng="pool"):
            e = S.engs[eng]
            return S.op(eng, lambda: e.memset(ap, val), (), w)

        def rstd_from_ss(out, ss_ap, inv_n, b):
            ts(out, ss_ap, inv_n, EPS, ALU.mult, ALU.add, r=[b], w=[b])
            act(out, out, AF.Sqrt, r=[b], w=[b])
            S.op("dve", lambda: nc.vector.reciprocal(out=out, in_=out), [b], [b])

        is_even = nc.gpsimd.snap(nc.gpsimd.partition_id() % 2 == 0)

        ident_bf = sbuf(st, "ident_bf", [128, 128], BF16)
        ident_f = sbuf(st, "ident_f", [128, 128], F32)
        ones_bf = sbuf(st, "ones_bf", [128, 128], BF16)
        ones_f = sbuf(st, "ones_f", [128, 128], F32)
        maskb = sbuf(st, "maskb", [128, 2, 384], BF16)
        rotT = sbuf(st, "rotT", [128, 128], BF16)
        invf = sbuf(st, "invf", [128, 1], F32)
        dftcc = sbuf(st, "dftcc", [128, 128], BF16)
        dftsc = sbuf(st, "dftsc", [128, 128], BF16)
        cB = Buf("consts")
        for nm, t_ in (("ident_bf", ident_bf), ("ident_f", ident_f), ("ones_bf", ones_bf), ("ones_f", ones_f),
                       ("maskb", maskb), ("rotT", rotT), ("invf", invf), ("dftcc", dftcc), ("dftsc", dftsc)):
            S.dma("sp", t_[:], cst[nm], w=[cB])
        cosT = sbuf(st, "cosT", [128, SEQ], F32)
        sinT = sbuf(st, "sinT", [128, SEQ], F32)
        ropeB = Buf("rope")
        hT = sbuf(st, "hT", [128, NK, SEQ], BF16)
        hT_b = [Buf("hT%d" % i) for i in range(NT)]
        modT = sbuf(st, "modT", [128, 96], F32)
        modB = Buf("mod")
        gsm = sbuf(st, "gsm", [128, 16], F32)
        gsf = sbuf(st, "gsf", [128, 16], F32)
        small = sbuf(st, "small", [128, 64], F32)
        smallB = Buf("small")
        NWB = 2
        wbuf = [sbuf(st, "wbuf%d" % i, [128, NK * 512], BF16) for i in range(NWB)]
        wbuf_b = [Buf("wbuf%d" % i) for i in range(NWB)]
        wstate = {"i": 0, "bufs": list(zip(wbuf, wbuf_b))}

        def wload(src_ap, ncols, kt=NK):
            i = wstate["i"] % len(wstate["bufs"])
            wstate["i"] = i + 1
            wt_, wb_ = wstate["bufs"][i]
            view = wt_[:, 0:kt * ncols].rearrange("p (k n) -> p k n", n=ncols)
            S.dma("pool", view, src_ap, w=[wb_])
            return view, wb_

        class WStream:
            def __init__(self, bufs):
                self.bufs = bufs
                self.free = list(range(len(bufs)))
                self.pending = []
                self.loaded = {}
                self.extra = {}

            def add_bufs(self, bufs, extra_w):
                for tb in bufs:
                    self.bufs.append(tb)
                    self.free.append(len(self.bufs) - 1)
                    self.extra[len(self.bufs) - 1] = list(extra_w)
                self.pump()

            def request(self, key, src_ap, ncols, kt=NK):
                self.pending.append((key, src_ap, ncols, kt))

            def pump(self):
                while self.pending and self.free:
                    key, src_ap, ncols, kt = self.pending.pop(0)
                    i = self.free.pop(0)
                    t_, b_ = self.bufs[i][0], self.bufs[i][1]
                    extra = self.extra.pop(i, [])
                    view = t_[:, 0:kt * ncols].rearrange("p (k n) -> p k n", n=ncols)
                    S.dma("pool", view, src_ap, w=[b_] + extra, only_if=is_even)
                    self.loaded[key] = (view, b_, i)

            def get(self, key):
                self.pump()
                v, b_, _ = self.loaded[key]
                return v, b_

            def release(self, key):
                _, _, i = self.loaded.pop(key)
                self.free.append(i)
                self.pump()

        def hT_as_wbufs():
            return [(hT[:, 4 * i:4 * i + 4, :].rearrange("p a n -> p (a n)"), Buf("hTw%d" % i)) for i in range(4)]

        ps = [psum(st, "ps%d" % i, [128, 512], F32) for i in range(8)]
        ps_b = [Buf("ps%d" % i) for i in range(8)]
        psb = [ps[6][:].bitcast(BF16), ps[7][:].bitcast(BF16)]
        psb_b = [ps_b[6], ps_b[7]]

        with contextlib.ExitStack() as ph:
            posi = sbuf(ph, "posi", [128, SEQ], I32)
            ang = sbuf(ph, "ang", [128, SEQ], F32)
            tmpa = sbuf(ph, "tmpa", [128, SEQ], F32)
            pB, aB, tB = Buf(), Buf(), Buf()
            S.dma("sp", posi[:], posb_in, w=[pB])
            cp(ang[:], posi[:], r=[pB], w=[aB])
            ts(ang[:], ang[:], invf[:, 0:1], None, ALU.mult, r=[aB, cB], w=[aB])
            ki = sbuf(ph, "ki", [128, SEQ], I32)
            kf = sbuf(ph, "kf", [128, SEQ], F32)
            kB = Buf()

            def sin_of(dst, offset):
                ts(tmpa[:], ang[:], offset, None, ALU.add, r=[aB], w=[tB])
                ts(kf[:], tmpa[:], 1.0 / (2 * math.pi), None, ALU.mult, r=[tB], w=[kB])
                cp(ki[:], kf[:], r=[kB], w=[kB])
                cp(kf[:], ki[:], r=[kB], w=[kB])
                stt(tmpa[:], kf[:], -2 * math.pi, tmpa[:], ALU.mult, ALU.add, r=[kB, tB], w=[tB])
                ts(kf[:], tmpa[:], math.pi, None, ALU.is_gt, r=[tB], w=[kB])
                stt(tmpa[:], kf[:], -2 * math.pi, tmpa[:], ALU.mult, ALU.add, r=[kB, tB], w=[tB])
                ts(kf[:], tmpa[:], -math.pi, None, ALU.is_lt, r=[tB], w=[kB])
                stt(tmpa[:], kf[:], 2 * math.pi, tmpa[:], ALU.mult, ALU.add, r=[kB, tB], w=[tB])
                ts(tmpa[:], tmpa[:], math.pi, -math.pi, ALU.min, ALU.max, r=[tB], w=[tB])
                act(dst, tmpa[:], AF.Sin, r=[tB], w=[ropeB])

            sin_of(sinT[:], 0.0)
            sin_of(cosT[:], 0.5 * math.pi)
            dump("cosT", cosT[:], [128, SEQ], F32, [ropeB])
            dump("sinT", sinT[:], [128, SEQ], F32, [ropeB])
            S.barrier()

        xres_b = [Buf("xres%d" % i) for i in range(NT)]
        xresB = Buf("xres_scatter")
        h2B = Buf("h2_d")

        def bcast_row(ph, dst, src, srcB, dstB):
            dg = [sbuf(ph, "dg%d" % i, [128, 128], F32) for i in range(2)]
            dgB = [Buf(), Buf()]
            for j in range(16):
                i = j % 2
                ts(dg[i][:], ident_f[:, :], src[:, j:j + 1], None, ALU.mult, r=[cB, srcB], w=[dgB[i]])
                mm(ps[j // 4][:, (j % 4) * 128:(j % 4 + 1) * 128], ones_f[:, :], dg[i][:], True, True, r=[cB, dgB[i]], w=[ps_b[j // 4]])
                if j % 4 == 3:
                    cp(dst[:, (j // 4) * 512:(j // 4 + 1) * 512], ps[j // 4][:, :], r=[ps_b[j // 4]], w=[dstB], eng="act")

        for l in range(L):
            x_src = x_in if l == 0 else xres
            with contextlib.ExitStack() as ph:
                cT = sbuf(ph, "cT", [128, 16], F32)
                scb = sbuf(ph, "scb", [128, 16], BF16)
                abT = sbuf(ph, "abT", [128, 96], F32)
                nm = sbuf(ph, "nm", [128, 16], F32)
                nf = sbuf(ph, "nf", [128, 16], F32)
                b1, b2, b3 = Buf(), Buf(), Buf()
                S.dma("sp", cT[:], cT_in, w=[b1])
                S.dma("sp", abT[:], ada_bT[l], w=[b3])
                S.dma("sp", nm[:], nmT_in[l], w=[b3])
                S.dma("sp", nf[:], nfT_in[l], w=[b3])
                act(scb[:], cT[:], AF.Silu, r=[b1], w=[b2])
                aw = ada_w[l].rearrange("(kt p) n -> p kt n", p=128)
                ws = WStream([(wbuf[i][:, :], wbuf_b[i]) for i in range(NWB)] + hT_as_wbufs())
                for cb in range(24):
                    ws.request(cb, aw[:, :, cb * 512:(cb + 1) * 512], 512)
                for cb in range(24):
                    wt, wb = ws.get(cb)
                    for sc in range(4):
                        j = cb * 4 + sc
                        for kt in range(NK):
                            mm(ps[0][:, j:j + 1], wt[:, kt, sc * 128:(sc + 1) * 128], scb[:, kt:kt + 1],
                               kt == 0, kt == NK - 1, r=[wb, b2], w=[ps_b[0]])
                    ws.release(cb)
                tt(modT[:], ps[0][:, 0:96], abT[:], ALU.add, r=[ps_b[0], b3], w=[modB])
                stt(gsm[:], modT[:, 16:32], 1.0, nm[:], ALU.add, ALU.mult, r=[modB, b3], w=[modB])
                stt(gsf[:], modT[:, 64:80], 1.0, nf[:], ALU.add, ALU.mult, r=[modB, b3], w=[modB])
                dump("modT%d" % l, modT[:], [128, 96], F32, [modB])
                S.barrier()
            if stop_after == "mod":
                break

            def norm_to_hT(x_src, gs, shift_off, also_h2=False):
                with contextlib.ExitStack() as ph:
                    xt = [sbuf(ph, "xt%d" % i, [128, D], F32) for i in range(2)]
                    xt_b = [Buf(), Buf()]
                    xn = [sbuf(ph, "xn%d" % i, [128, D], BF16) for i in range(2)]
                    xn_b = [Buf(), Buf()]
                    junk = sbuf(ph, "junk", [128, D], BF16)
                    jB = Buf()
                    ss = sbuf(ph, "ss", [128, 4], F32)
                    ssB = [Buf(), Buf()]
                    def ld_x(k_):
                        S.dma("sp", xt[k_ % 2][:], x_src[k_ * 128:(k_ + 1) * 128, :], r=[xres_b[k_]], w=[xt_b[k_ % 2]])

                    ld_x(0)
                    for t_ in range(NT):
                        i = t_ % 2
                        if t_ + 1 < NT:
                            ld_x(t_ + 1)
                        act(junk[:], xt[i][:], AF.Square, r=[xt_b[i]], w=[jB, ssB[i]], accum_out=ss[:, i:i + 1])
                        rstd_from_ss(ss[:, 2 + i:3 + i], ss[:, i:i + 1], 1.0 / D, ssB[i])
                        ts(xn[i][:], xt[i][:], ss[:, 2 + i:3 + i], None, ALU.mult, r=[xt_b[i], ssB[i]], w=[xn_b[i]])
                        if also_h2:
                            S.dma("sp", h2_d[t_ * 128:(t_ + 1) * 128, :], xn[i][:], r=[xn_b[i]], w=[h2B])
                        for hb in range(2):
                            for jj in range(8):
                                j = hb * 8 + jj
                                tr(psb[hb][:, jj * 128:(jj + 1) * 128], xn[i][:, j * 128:(j + 1) * 128], ident_bf[:, :],
                                   r=[xn_b[i], cB], w=[psb_b[hb]])
                            for jj in range(8):
                                j = hb * 8 + jj
                                ts(hT[:, j, t_ * 128:(t_ + 1) * 128], psb[hb][:, jj * 128:(jj + 1) * 128],
                                   gs[:, j:j + 1], modT[:, shift_off + j:shift_off + j + 1], ALU.mult, ALU.add,
                                   r=[psb_b[hb], modB], w=[hT_b[t_]])
                    S.barrier()

            norm_to_hT(x_src, gsm, 0)
            dump("hT%d" % l, hT[:], [128, NK, SEQ], BF16, hT_b)
            if stop_after == "hT":
                break

            wi = w_in[l].rearrange("(kt p) n -> p kt n", p=128)

            def gn_alloc(ph, N):
                return (sbuf(ph, "gn_sq", [128, 4, N], BF16), sbuf(ph, "gn_rs", [128, N], F32),
                        sbuf(ph, "gn_yn", [128, 4, N], BF16), Buf(), Buf(), Buf())

            def gn_store(ga, ysrc, yB, m, tok0, N, gn):
                sq, rs, yn, b_sq, b_rs, b_yn = ga
                for ch in range(4):
                    act(sq[:, ch, :], ysrc[:, ch, :], AF.Square, r=[yB], w=[b_sq])
                for ch in range(4):
                    mm(ps[5][:, 0:N], ones_bf[:, :], sq[:, ch, :], ch == 0, ch == 3, r=[b_sq, cB], w=[ps_b[5]])
                ts(rs[:], ps[5][:, 0:N], 1.0 / 512, EPS, ALU.mult, ALU.add, r=[ps_b[5]], w=[b_rs])
                act(rs[:], rs[:], AF.Ln, r=[b_rs], w=[b_rs])
                act(rs[:], rs[:], AF.Exp, r=[b_rs], w=[b_rs], scale=-0.5)
                for ch in range(4):
                    stt(yn[:, ch, :], ysrc[:, ch, :], gn[:, 4 * m + ch:4 * m + ch + 1], rs[:], ALU.mult, ALU.mult,
                        r=[yB, b_rs, gnB], w=[b_yn])
                t0 = tok0 // 128
                for ch in range(4):
                    S.dma("sp", ynT_d[:, t0:t0 + N // 128, 4 * m + ch, :],
                          yn[:, ch, :].rearrange("p (a t) -> p a t", t=128), r=[b_yn], w=[ynB])

            def gelu(dst, src_ps, xs, t1, r, w, bx, bt):
                cp(xs, src_ps, r=r, w=[bx], eng="act")
                tt(t1, xs, xs, ALU.mult, r=[bx], w=[bt])
                ts(t1, t1, 0.044715, 1.0, ALU.mult, ALU.add, r=[bt], w=[bt])
                tt(t1, t1, xs, ALU.mult, r=[bt, bx], w=[bt])
                act(t1, t1, AF.Sigmoid, r=[bt], w=[bt], scale=1.5957691216057308)
                tt(dst, t1, xs, ALU.mult, r=[bt, bx], w=w)

            gn_sb = sbuf(st, "gn%d" % l, [128, 16], F32)
            gnB = Buf()
            S.dma("sp", gn_sb[:], gnT_in[l], w=[gnB])
            ynB = Buf("ynT_d")

            if "fnet" not in skip:
              with contextlib.ExitStack() as ph:
                xa = sbuf(ph, "xa", [128, NT, 512], BF16)
                xaB = Buf()
                fw = sbuf(ph, "fw", [128, 4, 64], BF16)
                fwB = Buf()
                AB = sbuf(ph, "AB", [128, 8, 128], BF16)
                ABb = Buf()
                dC = [sbuf(ph, "dC%d" % i, [128, NK, 256], BF16) for i in range(2)]
                dS = [sbuf(ph, "dS%d" % i, [128, NK, 256], BF16) for i in range(2)]
                dB = [Buf(), Buf()]
                pq = sbuf(ph, "pq", [128, 4, 512], BF16)
                pqB = Buf()
                ya = sbuf(ph, "ya", [128, 4, 256], F32)
                yaB = Buf()
                ga = gn_alloc(ph, 256)
                S.dma("pool", fw[:], fnet_wS[l].rearrange("c p d -> p c d"), w=[fwB])
                memset(AB[:], 0.0, [ABb])
                for ch in range(4):
                    for which, tab in ((0, dftcc), (1, dftsc)):
                        mm(ps[4][:, 0:64], tab[:, :], fw[:, ch, :], True, True, r=[fwB, cB], w=[ps_b[4]])
                        cp(AB[0:64, which * 4 + ch, 0:64], ps[4][0:64, 0:64], r=[ps_b[4]], w=[ABb])
                        cp(AB[64:128, which * 4 + ch, 64:128], ps[4][64:128, 0:64], r=[ps_b[4]], w=[ABb])
                wt, wb = wload(wi[:, :, 0:512], 512)
                for t_ in range(NT):
                    pi = t_ % 2
                    for kt in range(NK):
                        mm(ps[pi][:, :], hT[:, kt, t_ * 128:(t_ + 1) * 128], wt[:, kt, :], kt == 0, kt == NK - 1,
                           r=[hT_b[t_], wb], w=[ps_b[pi]])
                    cp(xa[:, t_, :], ps[pi][:, :], r=[ps_b[pi]], w=[xaB], eng="act")
                dcv = cst["dftc"].rearrange("(kt p) n -> p kt n", p=128)
                dsv = cst["dfts"].rearrange("(kt p) n -> p kt n", p=128)
                def ld_tab(k_):
                    S.dma("sp", dC[k_ % 2][:], dcv[:, :, k_ * 256:(k_ + 1) * 256], w=[dB[k_ % 2]])
                    S.dma("sp", dS[k_ % 2][:], dsv[:, :, k_ * 256:(k_ + 1) * 256], w=[dB[k_ % 2]])

                ld_tab(0)
                for sbk in range(8):
                    bi = sbk % 2
                    if sbk + 1 < 8:
                        ld_tab(sbk + 1)
                    for ch in range(4):
                        for which, tab in ((0, dC[bi]), (1, dS[bi])):
                            for kt in range(NK):
                                mm(ps[ch][:, which * 256:(which + 1) * 256], xa[:, kt, ch * 128:(ch + 1) * 128],
                                   tab[:, kt, :], kt == 0, kt == NK - 1, r=[xaB, dB[bi]], w=[ps_b[ch]])
                        cp(pq[:, ch, :], ps[ch][:, :], r=[ps_b[ch]], w=[pqB], eng="act")
                    for ch in range(4):
                        o_ = ps[4][:, (ch % 2) * 256:(ch % 2 + 1) * 256] if ch < 2 else ps[6][:, (ch % 2) * 256:(ch % 2 + 1) * 256]
                        ob = ps_b[4] if ch < 2 else ps_b[6]
                        mm(o_, AB[:, ch, :], pq[:, ch, 0:256], True, False, r=[ABb, pqB], w=[ob])
                        mm(o_, AB[:, 4 + ch, :], pq[:, ch, 256:512], False, True, r=[ABb, pqB], w=[ob])
                        cp(ya[:, ch, :], o_, r=[ob], w=[yaB])
                    gn_store(ga, ya[:], yaB, 0, sbk * 256, 256, gn_sb)
                S.barrier()

            if "gmlp" not in skip:
              with contextlib.ExitStack() as ph:
                sgun = sbuf(ph, "sgun", [128, 512], F32)
                sguw = sbuf(ph, "sguw", [128, 8, 128], BF16)
                sgub = sbuf(ph, "sgub", [128, 4, 128], F32)
                gB = Buf()
                S.dma("sp", sgun[:], sgun_bc[l], w=[gB])
                S.dma("pool", sguw[:], sgu_wT[l], w=[gB])
                S.dma("sp", sgub[:], sgub_bc[l], w=[gB])
                wu, wub = wload(wi[:, :, 512:1024], 512)
                wv, wvb = wload(wi[:, :, 1024:1536], 512)
                uT = sbuf(ph, "uT", [128, 4, 512], F32)
                uB = Buf()
                yb = sbuf(ph, "yb", [128, 4, 512], F32)
                ybB = Buf()
                xs = sbuf(ph, "gxs", [128, 512], F32)
                t1 = sbuf(ph, "gt1", [128, 512], F32)
                bx, bt = Buf(), Buf()
                vs = sbuf(ph, "vs", [128, 512], F32)
                vB = Buf()
                vn = sbuf(ph, "vn", [128, 512], BF16)
                vnB = Buf()
                sv = sbuf(ph, "sv", [128, 2], F32)
                svB = Buf()
                zt = sbuf(ph, "zt", [128, 4, 128], F32)
                ztB = Buf()
                ga = gn_alloc(ph, 512)
                for tb in range(4):
                    for ch in range(4):
                        for kt in range(NK):
                            mm(ps[ch][:, :], wu[:, kt, ch * 128:(ch + 1) * 128], hT[:, kt, tb * 512:(tb + 1) * 512],
                               kt == 0, kt == NK - 1, r=[wub] + hT_b[tb * 4:tb * 4 + 4], w=[ps_b[ch]])
                        act(uT[:, ch, :], ps[ch][:, :], AF.Gelu_apprx_tanh, r=[ps_b[ch]], w=[uB])
                    for pc in range(4):
                        t_ = tb * 4 + pc
                        for kt in range(NK):
                            mm(ps[4][:, :], hT[:, kt, t_ * 128:(t_ + 1) * 128], wv[:, kt, :], kt == 0, kt == NK - 1,
                               r=[wvb, hT_b[t_]], w=[ps_b[4]])
                        act(vs[:], ps[4][:, :], AF.Gelu_apprx_tanh, r=[ps_b[4]], w=[vB])
                        act(t1[:], vs[:], AF.Square, r=[vB], w=[bt, svB], accum_out=sv[:, 0:1])
                        rstd_from_ss(sv[:, 1:2], sv[:, 0:1], 1.0 / 512, svB)
                        stt(vn[:], vs[:], sv[:, 1:2], sgun[:], ALU.mult, ALU.mult, r=[vB, svB, gB], w=[vnB])
                        for h_ in range(8):
                            ch, hh = h_ // 2, h_ % 2
                            mm(ps[5][hh * 64:(hh + 1) * 64, ch * 128:(ch + 1) * 128], vn[:, h_ * 64:(h_ + 1) * 64],
                               sguw[:, h_, :], True, True, r=[vnB, gB], w=[ps_b[5]])
                        tt(zt[:], ps[5][:, :].rearrange("p (c t) -> p c t", t=128), sgub[:], ALU.add, r=[ps_b[5], gB], w=[ztB])
                        tt(yb[:, :, pc * 128:(pc + 1) * 128], zt[:], uT[:, :, pc * 128:(pc + 1) * 128], ALU.mult,
                           r=[ztB, uB], w=[ybB])
                    gn_store(ga, yb[:], ybB, 1, tb * 512, 512, gn_sb)
                S.barrier()
            if stop_after == "mix01":
                break

            def proj_stream(cols):
                subs = [(wbuf[i][:, j * 2048:(j + 1) * 2048], Buf("wsub%d_%d" % (i, j))) for i in range(NWB) for j in range(4)]
                ws_ = WStream(subs)
                for c_ in cols:
                    ws_.request(c_, wi[:, :, c_:c_ + 128], 128)
                ws_.pump()
                return ws_

            def proj_fm(col0, evac=None):
                wt, wb = pstate["ws"].get(col0)

                def proj(tb):
                    rr = [wb] + hT_b[tb * 4:tb * 4 + 4]
                    for kt in range(NK):
                        mm(ps[tb][:, :], wt[:, kt, :], hT[:, kt, tb * 512:(tb + 1) * 512], kt == 0, kt == NK - 1, r=rr, w=[ps_b[tb]])

                proj(0)
                proj(1)
                for tb in range(4):
                    if evac is not None:
                        evac(tb)
                    if tb + 2 < 4:
                        proj(tb + 2)
                pstate["ws"].release(col0)

            def rope_tb(rs_, dst, dstB, placed=None):
                qr, qrB, t1, t1B, t2, t2B = rs_

                def f(tb):
                    blk = slice(tb * 512, (tb + 1) * 512)
                    pj = 4 + tb % 2
                    cp(qr[:], ps[tb][:, :], r=[ps_b[tb]], w=[qrB], eng="act")
                    mm(ps[pj][:, :], rotT[:, :], qr[:], True, True, r=[qrB, cB], w=[ps_b[pj]])
                    cp(t1[:], ps[tb][:, :], r=[ps_b[tb]], w=[t1B], eng="act")
                    cp(t2[:], ps[pj][:, :], r=[ps_b[pj]], w=[t2B], eng="act")
                    tt(t1[:], t1[:], cosT[:, blk], ALU.mult, r=[ropeB], w=[t1B])
                    tt(t2[:], t2[:], sinT[:, blk], ALU.mult, r=[ropeB], w=[t2B], eng="pool")
                    if placed is None:
                        tt(dst[:, blk], t1[:], t2[:], ALU.add, r=[t1B, t2B], w=[dstB])
                    else:
                        for (tl, tlB, sb_, tg_) in placed:
                            tt(tl[tg_:tg_ + 64, blk], t1[sb_:sb_ + 64, :], t2[sb_:sb_ + 64, :], ALU.add,
                               r=[t1B, t2B], w=[tlB])
                return f

            def plain_tb(dst, dstB):
                def f(tb):
                    cp(dst[:, tb * 512:(tb + 1) * 512], ps[tb][:, :], r=[ps_b[tb]], w=[dstB], eng="act")
                return f

            def gslice(r_, m_, d):
                start = r_ + d * 128 * m_
                return slice(start, start + 128) if d == 1 else slice(start, start + d * 127 + 1, d)

            def v_to_tok(vT, vTB, vtok, vtokB, d):
                nper = 16 // d
                for r_ in range(d):
                    for m_ in range(nper):
                        g = r_ * nper + m_
                        bi = (g // 8) % 2
                        o_ = psb[bi][:, (g % 8) * 128:(g % 8 + 1) * 128]
                        tr(o_, vT[:, gslice(r_, m_, d)], ident_bf[:, :], r=[vTB, cB], w=[psb_b[bi]])
                        cp(vtok[:, g, :, 0:64], o_.rearrange("p (h c) -> p h c", c=64), r=[psb_b[bi]], w=[vtokB],
                           eng=("act" if g % 2 else "dve"))

            astate = {"s": 0, "o": 0, "p": 0, "e": 0}
            pstate = {}

            def attend(pts, qT, qB, kT, kB, vtok, vtokB, vh, W, d, acc, accB, first):
                nper = 16 // d
                wi_ = 0 if W == 128 else 1
                tiles = [(r_, m_) for r_ in range(d) for m_ in range(nper)]
                otmp, otmpB = pts[0][2], pts[0][3]

                def emit_S(r_, m_):
                    qs = gslice(r_, m_, d)
                    dls = [dl for dl in (-1, 0, 1) if 0 <= m_ + dl < nper]
                    c0, n = (dls[0] + 1) * 128, len(dls) * 128
                    si = astate["s"] % 4
                    astate["s"] += 1
                    pi = astate["p"] % len(pts)
                    astate["p"] += 1
                    mm(ps[si][:, 0:n], ident_bf[:, :], maskb[:, wi_, c0:c0 + n], True, False, r=[cB], w=[ps_b[si]])
                    for j, dl in enumerate(dls):
                        mm(ps[si][:, j * 128:(j + 1) * 128], kT[:, gslice(r_, m_ + dl, d)], qT[:, qs], False, j == len(dls) - 1,
                           r=[kB, qB], w=[ps_b[si]])
                    act(pts[pi][0][:, 0:n], ps[si][:, 0:n], AF.Exp, r=[ps_b[si]], w=[pts[pi][1]], scale=0.125)
                    return dls, pi

                def emit_PV(ti, oi, r_, m_, dls, pi):
                    o_ = ps[oi][:, ti * 128:(ti + 1) * 128]
                    for j, dl in enumerate(dls):
                        g = r_ * nper + m_ + dl
                        mm(o_, vtok[:, g, vh, :], pts[pi][0][:, j * 128:(j + 1) * 128], j == 0, j == len(dls) - 1,
                           r=[vtokB, pts[pi][1]], w=[ps_b[oi]])

                def evac(grp, oi):
                    n = len(grp) * 128
                    src = ps[oi][:, 0:n]
                    r0, m0 = grp[0]
                    if d == 1:
                        dst = acc[:, m0 * 128:m0 * 128 + n]
                        view = lambda a: a
                    elif d == 4:
                        dst = acc[:, slice(r0, r0 + 4 * (n - 1) + 1, 4)]
                        view = lambda a: a
                    else:
                        dst = acc[:, :].rearrange("p (i r) -> p r i", r=16)[:, r0:r0 + len(grp), :]
                        view = lambda a: a.rearrange("p (t i) -> p t i", i=128)
                    if first:
                        cp(dst, view(src), r=[ps_b[oi]], w=[accB])
                    else:
                        oj = astate["e"] % 2
                        astate["e"] += 1
                        cp(otmp[oj][:, 0:n], src, r=[ps_b[oi]], w=[otmpB[oj]], eng="act")
                        tt(dst, dst, view(otmp[oj][:, 0:n]), ALU.add, r=[otmpB[oj], accB], w=[accB])

                pend = []

                def drain(keep):
                    while len(pend) > keep:
                        pv, ev = pend.pop(0)
                        emit_PV(*pv)
                        if ev is not None:
                            evac(*ev)

                for gi in range(0, len(tiles), 4):
                    grp = tiles[gi:gi + 4]
                    oi = 6 + astate["o"] % 2
                    astate["o"] += 1
                    for ti, (r_, m_) in enumerate(grp):
                        dls, pi = emit_S(r_, m_)
                        pend.append(((ti, oi, r_, m_, dls, pi), (grp, oi) if ti == len(grp) - 1 else None))
                        drain(3)
                drain(0)

            def attn_finish(fs, acc, accB, yall, yallB, chunk, base, esink_col):
                dtmp, rd0, fB = fs
                for tb in range(4):
                    blk = slice(tb * 512, (tb + 1) * 512)
                    if esink_col is not None:
                        ts(dtmp[64:128, :], acc[64:128, blk], esink_col, None, ALU.add, r=[accB, sinkB], w=[fB])
                        act(dtmp[64:128, :], dtmp[64:128, :], AF.Ln, r=[fB], w=[fB])
                    else:
                        act(dtmp[64:128, :], acc[64:128, blk], AF.Ln, r=[accB], w=[fB])
                    act(dtmp[64:128, :], dtmp[64:128, :], AF.Exp, r=[fB], w=[fB], scale=-1.0)
                    cp(rd0[0:64, :], dtmp[64:128, :], r=[fB], w=[fB])
                    tt(yall[base:base + 64, chunk, blk], acc[0:64, blk], rd0[0:64, :], ALU.mult, r=[accB, fB], w=[yallB])

            def attn_alloc(ph):
                d_ = {}
                d_["rs"] = (sbuf(ph, "qr", [128, 512], BF16), Buf(), sbuf(ph, "rt1", [128, 512], F32), Buf(),
                            sbuf(ph, "rt2", [128, 512], F32), Buf())
                otmp = [sbuf(ph, "otmp%d" % i, [128, 512], F32) for i in range(2)]
                otmpB = [Buf(), Buf()]
                d_["pts"] = [(sbuf(ph, "pt%d" % i, [128, 384], BF16), Buf(), otmp, otmpB) for i in range(4)]
                d_["fs"] = (sbuf(ph, "dtmp", [128, 512], F32), sbuf(ph, "rd0", [128, 512], F32), Buf())
                d_["acc"] = sbuf(ph, "acc", [128, SEQ], F32)
                d_["accB"] = Buf()
                d_["yall"] = sbuf(ph, "yall", [128, 4, SEQ], BF16)
                d_["yallB"] = Buf()
                d_["ga"] = gn_alloc(ph, 512)
                d_["qm"] = [sbuf(ph, "qm%d" % i, [128, SEQ], BF16) for i in range(2)]
                d_["qmB"] = [Buf(), Buf()]
                d_["vT"] = sbuf(ph, "vT", [128, SEQ], BF16)
                d_["vTB"] = Buf()
                return d_

            if "swa" not in skip:
              with contextlib.ExitStack() as ph:
                A = attn_alloc(ph)
                kA = sbuf(ph, "kA", [128, SEQ], BF16)
                kAB = Buf()
                vtok = sbuf(ph, "vtok", [128, NT, 2, 128], BF16)
                vtokB = Buf()
                esink = sbuf(ph, "esink", [128, 8], F32)
                sinkB = Buf()
                S.dma("sp", esink[:], sinkT[l], w=[sinkB])
                act(esink[:], esink[:], AF.Exp, r=[sinkB], w=[sinkB])
                memset(vtok[:, :, :, 64:128], 1.0, [vtokB])
                pstate["ws"] = proj_stream([2048, 2176] + [1536 + c_ * 128 for c_ in range(4)])
                if stop_after == "swa_0":
                    return fin()
                proj_fm(2048, rope_tb(A["rs"], kA, kAB))
                if stop_after == "swa_k":
                    return fin()
                proj_fm(2176, plain_tb(A["vT"], A["vTB"]))
                v_to_tok(A["vT"], A["vTB"], vtok, vtokB, 1)
                if stop_after == "swa_v":
                    return fin()
                for chunk in range(4):
                    kv = chunk // 2
                    for hh in range(2):
                        memset(A["qm"][hh][:], 0.0, [A["qmB"][hh]])
                    proj_fm(1536 + chunk * 128,
                            rope_tb(A["rs"], None, None, placed=[(A["qm"][hh], A["qmB"][hh], hh * 64, kv * 64) for hh in range(2)]))
                    for hh in range(2):
                        h_ = chunk * 2 + hh
                        if stop_after == "swa_q":
                            return fin()
                        attend(A["pts"], A["qm"][hh], A["qmB"][hh], kA, kAB, vtok, vtokB, kv, 128, 1, A["acc"], A["accB"], True)
                        if stop_after == "swa_a":
                            return fin()
                        attn_finish(A["fs"], A["acc"], A["accB"], A["yall"], A["yallB"], chunk, hh * 64, esink[64:128, h_:h_ + 1])
                        if stop_after == "swa_f":
                            return fin()
                        if stop_after == "swa_f2" and hh == 1:
                            return fin()
                for tb in range(4):
                    gn_store(A["ga"], A["yall"][:, :, tb * 512:(tb + 1) * 512], A["yallB"], 2, tb * 512, 512, gn_sb)
                S.barrier()

            if "dil" not in skip:
              with contextlib.ExitStack() as ph:
                A = attn_alloc(ph)
                kT = sbuf(ph, "kT", [128, SEQ], BF16)
                kTB = Buf()
                vtok1 = sbuf(ph, "vtokd", [128, NT, 2, 128], BF16)
                vtok1B = Buf()
                acc2 = sbuf(ph, "acc2", [128, SEQ], F32)
                accs = [(A["acc"], A["accB"]), (acc2, Buf())]
                sinkB = None
                memset(vtok1[:, :, :, 64:128], 1.0, [vtok1B])
                pstate["ws"] = proj_stream([c0_ + c_ * 128 for c_ in range(4) for c0_ in (2304, 2816, 3328)])
                for chunk in range(4):
                    if chunk == 0:
                        for hh in range(2):
                            memset(A["qm"][hh][:], 0.0, [A["qmB"][hh]])
                    proj_fm(2304 + chunk * 128,
                            rope_tb(A["rs"], None, None, placed=[(A["qm"][hh], A["qmB"][hh], hh * 64, hh * 64) for hh in range(2)]))
                    proj_fm(2816 + chunk * 128, rope_tb(A["rs"], kT, kTB))
                    proj_fm(3328 + chunk * 128, plain_tb(A["vT"], A["vTB"]))
                    for ci, d_ in enumerate((1, 4, 16)):
                        v_to_tok(A["vT"], A["vTB"], vtok1, vtok1B, d_)
                        for hh in range(2):
                            attend(A["pts"], A["qm"][hh], A["qmB"][hh], kT, kTB, vtok1, vtok1B, hh, 64, d_,
                                   accs[hh][0], accs[hh][1], ci == 0)
                    for hh in range(2):
                        attn_finish(A["fs"], accs[hh][0], accs[hh][1], A["yall"], A["yallB"], chunk, hh * 64, None)
                for tb in range(4):
                    gn_store(A["ga"], A["yall"][:, :, tb * 512:(tb + 1) * 512], A["yallB"], 3, tb * 512, 512, gn_sb)
                S.barrier()
            if stop_after == "mix":
                break

            with contextlib.ExitStack() as ph:
                woB = Buf()
                wo = w_out[l].rearrange("(ec p) d -> p ec d", p=128)
                woQ = [Buf("wo%d" % q4) for q4 in range(4)]
                for q4 in range(4):
                    S.dma("pool", hT[:, q4 * 4:(q4 + 1) * 4, :], wo[:, q4 * 4:(q4 + 1) * 4, :], w=[woQ[q4]])
                gmbc = sbuf(ph, "gmbc", [128, D], F32)
                gmB = Buf()
                bcast_row(ph, gmbc, modT[:, 32:48], modB, gmB)
                ynt = [sbuf(ph, "ynt%d" % i, [128, 16, 128], BF16) for i in range(2)]
                yntB = [Buf(), Buf()]
                xt = [sbuf(ph, "dxt%d" % i, [128, D], F32) for i in range(2)]
                xtB = [Buf(), Buf()]
                tmp = [sbuf(ph, "dtmp%d" % i, [128, 512], F32) for i in range(2)]
                tmpB = [Buf(), Buf()]
                def ld_tile(k_):
                    S.dma("sp", ynt[k_ % 2][:], ynT_d[:, k_], r=[ynB], w=[yntB[k_ % 2]])
                    S.dma("sp", xt[k_ % 2][:], x_src[k_ * 128:(k_ + 1) * 128, :], r=[xres_b[k_]], w=[xtB[k_ % 2]])

                ld_tile(0)
                for t_ in range(NT):
                    i = t_ % 2
                    if t_ + 1 < NT:
                        ld_tile(t_ + 1)
                    for db in range(4):
                        dblk = slice(db * 512, (db + 1) * 512)
                        ti = db % 2
                        for ec in range(16):
                            mm(ps[db][:, :], ynt[i][:, ec, :], hT[:, ec, dblk], ec == 0, ec == 15, r=[yntB[i], woQ[ec // 4]], w=[ps_b[db]])
                        cp(tmp[ti][:], ps[db][:, :], r=[ps_b[db]], w=[tmpB[ti]], eng="act")
                        tt(tmp[ti][:], tmp[ti][:], gmbc[:, dblk], ALU.mult, r=[gmB], w=[tmpB[ti]])
                        tt(xt[i][:, dblk], xt[i][:, dblk], tmp[ti][:], ALU.add, r=[tmpB[ti]], w=[xtB[i]], eng="pool")
                    S.dma("sp", xres[t_ * 128:(t_ + 1) * 128, :], xt[i][:], r=[xtB[i]], w=[xres_b[t_]])
                S.barrier()
            if stop_after == "wout":
                break

            norm_to_hT(xres, gsf, 48, also_h2=True)
            with contextlib.ExitStack() as ph:
                idxT = sbuf(ph, "idxT", [128, 2, 16], U32)
                gateT = sbuf(ph, "gateT", [128, 2, 16], F32)
                rtB = Buf()
                gfbc = sbuf(ph, "gfbc", [128, D], F32)
                gfB = Buf()
                xw = sbuf(ph, "xwbuf", [128, NK * 512], BF16)
                xwB = Buf("xw")
                ws = WStream([(wbuf[i][:, :], wbuf_b[i]) for i in range(NWB)] + [(xw[:, :], xwB)])
                for e in range(N_EXP):
                    wgv = wg_in[l, e].rearrange("(kt p) f -> p kt f", p=128)
                    wuv = wu_in[l, e].rearrange("(kt p) f -> p kt f", p=128)
                    wdv = wd_in[l, e].rearrange("(ft p) d -> p ft d", p=128)
                    for half in range(2):
                        ws.request((e, "g", half), wgv[:, :, half * 512:(half + 1) * 512], 512)
                        ws.request((e, "u", half), wuv[:, :, half * 512:(half + 1) * 512], 512)
                    for dh in range(2):
                        ws.request((e, "d", dh), wdv[:, :, dh * 1024:(dh + 1) * 1024], 1024, 8)
                ws.pump()
                with contextlib.ExitStack() as ph2:
                    bcast_row(ph2, gfbc, modT[:, 80:96], modB, gfB)
                    rw = sbuf(ph2, "rw", [128, NK, N_EXP], BF16)
                    rwB = Buf()
                    S.dma("pool", rw[:], router_w[l].rearrange("(kt p) e -> p kt e", p=128), w=[rwB])
                    affp = sbuf(ph2, "affp", [128, 128], F32)
                    affB = Buf()
                    memset(affp[:], 0.0, [affB])
                    lg = sbuf(ph2, "lg", [128, 16], F32)
                    ex = sbuf(ph2, "ex", [128, 16], F32)
                    st_ = sbuf(ph2, "st_", [128, 4], F32)
                    lgB = Buf()
                    affT = sbuf(ph2, "affT", [128, SEQ], F32)
                    affTB = Buf()
                    for t_ in range(NT):
                        for kt in range(NK):
                            mm(ps[0][:, 0:16], hT[:, kt, t_ * 128:(t_ + 1) * 128], rw[:, kt, :], kt == 0, kt == NK - 1,
                               r=[hT_b[t_], rwB], w=[ps_b[0]])
                        cp(lg[:], ps[0][:, 0:16], r=[ps_b[0]], w=[lgB], eng="act")
                        S.op("dve", lambda: nc.vector.reduce_max(out=st_[:, 0:1], in_=lg[:], axis=AX.X), [lgB], [lgB])
                        ts(st_[:, 1:2], st_[:, 0:1], -1.0, None, ALU.mult, r=[lgB], w=[lgB])
                        act(ex[:], lg[:], AF.Exp, r=[lgB], w=[lgB], bias=st_[:, 1:2], accum_out=st_[:, 2:3])
                        S.op("dve", lambda: nc.vector.reciprocal(out=st_[:, 3:4], in_=st_[:, 2:3]), [lgB], [lgB])
                        ts(affp[:, 0:16], ex[:], st_[:, 3:4], None, ALU.mult, r=[lgB], w=[affB])
                        tr(ps[1][:, 0:128], affp[:, :], ident_f[:, :], r=[affB, cB], w=[ps_b[1]])
                        cp(affT[0:16, t_ * 128:(t_ + 1) * 128], ps[1][0:16, 0:128], r=[ps_b[1]], w=[affTB], eng="act")
                    ws.add_bufs(hT_as_wbufs()[0:3], hT_b)
                    dump("affT%d" % l, affT[0:16, :], [16, SEQ], F32, [affTB])
                    vals = sbuf(ph2, "vals", [128, CAP], F32)
                    idxu = sbuf(ph2, "idxu", [128, CAP], U32)
                    idxf = sbuf(ph2, "idxf", [128, CAP], F32)
                    tkB = Buf()
                    memset(vals[:], 0.0, [tkB])
                    memset(idxf[:], 0.0, [tkB])
                    for it in range(CAP // 8):
                        sl = slice(it * 8, it * 8 + 8)
                        S.op("dve", lambda sl=sl: nc.vector.max(out=vals[0:16, sl], in_=affT[0:16, :]), [affTB], [tkB])
                        S.op("dve", lambda sl=sl: nc.vector.max_index(out=idxu[0:16, sl], in_max=vals[0:16, sl], in_values=affT[0:16, :]),
                             [affTB, tkB], [tkB])
                        S.op("dve", lambda sl=sl: nc.vector.match_replace(out=affT[0:16, :], in_to_replace=vals[0:16, sl],
                                                                     in_values=affT[0:16, :], imm_value=-1.0), [tkB], [affTB])
                    cp(idxf[0:16, :], idxu[0:16, :], r=[tkB], w=[tkB])
                    for hf in range(2):
                        tr(ps[2][:, hf * 128:(hf + 1) * 128], idxf[:, hf * 128:(hf + 1) * 128], ident_f[:, :], r=[tkB, cB], w=[ps_b[2]])
                        tr(ps[3][:, hf * 128:(hf + 1) * 128], vals[:, hf * 128:(hf + 1) * 128], ident_f[:, :], r=[tkB, cB], w=[ps_b[3]])
                        cp(idxT[:, hf, :], ps[2][:, hf * 128:hf * 128 + 16], r=[ps_b[2]], w=[rtB])
                        cp(gateT[:, hf, :], ps[3][:, hf * 128:hf * 128 + 16], r=[ps_b[3]], w=[rtB])
                    dump("idxT%d" % l, idxT[:], [128, 2, 16], U32, [rtB])
                    dump("gateT%d" % l, gateT[:], [128, 2, 16], F32, [rtB])
                    S.barrier()
                if stop_after == "route":
                    return fin()
                xe = [sbuf(ph, "xe%d" % i, [128, D], BF16)[:, :] for i in range(2)] + [hT[:, 12, :], hT[:, 13, :]]
                xeB = [Buf() for _ in range(4)]
                xeT = sbuf(ph, "xeT", [128, NK, CAP], BF16)
                xeTB = Buf()
                gT = sbuf(ph, "gT", [128, 8, CAP], BF16)
                gTB = Buf()
                sa = [sbuf(ph, "sa%d" % i, [128, CAP], F32) for i in range(2)]
                ub = [sbuf(ph, "ub%d" % i, [128, CAP], F32) for i in range(2)]
                saB = [Buf(), Buf()]
                osb = [sbuf(ph, "osb%d" % i, [128, D], F32) for i in range(2)]
                osbB = [Buf(), Buf()]
                ob = [sbuf(ph, "ob%d" % i, [128, 512], F32) for i in range(2)]
                obB = [Buf(), Buf()]

                def gather(e):
                    for hf in range(2):
                        k = (e % 2) * 2 + hf
                        S.dma("pool", None, None, r=[h2B, rtB], w=[xeB[k]],
                              indirect=lambda hf=hf, e=e, k=k: nc.gpsimd.indirect_dma_start(
                                  out=xe[k], out_offset=None, in_=h2_d,
                                  in_offset=bass.IndirectOffsetOnAxis(ap=idxT[:, hf, e:e + 1], axis=0)))

                gather(0)
                ws.pump()
                for e in range(N_EXP):
                    for hf in range(2):
                        k = (e % 2) * 2 + hf
                        for hb in range(2):
                            for jj in range(8):
                                j = hb * 8 + jj
                                tr(psb[hb][:, jj * 128:(jj + 1) * 128], xe[k][:, j * 128:(j + 1) * 128], ident_bf[:, :],
                                   r=[xeB[k], cB], w=[psb_b[hb]])
                            for jj in range(8):
                                j = hb * 8 + jj
                                ts(xeT[:, j, hf * 128:(hf + 1) * 128], psb[hb][:, jj * 128:(jj + 1) * 128],
                                   gsf[:, j:j + 1], modT[:, 48 + j:49 + j], ALU.mult, ALU.add, r=[psb_b[hb], modB], w=[xeTB])
                    if e + 1 < N_EXP:
                        gather(e + 1)
                    for half in range(2):
                        wg_, wgb = ws.get((e, "g", half))
                        wu_, wub = ws.get((e, "u", half))
                        for fl in range(4):
                            fb = half * 4 + fl
                            pi = fb % 2
                            fsl = slice(fl * 128, (fl + 1) * 128)
                            for kt in range(NK):
                                mm(ps[2 * pi][:, 0:CAP], wg_[:, kt, fsl], xeT[:, kt, :], kt == 0, kt == NK - 1, r=[wgb, xeTB], w=[ps_b[2 * pi]])
                            for kt in range(NK):
                                mm(ps[2 * pi + 1][:, 0:CAP], wu_[:, kt, fsl], xeT[:, kt, :], kt == 0, kt == NK - 1, r=[wub, xeTB], w=[ps_b[2 * pi + 1]])
                            act(sa[pi][:], ps[2 * pi][:, 0:CAP], AF.Silu, r=[ps_b[2 * pi]], w=[saB[pi]])
                            tt(gT[:, fb, :], ps[2 * pi + 1][:, 0:CAP], sa[pi][:], ALU.mult, r=[ps_b[2 * pi + 1], saB[pi]], w=[gTB])
                        ws.release((e, "g", half))
                        ws.release((e, "u", half))
                    for dh in range(2):
                        wd_, wdb = ws.get((e, "d", dh))
                        for hf in range(2):
                            for dq in range(2):
                                db = dh * 2 + dq
                                oi = (hf * 2 + dq) % 2
                                for ft in range(8):
                                    mm(ps[4 + oi][:, :], gT[:, ft, hf * 128:(hf + 1) * 128], wd_[:, ft, dq * 512:(dq + 1) * 512],
                                       ft == 0, ft == 7, r=[gTB, wdb], w=[ps_b[4 + oi]])
                                ts(ob[oi][:], ps[4 + oi][:, :], gateT[:, hf, e:e + 1], None, ALU.mult, r=[ps_b[4 + oi], rtB], w=[obB[oi]])
                                tt(osb[hf][:, db * 512:(db + 1) * 512], ob[oi][:], gfbc[:, db * 512:(db + 1) * 512], ALU.mult,
                                   r=[obB[oi], gfB], w=[osbB[hf]])
                        ws.release((e, "d", dh))
                    for hf in range(2):
                        S.dma("pool", None, None, r=[osbB[hf], rtB] + xres_b, w=[xresB],
                              indirect=lambda hf=hf, e=e: nc.gpsimd.indirect_dma_start(
                                  out=xres, out_offset=bass.IndirectOffsetOnAxis(ap=idxT[:, hf, e:e + 1], axis=0),
                                  in_=osb[hf][:], in_offset=None, compute_op=ALU.add))
                S.barrier()
                for t_ in range(NT):
                    xres_b[t_].w = xresB.w
            if stop_after == "moe":
                break

        if stop_after is None:
            with contextlib.ExitStack() as ph:
                fbc = sbuf(ph, "fbc", [128, D], F32)
                fB = Buf()
                S.dma("sp", fbc[:], fin_bc, w=[fB])
                xt = [sbuf(ph, "fxt%d" % i, [128, D], F32) for i in range(2)]
                xtB = [Buf(), Buf()]
                junk = sbuf(ph, "fjunk", [128, D], BF16)
                jB = Buf()
                ss = sbuf(ph, "fss", [128, 4], F32)
                ssB = [Buf(), Buf()]
                def ld_f(k_):
                    S.dma("sp", xt[k_ % 2][:], xres[k_ * 128:(k_ + 1) * 128, :], r=[xres_b[k_]], w=[xtB[k_ % 2]])

                ld_f(0)
                for t_ in range(NT):
                    i = t_ % 2
                    if t_ + 1 < NT:
                        ld_f(t_ + 1)
                    act(junk[:], xt[i][:], AF.Square, r=[xtB[i]], w=[jB, ssB[i]], accum_out=ss[:, i:i + 1])
                    rstd_from_ss(ss[:, 2 + i:3 + i], ss[:, i:i + 1], 1.0 / D, ssB[i])
                    ts(xt[i][:], xt[i][:], ss[:, 2 + i:3 + i], None, ALU.mult, r=[ssB[i]], w=[xtB[i]])
                    tt(xt[i][:], xt[i][:], fbc[:], ALU.mult, r=[fB], w=[xtB[i]], eng="pool")
                    S.dma("sp", out_ap[t_ * 128:(t_ + 1) * 128, :], xt[i][:], r=[xtB[i]])

        return fin()


def prep_shared(inp, L=2):
    f = np.float32
    g = {}
    g["ada_w"] = np.ascontiguousarray(inp["ada_w"][:L], dtype=f)
    g["ada_bT"] = np.ascontiguousarray(inp["ada_b"][:L].reshape(L, 96, 128).transpose(0, 2, 1), dtype=f)
    for k, n in (("nmT", "norm_mix"), ("nfT", "norm_ffn"), ("gnT", "group_norm")):
        g[k] = np.ascontiguousarray(inp[n][:L].reshape(L, 16, 128).transpose(0, 2, 1), dtype=f)
    g["w_in"] = np.ascontiguousarray(inp["w_in"][:L], dtype=f)
    g["fnet_wS"] = np.ascontiguousarray(inp["fnet_w"][:L].reshape(L, 4, 128, 64), dtype=f)
    g["sgun_bc"] = np.ascontiguousarray(np.broadcast_to(inp["sgu_norm"][:L][:, None, :], (L, 128, 512)), dtype=f)
    g["sgu_wT"] = np.ascontiguousarray(inp["sgu_w"][:L].transpose(0, 3, 1, 2), dtype=f)
    sb = inp["sgu_b"][:L]
    sbb = sb.reshape(L, 4, 2, 1, 128)
    sbb = np.broadcast_to(sbb, (L, 4, 2, 64, 128)).reshape(L, 4, 128, 128).transpose(0, 2, 1, 3)
    g["sgub_bc"] = np.ascontiguousarray(sbb, dtype=f)
    g["sinkT"] = np.ascontiguousarray(np.broadcast_to(inp["swa_sink"][:L][:, None, :], (L, 128, 8)), dtype=f)
    g["w_out"] = np.ascontiguousarray(inp["w_out"][:L], dtype=f)
    g["router_w"] = np.ascontiguousarray(inp["router_w"][:L], dtype=f)
    g["exp_w_gate"] = np.ascontiguousarray(inp["exp_w_gate"][:L], dtype=f)
    g["exp_w_up"] = np.ascontiguousarray(inp["exp_w_up"][:L], dtype=f)
    g["exp_w_down"] = np.ascontiguousarray(inp["exp_w_down"][:L], dtype=f)
    g["fin_bc"] = np.ascontiguousarray(np.broadcast_to(inp["final_norm"][None, :], (128, D)), dtype=f)
    g.update(host_consts())
    return g


def prep_core(inp, b):
    m = {}
    m["x"] = np.ascontiguousarray(inp["x"][b], dtype=np.float32)
    m["cT"] = np.ascontiguousarray(inp["c"][b].reshape(16, 128).T, dtype=np.float32)
    m["posb"] = np.ascontiguousarray(np.broadcast_to(inp["positions"][b][None, :], (128, SEQ)), dtype=np.int32)
    return m


def kernel(**inputs):
    inp = {k: np.asarray(v) for k, v in inputs.items()}
    nc, _ = build(L=2)
    shared = prep_shared(inp)
    in_maps = []
    for core in range(8):
        m = dict(shared)
        m.update(prep_core(inp, core // 2))
        in_maps.append(m)
    res = run_bass_kernel_spmd(nc, in_maps, core_ids=list(range(8)))
    out = np.stack([np.asarray(res.results[2 * b]["out"]) for b in range(4)], axis=0)
    return out.astype(np.float32)
```
